# Optimizing a Trainium2 kernel written in Bass

```python
import math
import numpy as np
import jax
import jax.numpy as jnp
from jax import lax

D_MODEL = 1024
BATCH = 8
SEQ = 4096
DEPTH = 1

HEAD_DIM = 64
GRID_W = 64
NA_HEADS = 8
NA_WIN_ROWS = 8
NA_WIN_COLS = 16
NA_QCOL_BLOCK = 16
NA_KCOL_BLOCK = NA_QCOL_BLOCK + NA_WIN_COLS
DIL_CONFIGS = ((128, 1), (512, 4), (2048, 16))
DIL_HEADS_PER_GROUP = 4
DIL_N_GROUPS = len(DIL_CONFIGS)
DIL_QBLOCK = 64
ROT_DIM = HEAD_DIM // 4
ROPE_THETA = 500000.0
NA_WIDTH = NA_HEADS * HEAD_DIM
DIL_WIDTH = DIL_N_GROUPS * DIL_HEADS_PER_GROUP * HEAD_DIM
DIL_OUT_WIDTH = DIL_HEADS_PER_GROUP * HEAD_DIM
IN_WIDTH = 3 * NA_WIDTH + 3 * DIL_WIDTH + 2 * D_MODEL
PEER_HEADS = 8
PEER_NKEYS = 128
PEER_NEXPERTS = PEER_NKEYS * PEER_NKEYS
PEER_QDIM = 256
PEER_TOPK = 16
PEER_CHUNK = 256
PLE_DIM = 256
RMS_EPS = 1e-6

kernel_name = "hybrid_na_dilated_peer_block"


def rms_norm(x, gain):
    x32 = x.astype(jnp.float32)
    y = x32 * lax.rsqrt(jnp.mean(x32 * x32, axis=-1, keepdims=True) + RMS_EPS)
    return (y * gain.astype(jnp.float32)).astype(x.dtype)


def partial_rotary(x, pos):
    half = ROT_DIM // 2
    inv_freq = jnp.power(jnp.float32(ROPE_THETA), -jnp.arange(half, dtype=jnp.float32) * 2.0 / ROT_DIM)
    ang = pos.astype(jnp.float32)[:, None] * inv_freq[None, :]
    cos = jnp.cos(ang)[:, None, :]
    sin = jnp.sin(ang)[:, None, :]
    xr = x[..., :ROT_DIM].astype(jnp.float32)
    x1, x2 = xr[..., :half], xr[..., half:]
    rot = jnp.concatenate([x1 * cos - x2 * sin, x2 * cos + x1 * sin], axis=-1)
    return jnp.concatenate([rot.astype(x.dtype), x[..., ROT_DIM:]], axis=-1)


def _na_static(width):
    ncb = width // NA_QCOL_BLOCK
    qc = np.arange(width).reshape(ncb, NA_QCOL_BLOCK)
    kc_start = np.clip(np.arange(ncb) * NA_QCOL_BLOCK - NA_WIN_COLS // 2, 0, width - NA_KCOL_BLOCK)
    kc = kc_start[:, None] + np.arange(NA_KCOL_BLOCK)[None, :]
    cs = np.clip(qc - NA_WIN_COLS // 2, 0, width - NA_WIN_COLS)
    kcb = kc[:, None, :]
    mask = (kcb >= cs[..., None]) & (kcb < cs[..., None] + NA_WIN_COLS)
    dc_idx = np.clip(kcb - qc[..., None] + NA_WIN_COLS - 1, 0, 2 * NA_WIN_COLS - 2)
    return kc, mask, dc_idx


def neighbourhood_attention(q, k, v, rpb):
    B, S, H, dh = q.shape
    rows = S // GRID_W
    kh = min(NA_WIN_ROWS, rows)
    ncb = GRID_W // NA_QCOL_BLOCK
    kc, mask, dc_idx = _na_static(GRID_W)
    mask_j = jnp.asarray(mask)[None, None, :, :, None, :]
    qg = q.reshape(B, rows, GRID_W, H, dh)
    kg = k.reshape(B, rows, GRID_W, H, dh)
    vg = v.reshape(B, rows, GRID_W, H, dh)
    scale = HEAD_DIM ** -0.5

    def one_row(r):
        rs = jnp.clip(r - kh // 2, 0, rows - kh)
        q_row = lax.dynamic_index_in_dim(qg, r, axis=1, keepdims=False)
        k_rows = lax.dynamic_slice_in_dim(kg, rs, kh, axis=1)
        v_rows = lax.dynamic_slice_in_dim(vg, rs, kh, axis=1)
        q_cb = q_row.reshape(B, ncb, NA_QCOL_BLOCK, H, dh)
        k_cb = jnp.take(k_rows, kc, axis=2)
        v_cb = jnp.take(v_rows, kc, axis=2)
        s = jnp.einsum('bjqhd,bljkhd->bhjqlk', q_cb, k_cb).astype(jnp.float32) * scale
        dr_idx = rs + jnp.arange(kh) - r + NA_WIN_ROWS - 1
        bias = rpb[:, dr_idx][:, :, dc_idx]
        bias = bias.transpose(0, 2, 3, 1, 4).astype(jnp.float32)
        s = jnp.where(mask_j, s + bias[None], -jnp.inf)
        shp = s.shape
        pr = jax.nn.softmax(s.reshape(shp[:4] + (kh * NA_KCOL_BLOCK,)), axis=-1).reshape(shp)
        o = jnp.einsum('bhjqlk,bljkhd->bjqhd', pr.astype(v.dtype), v_cb)
        return o.reshape(B, GRID_W, H, dh)

    out = lax.map(one_row, jnp.arange(rows))
    return out.transpose(1, 0, 2, 3, 4).reshape(B, S, H * dh)


def dilated_window_attention(q, k, v, window, dilation):
    B, S, H, dh = q.shape
    n = window // (2 * dilation)
    L = S // dilation
    qbs = math.gcd(L, DIL_QBLOCK)
    nblk = L // qbs
    kbs = qbs + 2 * n

    def to_res(t):
        return t.reshape(B, L, dilation, H, dh).transpose(0, 2, 1, 3, 4)

    qr, kr, vr = to_res(q), to_res(k), to_res(v)
    pad = ((0, 0), (0, 0), (n, n), (0, 0), (0, 0))
    kp = jnp.pad(kr, pad)
    vp = jnp.pad(vr, pad)
    kidx = (np.arange(nblk) * qbs)[:, None] + np.arange(kbs)[None, :]
    kb = jnp.take(kp, kidx, axis=2)
    vb = jnp.take(vp, kidx, axis=2)
    qb = qr.reshape(B, dilation, nblk, qbs, H, dh)
    s = jnp.einsum('brnqhd,brnkhd->brnhqk', qb, kb).astype(jnp.float32) * (HEAD_DIM ** -0.5)
    qi = np.arange(qbs)[:, None]
    kj = np.arange(kbs)[None, :]
    band = (kj - qi >= 0) & (kj - qi <= 2 * n)
    m_key = kidx - n
    inside = (m_key >= 0) & (m_key < L)
    mask = band[None] & inside[:, None, :]
    s = jnp.where(jnp.asarray(mask)[None, None, :, None, :, :], s, -jnp.inf)
    lse = jax.nn.logsumexp(s, axis=-1, keepdims=True)
    pr = jnp.exp(s - lse).astype(v.dtype)
    o = jnp.einsum('brnhqk,brnkhd->brnqhd', pr, vb)
    o = o.reshape(B, dilation, L, H, dh).transpose(0, 2, 1, 3, 4).reshape(B, S, H, dh)
    lse = lse[..., 0].transpose(0, 1, 2, 4, 3).reshape(B, dilation, L, H)
    lse = lse.transpose(0, 2, 1, 3).reshape(B, S, H)
    return o, lse


def peer_ffn(h, w_query, sub_keys, expert_u, expert_v):
    B, S, D = h.shape
    T = B * S
    chunk = math.gcd(T, PEER_CHUNK)
    half = PEER_QDIM // 2
    hf = h.reshape(T // chunk, chunk, D)
    k1 = sub_keys[0].astype(jnp.float32)
    k2 = sub_keys[1].astype(jnp.float32)

    def run(hc):
        q = (hc @ w_query).reshape(chunk, PEER_HEADS, PEER_QDIM).astype(jnp.float32)
        s1 = jnp.einsum('thd,nd->thn', q[..., :half], k1)
        s2 = jnp.einsum('thd,nd->thn', q[..., half:], k2)
        v1, i1 = lax.top_k(s1, PEER_TOPK)
        v2, i2 = lax.top_k(s2, PEER_TOPK)
        cand = (v1[..., :, None] + v2[..., None, :]).reshape(chunk, PEER_HEADS, PEER_TOPK * PEER_TOPK)
        cidx = (i1[..., :, None] * PEER_NKEYS + i2[..., None, :]).reshape(chunk, PEER_HEADS, PEER_TOPK * PEER_TOPK)
        sc, pos = lax.top_k(cand, PEER_TOPK)
        eidx = jnp.take_along_axis(cidx, pos, axis=-1)
        g = jax.nn.softmax(sc, axis=-1)
        u = jnp.take(expert_u, eidx, axis=0)
        a = jnp.einsum('thkd,td->thk', u, hc).astype(jnp.float32)
        act = (jax.nn.gelu(a, approximate=False) * g).astype(hc.dtype)
        vv = jnp.take(expert_v, eidx, axis=0)
        return jnp.einsum('thk,thkd->td', act, vv)

    return lax.map(run, hf).reshape(B, S, D)


def setup_inputs(seed: int = 0) -> dict:
    key = jax.random.key(seed)
    ks = jax.random.split(key, 20)

    def nrm(k, shape, scale):
        return jax.random.normal(k, shape, jnp.float32) * scale

    def gain(k, shape):
        return 1.0 + 0.05 * jax.random.normal(k, shape, jnp.float32)

    return {
        "x": nrm(ks[0], (BATCH, SEQ, D_MODEL), 1.0),
        "p": nrm(ks[1], (DEPTH, BATCH, SEQ, PLE_DIM), 1.0),
        "norm_mix": gain(ks[2], (DEPTH, D_MODEL)),
        "w_in": nrm(ks[3], (DEPTH, D_MODEL, IN_WIDTH), D_MODEL ** -0.5),
        "qk_norm_na": gain(ks[4], (DEPTH, 2, HEAD_DIM)),
        "na_rel_bias": nrm(ks[5], (DEPTH, NA_HEADS, 2 * NA_WIN_ROWS - 1, 2 * NA_WIN_COLS - 1), 0.2),
        "qk_norm_dil": gain(ks[6], (DEPTH, 2, HEAD_DIM)),
        "w_branch_na": nrm(ks[7], (DEPTH, NA_WIDTH, D_MODEL), NA_WIDTH ** -0.5),
        "w_branch_dil": nrm(ks[8], (DEPTH, DIL_OUT_WIDTH, D_MODEL), DIL_OUT_WIDTH ** -0.5),
        "w_out": nrm(ks[9], (DEPTH, D_MODEL, D_MODEL), D_MODEL ** -0.5),
        "norm_ffn": gain(ks[10], (DEPTH, D_MODEL)),
        "peer_w_query": nrm(ks[11], (DEPTH, D_MODEL, PEER_HEADS * PEER_QDIM), D_MODEL ** -0.5),
        "peer_sub_keys": nrm(ks[12], (DEPTH, 2, PEER_NKEYS, PEER_QDIM // 2), (PEER_QDIM // 2) ** -0.5),
        "peer_expert_u": nrm(ks[13], (DEPTH, PEER_NEXPERTS, D_MODEL), D_MODEL ** -0.5),
        "peer_expert_v": nrm(ks[14], (DEPTH, PEER_NEXPERTS, D_MODEL), PEER_HEADS ** -0.5),
        "norm_ple": gain(ks[15], (DEPTH, D_MODEL)),
        "w_ple_gate": nrm(ks[16], (DEPTH, D_MODEL, D_MODEL), D_MODEL ** -0.5),
        "w_ple": nrm(ks[17], (DEPTH, PLE_DIM, D_MODEL), PLE_DIM ** -0.5),
    }


def reference(x, p, norm_mix, w_in, qk_norm_na, na_rel_bias, qk_norm_dil, w_branch_na, w_branch_dil,
              w_out, norm_ffn, peer_w_query, peer_sub_keys, peer_expert_u, peer_expert_v,
              norm_ple, w_ple_gate, w_ple):
    B, S, D = x.shape
    pos = jnp.arange(S)
    splits = [int(c) for c in np.cumsum([NA_WIDTH] * 3 + [DIL_WIDTH] * 3 + [D_MODEL])]
    n_dil_heads = DIL_N_GROUPS * DIL_HEADS_PER_GROUP
    for i in range(DEPTH):
        h = rms_norm(x, norm_mix[i])
        proj = h @ w_in[i]
        qa, ka, va, qd, kd, vd, gate_na, gate_dil = jnp.split(proj, splits, axis=-1)

        qa = rms_norm(qa.reshape(B, S, NA_HEADS, HEAD_DIM), qk_norm_na[i, 0])
        ka = rms_norm(ka.reshape(B, S, NA_HEADS, HEAD_DIM), qk_norm_na[i, 1])
        va = va.reshape(B, S, NA_HEADS, HEAD_DIM)
        out_na = neighbourhood_attention(qa, ka, va, na_rel_bias[i])

        qd = partial_rotary(rms_norm(qd.reshape(B, S, n_dil_heads, HEAD_DIM), qk_norm_dil[i, 0]), pos)
        kd = partial_rotary(rms_norm(kd.reshape(B, S, n_dil_heads, HEAD_DIM), qk_norm_dil[i, 1]), pos)
        qd = qd.reshape(B, S, DIL_N_GROUPS, DIL_HEADS_PER_GROUP, HEAD_DIM)
        kd = kd.reshape(B, S, DIL_N_GROUPS, DIL_HEADS_PER_GROUP, HEAD_DIM)
        vd = vd.reshape(B, S, DIL_N_GROUPS, DIL_HEADS_PER_GROUP, HEAD_DIM)
        outs, lses = [], []
        for g, (window, dilation) in enumerate(DIL_CONFIGS):
            o_g, lse_g = dilated_window_attention(qd[:, :, g], kd[:, :, g], vd[:, :, g], window, dilation)
            outs.append(o_g)
            lses.append(lse_g)
        wts = jax.nn.softmax(jnp.stack(lses, axis=2), axis=2)
        out_dil = jnp.sum(wts[..., None] * jnp.stack(outs, axis=2).astype(jnp.float32), axis=2)
        out_dil = out_dil.astype(x.dtype).reshape(B, S, DIL_OUT_WIDTH)

        merged = (jax.nn.sigmoid(gate_na) * (out_na @ w_branch_na[i])
                  + jax.nn.sigmoid(gate_dil) * (out_dil @ w_branch_dil[i]))
        x = x + merged @ w_out[i]

        h = rms_norm(x, norm_ffn[i])
        x = x + peer_ffn(h, peer_w_query[i], peer_sub_keys[i], peer_expert_u[i], peer_expert_v[i])

        h = rms_norm(x, norm_ple[i])
        x = x + jax.nn.sigmoid(h @ w_ple_gate[i]) * (p[i] @ w_ple[i])
    return x
```

```python
import os
import numpy as np
import ml_dtypes
from contextlib import ExitStack
import concourse.bass as bass
import concourse.mybir as mybir
from concourse.bass_utils import run_bass_kernel_spmd

F32 = mybir.dt.float32
BF16 = mybir.dt.bfloat16
I32 = mybir.dt.int32
U32 = mybir.dt.uint32
ALU = mybir.AluOpType
AF = mybir.ActivationFunctionType
AX = mybir.AxisListType

ENGS = ("pe", "act", "dve", "pool", "sp")

S = 4096
D = 1024
NT = S // 128
INW = 5888
QKVW = 3840
EPS = 1e-6


class Buf:
    __slots__ = ("name", "writers", "readers")

    def __init__(self, name=""):
        self.name = name
        self.writers = {}
        self.readers = {}


class Lane:
    __slots__ = ("key", "count")

    def __init__(self, key):
        self.key = key
        self.count = 0


def _upd(d, s):
    for k, v in s.items():
        if d.get(k, 0) < v:
            d[k] = v


class Prog:
    def __init__(self, nc):
        self.nc = nc
        self.cnt = {e: 0 for e in ENGS}
        self.seen = {e: {} for e in ENGS}
        self.sems = {}
        self.lanes = {}
        self.ops = {e: [] for e in ENGS}
        self.nops = 0
        for e in ENGS:
            self._sem(e)

    def _sem(self, key):
        if key not in self.sems:
            self.sems[key] = self.nc.alloc_semaphore("s_" + key)
        return self.sems[key]

    def lane(self, name):
        if name not in self.lanes:
            self.lanes[name] = Lane("L_" + name)
            self._sem("L_" + name)
        return self.lanes[name]

    def op(self, eng, fn, reads=(), writes=(), pwrites=(), sig=True, lane=None, after=()):
        deps = {}
        for b in after:
            _upd(deps, b.writers)
        for b in reads:
            _upd(deps, b.writers)
        for b in writes:
            _upd(deps, b.readers)
            _upd(deps, b.writers)
        for b in pwrites:
            _upd(deps, b.readers)
        waits = []
        seen = self.seen[eng]
        for k, v in deps.items():
            if k == "pe" and eng == "pe":
                continue
            if seen.get(k, 0) >= v:
                continue
            seen[k] = v
            waits.append((k, v))
        if lane is not None:
            lane.count += 16
            mykey, myval, inc = lane.key, lane.count, 16
        else:
            if sig:
                self.cnt[eng] += 1
                myval = self.cnt[eng]
                inc = 1
            else:
                myval = self.cnt[eng] + 1
                inc = 0
            mykey = eng
        for b in reads:
            if b.readers.get(mykey, 0) < myval:
                b.readers[mykey] = myval
        for b in writes:
            b.writers = {mykey: myval}
            b.readers = {}
        for b in pwrites:
            if b.writers.get(mykey, 0) < myval:
                b.writers[mykey] = myval
        self.ops[eng].append((waits, fn, mykey, inc))
        self.nops += 1

    def pe(self, fn, **kw):
        self.op("pe", fn, **kw)

    def act(self, fn, **kw):
        self.op("act", fn, **kw)

    def dve(self, fn, **kw):
        self.op("dve", fn, **kw)

    def pool(self, fn, **kw):
        self.op("pool", fn, **kw)

    def dma(self, q, lane, fn, **kw):
        self.op(q, fn, lane=lane, **kw)

    def flush(self):
        nc = self.nc
        ops = self.ops
        sems = self.sems

        def emit(engobj, lst):
            for waits, fn, mykey, inc in lst:
                for k, v in waits:
                    engobj.wait_ge(sems[k], v)
                if fn is None:
                    continue
                inst = fn(engobj)
                if inc:
                    inst.then_inc(sems[mykey], inc)

        with nc.Block() as block:
            @block.tensor
            def _(e):
                emit(e, ops["pe"])

            @block.scalar
            def _(e):
                emit(e, ops["act"])

            @block.vector
            def _(e):
                emit(e, ops["dve"])

            @block.gpsimd
            def _(e):
                emit(e, ops["pool"])

            @block.sync
            def _(e):
                emit(e, ops["sp"])
        self.ops = {e: [] for e in ENGS}

    def wait_all(self, eng, bufs):
        self.op(eng, None, reads=bufs, sig=False)

    def drain(self, eng="sp"):
        b = Buf("drain")
        for ln in self.lanes.values():
            if ln.count:
                b.writers[ln.key] = ln.count
        for e in ENGS:
            if self.cnt[e]:
                b.writers[e] = self.cnt[e]
        self.op(eng, None, reads=[b], sig=False)


class Ring:
    def __init__(self, es, nc, name, shape, dt, n, psum=False):
        self.t = []
        self.b = []
        for i in range(n):
            if psum:
                t = es.enter_context(nc.psum_tensor(f"{name}{i}", shape, dt))
            else:
                t = es.enter_context(nc.sbuf_tensor(f"{name}{i}", shape, dt))
            self.t.append(t)
            self.b.append(Buf(f"{name}{i}"))
        self.i = -1
        self.n = n

    def next(self):
        self.i = (self.i + 1) % self.n
        return self.t[self.i], self.b[self.i], self.i


def phase_a(nc, p, T):
    x, w_in = T["x"], T["w_in"]
    qkv_s, sg_s = T["qkv_s"], T["sg_s"]
    with ExitStack() as es:
        def sb(name, shape, dt):
            return es.enter_context(nc.sbuf_tensor(name, shape, dt))

        wb = sb("a_wb", [128, 8, INW], BF16)
        b_wb = Buf("wb")
        ident = sb("a_ident", [128, 128], BF16)
        gmix = sb("a_gmix", [128, D], F32)
        gfull = sb("a_gfull", [128, 3072], F32)
        cs = sb("a_cs", [128, NT, 16], F32)
        b_id = b_gmix = b_gfull = b_cs = Buf("a_const")
        stage = Ring(es, nc, "a_stage", [128, 1472], F32, 3)
        xr = Ring(es, nc, "a_x", [128, D], F32, 2)
        junk = Ring(es, nc, "a_junk", [128, D], BF16, 1)
        st = Ring(es, nc, "a_st", [128, 8], F32, 2)
        hb = Ring(es, nc, "a_hb", [128, D], BF16, 2)
        hT = Ring(es, nc, "a_hT", [128, 8, 128], BF16, 2)
        sq = Ring(es, nc, "a_sq", [128, 512], F32, 2)
        qst = Ring(es, nc, "a_qst", [128, 24], F32, 3)
        qn = Ring(es, nc, "a_qn", [128, 512], F32, 2)
        qg = Ring(es, nc, "a_qg", [128, 512], F32, 2)
        rt = Ring(es, nc, "a_rt", [128, 4, 8, 8], F32, 2)
        qo = Ring(es, nc, "a_qo", [128, QKVW], BF16, 2)
        so = Ring(es, nc, "a_so", [128, 2048], BF16, 2)
        ptr = Ring(es, nc, "a_ptr", [128, 8, 128], BF16, 2, psum=True)
        pmm = Ring(es, nc, "a_pmm", [128, 512], F32, 6, psum=True)
        L = p.lane

        p.dma("sp", L("c0"), lambda e: e.dma_start(out=ident[:], in_=T["ident"]), pwrites=[b_id])
        p.dma("sp", L("c0"), lambda e: e.dma_start(out=gmix[:], in_=T["norm_mix"].partition_broadcast(128)), pwrites=[b_gmix])
        p.dma("sp", L("c0"), lambda e: e.dma_start(out=cs[:], in_=T["cs"].rearrange("(n p) c -> p n c", p=128)), pwrites=[b_cs])
        first = False
        for (c0, nh, src) in ((0, 8, T["qk_norm_na"][0, 0:1, :]), (512, 8, T["qk_norm_na"][0, 1:2, :]),
                              (1536, 12, T["qk_norm_dil"][0, 0:1, :]), (2304, 12, T["qk_norm_dil"][0, 1:2, :])):
            for j in range(nh):
                cc = c0 + 64 * j
                if first:
                    p.dma("sp", L("c0"), lambda e, cc=cc, src=src: e.dma_start(out=gfull[:, cc:cc + 64], in_=src.partition_broadcast(128)), writes=[b_gfull])
                    first = False
                else:
                    p.dma("sp", L("c0"), lambda e, cc=cc, src=src: e.dma_start(out=gfull[:, cc:cc + 64], in_=src.partition_broadcast(128)), pwrites=[b_gfull])
        casters = ("dve", "pool", "act")
        n = 0
        for k in range(8):
            for q in range(4):
                stt, sbf, si = stage.next()
                p.dma("sp", L(f"stg{si}"), lambda e, stt=stt, k=k, q=q: e.dma_start(out=stt[:], in_=w_in[128 * k:128 * k + 128, 1472 * q:1472 * q + 1472]), writes=[sbf])
                eng = casters[n % 3]
                n += 1
                if eng == "act":
                    p.act(lambda e, stt=stt, k=k, q=q: e.activation(out=wb[:, k, 1472 * q:1472 * q + 1472], in_=stt[:], func=AF.Copy), reads=[sbf], pwrites=[b_wb])
                else:
                    p.op(eng, lambda e, stt=stt, k=k, q=q: e.tensor_copy(out=wb[:, k, 1472 * q:1472 * q + 1472], in_=stt[:]), reads=[sbf], pwrites=[b_wb])

        def load_x(i):
            xt, xb, xi = xr.next()
            p.dma("sp", L(f"ax{xi}"), lambda e: e.dma_start(out=xt[:], in_=x[128 * i:128 * i + 128, :]), writes=[xb])
            return xt, xb

        nxt = load_x(0)
        for i in range(NT):
            xt, xb = nxt
            if i + 1 < NT:
                nxt = load_x(i + 1)
            jt, jb, _ = junk.next()
            stt, stb, _ = st.next()
            p.act(lambda e, jt=jt, xt=xt, stt=stt: e.activation(out=jt[:], in_=xt[:], func=AF.Square, accum_out=stt[:, 0:1]), reads=[xb], writes=[jb, stb])
            p.act(lambda e, stt=stt: e.activation(out=stt[:, 1:2], in_=stt[:, 0:1], func=AF.Sqrt, scale=1.0 / D, bias=EPS), reads=[stb], pwrites=[stb])
            p.dve(lambda e, stt=stt: e.reciprocal(out=stt[:, 2:3], in_=stt[:, 1:2]), reads=[stb], pwrites=[stb])
            hbt, hbb, _ = hb.next()
            p.dve(lambda e, hbt=hbt, xt=xt, stt=stt: e.scalar_tensor_tensor(out=hbt[:], in0=xt[:], scalar=stt[:, 2:3], in1=gmix[:], op0=ALU.mult, op1=ALU.mult), reads=[xb, stb, b_gmix], writes=[hbb])
            pt, ptb, _ = ptr.next()
            for k in range(8):
                if k == 0:
                    p.pe(lambda e, pt=pt, hbt=hbt, k=k: e.transpose(out=pt[:, k, :], in_=hbt[:, 128 * k:128 * k + 128], identity=ident[:]), reads=[hbb, b_id], writes=[ptb], sig=False)
                else:
                    p.pe(lambda e, pt=pt, hbt=hbt, k=k: e.transpose(out=pt[:, k, :], in_=hbt[:, 128 * k:128 * k + 128], identity=ident[:]), reads=[hbb, b_id], pwrites=[ptb], sig=(k == 7))
            hTt, hTb, _ = hT.next()
            p.act(lambda e, hTt=hTt, pt=pt: e.activation(out=hTt[:], in_=pt[:], func=AF.Copy), reads=[ptb], writes=[hTb])
            qot, qob, qoi = qo.next()
            sot, sob, soi = so.next()
            first_q = True
            first_s = True
            for blk in range(12):
                c0 = 512 * blk
                w = 256 if blk == 7 else 512
                if blk >= 8:
                    c0 = QKVW + 512 * (blk - 8)
                pm, pmb, _ = pmm.next()
                for k in range(8):
                    if k == 0:
                        p.pe(lambda e, pm=pm, hTt=hTt, k=k, c0=c0, w=w: e.matmul(pm[:, 0:w], lhsT=hTt[:, k, :], rhs=wb[:, k, c0:c0 + w], start=True, stop=False), reads=[hTb, b_wb], writes=[pmb], sig=False)
                    else:
                        p.pe(lambda e, pm=pm, hTt=hTt, k=k, c0=c0, w=w: e.matmul(pm[:, 0:w], lhsT=hTt[:, k, :], rhs=wb[:, k, c0:c0 + w], start=False, stop=(k == 7)), reads=[hTb, b_wb], pwrites=[pmb], sig=(k == 7))
                qkw = dict(pwrites=[qob])
                if blk in (0, 1, 3, 4, 5):
                    sqt, sqb, _ = sq.next()
                    qs, qsb, _ = qst.next()
                    p.act(lambda e, sqt=sqt, pm=pm: e.activation(out=sqt[:], in_=pm[:], func=AF.Square), reads=[pmb], writes=[sqb])
                    p.dve(lambda e, qs=qs, sqt=sqt: e.tensor_reduce(out=qs[:, 0:8], in_=sqt[:].rearrange("p (a b) -> p a b", b=64), axis=AX.X, op=ALU.add), reads=[sqb], writes=[qsb])
                    p.act(lambda e, qs=qs: e.activation(out=qs[:, 8:16], in_=qs[:, 0:8], func=AF.Sqrt, scale=1.0 / 64, bias=EPS), reads=[qsb], pwrites=[qsb])
                    p.dve(lambda e, qs=qs: e.reciprocal(out=qs[:, 16:24], in_=qs[:, 8:16]), reads=[qsb], pwrites=[qsb])
                    qnt, qnb, _ = qn.next()
                    p.dve(lambda e, qnt=qnt, pm=pm, qs=qs: e.tensor_tensor(out=qnt[:].rearrange("p (a b) -> p a b", b=64), in0=pm[:].rearrange("p (a b) -> p a b", b=64), in1=qs[:, 16:24].unsqueeze(2).to_broadcast([128, 8, 64]), op=ALU.mult), reads=[pmb, qsb], writes=[qnb])
                    if blk < 2:
                        p.dve(lambda e, qot=qot, qnt=qnt, c0=c0: e.tensor_tensor(out=qot[:, c0:c0 + 512], in0=qnt[:], in1=gfull[:, c0:c0 + 512], op=ALU.mult), reads=[qnb, b_gfull], **qkw)
                    else:
                        qgt, qgb, _ = qg.next()
                        p.dve(lambda e, qgt=qgt, qnt=qnt, c0=c0: e.tensor_tensor(out=qgt[:], in0=qnt[:], in1=gfull[:, c0:c0 + 512], op=ALU.mult), reads=[qnb, b_gfull], writes=[qgb])
                        q3 = qgt[:].rearrange("p (a b) -> p a b", b=64)
                        o3 = qot[:, c0:c0 + 512].rearrange("p (a b) -> p a b", b=64)
                        rtt, rtb, _ = rt.next()
                        cosb = cs[:, i, 0:8].unsqueeze(1).to_broadcast([128, 8, 8])
                        sinb = cs[:, i, 8:16].unsqueeze(1).to_broadcast([128, 8, 8])
                        p.pool(lambda e, rtt=rtt, q3=q3, cosb=cosb: e.tensor_tensor(out=rtt[:, 0], in0=q3[:, :, 0:8], in1=cosb, op=ALU.mult), reads=[qgb, b_cs], writes=[rtb])
                        p.pool(lambda e, rtt=rtt, q3=q3, sinb=sinb: e.tensor_tensor(out=rtt[:, 1], in0=q3[:, :, 8:16], in1=sinb, op=ALU.mult), reads=[qgb, b_cs], pwrites=[rtb])
                        p.pool(lambda e, rtt=rtt, q3=q3, cosb=cosb: e.tensor_tensor(out=rtt[:, 2], in0=q3[:, :, 8:16], in1=cosb, op=ALU.mult), reads=[qgb, b_cs], pwrites=[rtb])
                        p.pool(lambda e, rtt=rtt, q3=q3, sinb=sinb: e.tensor_tensor(out=rtt[:, 3], in0=q3[:, :, 0:8], in1=sinb, op=ALU.mult), reads=[qgb, b_cs], pwrites=[rtb])
                        p.pool(lambda e, rtt=rtt, o3=o3: e.tensor_tensor(out=o3[:, :, 0:8], in0=rtt[:, 0], in1=rtt[:, 1], op=ALU.subtract), reads=[rtb], **qkw)
                        p.pool(lambda e, rtt=rtt, o3=o3: e.tensor_tensor(out=o3[:, :, 8:16], in0=rtt[:, 2], in1=rtt[:, 3], op=ALU.add), reads=[rtb], pwrites=[qob])
                        p.pool(lambda e, q3=q3, o3=o3: e.tensor_copy(out=o3[:, :, 16:64], in_=q3[:, :, 16:64]), reads=[qgb], pwrites=[qob])
                    first_q = False
                elif blk in (2, 6, 7):
                    p.act(lambda e, qot=qot, pm=pm, c0=c0, w=w: e.activation(out=qot[:, c0:c0 + w], in_=pm[:, 0:w], func=AF.Copy), reads=[pmb], **qkw)
                    first_q = False
                else:
                    g0 = 512 * (blk - 8)
                    skw = dict(pwrites=[sob])
                    first_s = False
                    p.act(lambda e, sot=sot, pm=pm, g0=g0: e.activation(out=sot[:, g0:g0 + 512], in_=pm[:], func=AF.Sigmoid), reads=[pmb], **skw)
            p.dma("pool", L(f"aq{qoi}"), lambda e, qot=qot, i=i: e.dma_start(out=qkv_s[128 * i:128 * i + 128, :], in_=qot[:]), reads=[qob], pwrites=[T["b_qkv"]])
            p.dma("pool", L(f"as{soi}"), lambda e, sot=sot, i=i: e.dma_start(out=sg_s[128 * i:128 * i + 128, :], in_=sot[:]), reads=[sob], pwrites=[T["b_sg"]])
        p.drain("sp")
        p.flush()


def phase_b(nc, p, T):
    qkv_s = T["qkv_s"]
    L = p.lane
    with ExitStack() as es:
        def sb(name, shape, dt):
            return es.enter_context(nc.sbuf_tensor(name, shape, dt))

        ident = sb("b_ident", [128, 128], BF16)
        maskd = sb("b_maskd", [128, 3, 256], BF16)
        cmask = sb("b_cmask", [128, 64], F32)
        eb2 = sb("b_eb2", [128, 8, 14, 64], BF16)
        b_const = Buf("b_const")
        b_eb2 = Buf("eb2")
        qtok = sb("b_qtok", [128, NT, 256], BF16)
        ktok = sb("b_ktok", [128, NT, 256], BF16)
        b_qtok = Buf("qtok")
        b_ktok = Buf("ktok")
        qT = sb("b_qT", [128, 2, S], BF16)
        kT = sb("b_kT", [128, 2, S + 128], BF16)
        b_qT = Buf("qT")
        b_kT = Buf("kT")
        va = sb("b_va", [128, NT, 4, 65], BF16)
        vbr = [(sb("b_vb0", [128, NT + 16, 4, 65], BF16), Buf("vb0")), (sb("b_vb1", [128, NT + 16, 4, 65], BF16), Buf("vb1"))]
        b_va = Buf("va")
        bstage = Ring(es, nc, "b_bst", [128, 14, 64], F32, 2)
        er = Ring(es, nc, "b_e", [128, 256], BF16, 3)
        ptr_ = Ring(es, nc, "b_pt", [128, 256], BF16, 6)
        oev = Ring(es, nc, "b_oev", [128, 260], F32, 2)
        orec = Ring(es, nc, "b_orec", [64, 4], F32, 2)
        ona = Ring(es, nc, "b_ona", [64, 4, 64], BF16, 2)
        ptp = Ring(es, nc, "b_ptp", [128, 8, 128], BF16, 2, psum=True)
        pss = Ring(es, nc, "b_pss", [128, 512], F32, 4, psum=True)
        pso_ = Ring(es, nc, "b_pso", [128, 512], F32, 2, psum=True)

        class _PSO:
            def next(self):
                t, b, i = pso_.next()
                return t[:, 0:260].rearrange("p (a b) -> p a b", b=65), b, i
        pso = _PSO()

        p.dma("sp", L("c1"), lambda e: e.dma_start(out=ident[:], in_=T["ident"]), pwrites=[b_const])
        p.dma("sp", L("c1"), lambda e: e.dma_start(out=maskd[:], in_=T["maskd"]), pwrites=[b_const])
        p.dma("sp", L("c1"), lambda e: e.dma_start(out=cmask[:], in_=T["cmask"]), pwrites=[b_const])
        for h in range(8):
            bt, bb, bi = bstage.next()
            p.dma("sp", L(f"bst{bi}"), lambda e, bt=bt, h=h: e.dma_start(out=bt[:], in_=T["biasx"][:, h]), writes=[bb])
            p.act(lambda e, bt=bt: e.activation(out=bt[:], in_=bt[:], func=AF.Exp), reads=[bb], writes=[bb])
            p.dve(lambda e, bt=bt, h=h: e.tensor_tensor(out=eb2[:, h], in0=bt[:], in1=cmask[:].unsqueeze(1).to_broadcast([128, 14, 64]), op=ALU.mult), reads=[bb, b_const], pwrites=[b_eb2])
        p.pool(lambda e: e.memset(kT[:, :, 0:64], 0.0), pwrites=[b_kT])
        p.pool(lambda e: e.memset(kT[:, :, S + 64:S + 128], 0.0), pwrites=[b_kT])

        groups = [("na", 0), ("dil", 0), ("na", 1), ("dil", 1), ("dil", 2)]
        if os.environ.get("KB_STOP") == "const":
            groups = []
        if os.environ.get("KB_GROUPS"):
            groups = [groups[int(c)] for c in os.environ["KB_GROUPS"]]
        tgl = [0]
        pgen = p_chunks(nc, p, T, es)
        def gparams(gi):
            kind, gx = groups[gi]
            if kind == "na":
                d = 1
                qc0, kc0, vc0 = 256 * gx, 512 + 256 * gx, 1024 + 256 * gx
            else:
                d = (1, 4, 16)[gx]
                qc0, kc0, vc0 = 1536 + 256 * gx, 2304 + 256 * gx, 3072 + 256 * gx
            Lr = S // d
            nseg = Lr // 128
            vb, b_vb = vbr[gi % 2]
            lvb = L(f"bvb{gi % 2}")
            view = qkv_s.rearrange("(m r) c -> r m c", r=d)
            return kind, gx, d, qc0, kc0, vc0, Lr, nseg, vb, b_vb, lvb, view

        def load_v(gi):
            kind, gx, d, qc0, kc0, vc0, Lr, nseg, vb, b_vb, lvb, view = gparams(gi)
            p.pool(lambda e, vb=vb: e.memset(vb[:], 0.0), writes=[b_vb])
            p.pool(lambda e, vb=vb: e.memset(vb[:, :, :, 64:65], 1.0), after=[b_vb], pwrites=[b_vb])
            if kind == "na":
                p.pool(lambda e: e.memset(va[:, :, :, 64:65], 1.0), writes=[b_va])
                for hh in range(4):
                    p.dma("sp", lvb, lambda e, hh=hh, vc0=vc0, vb=vb: e.dma_start(out=vb[:, 0:NT - 1, hh, 0:64], in_=qkv_s[64:S - 64, vc0 + 64 * hh:vc0 + 64 * hh + 64].rearrange("(n p) c -> p n c", p=128)), reads=[T["b_qkv"]], after=[b_vb], pwrites=[b_vb])
                for hh in range(4):
                    p.dma("sp", L("bva"), lambda e, hh=hh, vc0=vc0: e.dma_start(out=va[:, :, hh, 0:64], in_=qkv_s[:, vc0 + 64 * hh:vc0 + 64 * hh + 64].rearrange("(n p) c -> p n c", p=128)), reads=[T["b_qkv"]], after=[b_va], pwrites=[b_va])
            else:
                for r in range(d):
                    base = r * (nseg + 1)
                    c = vc0
                    if nseg - 1 >= 4:
                        for hh in range(4):
                            p.dma("sp", lvb, lambda e, r=r, base=base, c=c, hh=hh, view=view, Lr=Lr, nseg=nseg, vb=vb: e.dma_start(out=vb[:, base + 1:base + nseg, hh, 0:64], in_=view[r, 64:Lr - 64, c + 64 * hh:c + 64 * hh + 64].rearrange("(n p) c -> p n c", p=128)), reads=[T["b_qkv"]], after=[b_vb], pwrites=[b_vb])
                    else:
                        for n_ in range(nseg - 1):
                            p.dma("sp", lvb, lambda e, r=r, base=base, c=c, n_=n_, view=view, vb=vb: e.dma_start(out=vb[:, base + 1 + n_, :, 0:64], in_=view[r, 64 + 128 * n_:64 + 128 * n_ + 128, c:c + 256].rearrange("p (h c) -> p h c", h=4)), reads=[T["b_qkv"]], after=[b_vb], pwrites=[b_vb])
                    p.dma("sp", lvb, lambda e, r=r, base=base, c=c, view=view, vb=vb: e.dma_start(out=vb[64:128, base, :, 0:64], in_=view[r, 0:64, c:c + 256].rearrange("p (h c) -> p h c", h=4)), reads=[T["b_qkv"]], after=[b_vb], pwrites=[b_vb])
                    p.dma("sp", lvb, lambda e, r=r, base=base, c=c, view=view, Lr=Lr, nseg=nseg, vb=vb: e.dma_start(out=vb[0:64, base + nseg, :, 0:64], in_=view[r, Lr - 64:Lr, c:c + 256].rearrange("p (h c) -> p h c", h=4)), reads=[T["b_qkv"]], after=[b_vb], pwrites=[b_vb])

        for gi, (kind, gx) in enumerate(groups):
            if kind == "na":
                d = 1
                qc0, kc0, vc0 = 256 * gx, 512 + 256 * gx, 1024 + 256 * gx
            else:
                d = (1, 4, 16)[gx]
                qc0, kc0, vc0 = 1536 + 256 * gx, 2304 + 256 * gx, 3072 + 256 * gx
            Lr = S // d
            nseg = Lr // 128
            vb, b_vb = vbr[gi % 2]
            lvb = L(f"bvb{gi % 2}")
            view = qkv_s.rearrange("(m r) c -> r m c", r=d)
            for r in range(d):
                kwq = dict(writes=[b_qtok]) if r == 0 else dict(pwrites=[b_qtok])
                kwk = dict(writes=[b_ktok]) if r == 0 else dict(pwrites=[b_ktok])
                p.dma("sp", L("bq"), lambda e, r=r, view=view, qc0=qc0, nseg=nseg: e.dma_start(out=qtok[:, r * nseg:(r + 1) * nseg, :], in_=view[r, :, qc0:qc0 + 256].rearrange("(n p) c -> p n c", p=128)), reads=[T["b_qkv"]], **kwq)
                p.dma("sp", L("bk"), lambda e, r=r, view=view, kc0=kc0, nseg=nseg: e.dma_start(out=ktok[:, r * nseg:(r + 1) * nseg, :], in_=view[r, :, kc0:kc0 + 256].rearrange("(n p) c -> p n c", p=128)), reads=[T["b_qkv"]], **kwk)
            if gi == 0:
                load_v(0)
            if gi + 1 < len(groups):
                load_v(gi + 1)
            if os.environ.get("KB_STOP") == "loads":
                continue
            for n in range(NT):
                pt, ptb, _ = ptp.next()
                for pr in range(2):
                    kw = dict(writes=[ptb]) if pr == 0 else dict(pwrites=[ptb])
                    p.pe(lambda e, pt=pt, n=n, pr=pr: e.transpose(out=pt[:, pr, :], in_=qtok[:, n, 128 * pr:128 * pr + 128], identity=ident[:]), reads=[b_qtok, b_const], sig=False, **kw)
                for pr in range(2):
                    p.pe(lambda e, pt=pt, n=n, pr=pr: e.transpose(out=pt[:, 2 + pr, :], in_=ktok[:, n, 128 * pr:128 * pr + 128], identity=ident[:]), reads=[b_ktok, b_const], pwrites=[ptb], sig=(pr == 1))
                if n % 2 == 0:
                    p.dve(lambda e, pt=pt, n=n: e.tensor_copy(out=qT[:, :, 128 * n:128 * n + 128], in_=pt[:, 0:2, :]), reads=[ptb], pwrites=[b_qT])
                    p.dve(lambda e, pt=pt, n=n: e.tensor_copy(out=kT[:, :, 64 + 128 * n:64 + 128 * n + 128], in_=pt[:, 2:4, :]), reads=[ptb], pwrites=[b_kT])
                else:
                    p.act(lambda e, pt=pt, n=n: e.activation(out=qT[:, :, 128 * n:128 * n + 128], in_=pt[:, 0:2, :], func=AF.Copy), reads=[ptb], pwrites=[b_qT])
                    p.act(lambda e, pt=pt, n=n: e.activation(out=kT[:, :, 64 + 128 * n:64 + 128 * n + 128], in_=pt[:, 2:4, :], func=AF.Copy), reads=[ptb], pwrites=[b_kT])
            units = []
            if kind == "na":
                for r in range(64):
                    rs = min(max(r - 4, 0), 56)
                    rel = r - rs
                    st_ = {}
                    for j in range(4):
                        def s1(st_=st_, r=r, rs=rs, rel=rel, j=j, gx=gx):
                            pr, hh = j // 2, j % 2
                            hg = 4 * gx + j
                            ps, psb, _ = pss.next()
                            for jj in range(4):
                                k0 = 64 + 64 * (rs + 2 * jj)
                                kw = dict(writes=[psb]) if jj == 0 else dict(pwrites=[psb])
                                p.pe(lambda e, ps=ps, jj=jj, k0=k0, pr=pr, hh=hh, r=r: e.matmul(ps[:, 64 * jj:64 * jj + 64], lhsT=kT[64 * hh:64 * hh + 64, pr, k0:k0 + 128], rhs=qT[64 * hh:64 * hh + 64, pr, 64 * r:64 * r + 64], start=True, stop=True), reads=[b_kT, b_qT], sig=(jj == 3), **kw)
                            et, eb, _ = er.next()
                            p.act(lambda e, et=et, ps=ps: e.activation(out=et[:], in_=ps[:, 0:256], func=AF.Exp, scale=0.125), reads=[psb], writes=[eb])
                            ptt, ptb2, _ = ptr_.next()
                            eng = "dve" if tgl[0] % 2 == 0 else "pool"
                            tgl[0] += 1
                            p.op(eng, lambda e, ptt=ptt, et=et, hg=hg, rel=rel: e.tensor_tensor(out=ptt[:].rearrange("p (a b) -> p a b", b=64), in0=et[:].rearrange("p (a b) -> p a b", b=64), in1=eb2[:, hg, 7 - rel:14 - rel:2, :], op=ALU.mult), reads=[eb, b_eb2], writes=[ptb2])
                            st_[j] = (ptt, ptb2)

                        def s2(st_=st_, r=r, rs=rs, j=j, gx=gx):
                            if j == 0:
                                st_["po"] = pso.next()
                            po, pob, _ = st_["po"]
                            ptt, ptb2 = st_[j]
                            for jj in range(4):
                                row = rs + 2 * jj
                                if row % 2 == 0:
                                    vt, vbuf, ti = va, b_va, row // 2
                                else:
                                    vt, vbuf, ti = vb, b_vb, (row - 1) // 2
                                kw = dict(writes=[pob]) if (j == 0 and jj == 0) else dict(pwrites=[pob])
                                p.pe(lambda e, po=po, ptt=ptt, jj=jj, vt=vt, ti=ti, j=j: e.matmul(po[0:64, j, :], lhsT=ptt[:, 64 * jj:64 * jj + 64], rhs=vt[:, ti, j, :], start=(jj == 0), stop=(jj == 3)), reads=[ptb2, vbuf], sig=(j == 3 and jj == 3), **kw)
                            if j == 3:
                                rc, rcb, _ = orec.next()
                                p.dve(lambda e, rc=rc, po=po: e.reciprocal(out=rc[:], in_=po[0:64, :, 64]), reads=[pob], writes=[rcb])
                                ot, otb, oi = ona.next()
                                p.dve(lambda e, ot=ot, po=po, rc=rc: e.tensor_tensor(out=ot[:], in0=po[0:64, :, 0:64], in1=rc[:].unsqueeze(2).to_broadcast([64, 4, 64]), op=ALU.mult), reads=[pob, rcb], writes=[otb])
                                p.dma("pool", L(f"bo{oi}"), lambda e, ot=ot, r=r, gx=gx: e.dma_start(out=T["ona_s"][64 * r:64 * r + 64, 256 * gx:256 * gx + 256], in_=ot[:].rearrange("p a b -> p (a b)")), reads=[otb], pwrites=[T["b_ona"]])
                        units.append((s1, s2))
            else:
                oview = T["odil_s"][gx].rearrange("(m r) c -> r m c", r=d)
                for r in range(d):
                    base = r * (nseg + 1)
                    for u in range(nseg):
                        var = 0 if u == 0 else (2 if u == nseg - 1 else 1)
                        t0 = r * Lr + 128 * u
                        st_ = {}
                        for j in range(4):
                            def s1(st_=st_, t0=t0, var=var, j=j):
                                pr, hh = j // 2, j % 2
                                ps, psb, _ = pss.next()
                                for ab in range(2):
                                    kw = dict(writes=[psb]) if ab == 0 else dict(pwrites=[psb])
                                    p.pe(lambda e, ps=ps, ab=ab, t0=t0, pr=pr, hh=hh: e.matmul(ps[:, 128 * ab:128 * ab + 128], lhsT=kT[64 * hh:64 * hh + 64, pr, t0 + 128 * ab:t0 + 128 * ab + 128], rhs=qT[64 * hh:64 * hh + 64, pr, t0:t0 + 128], start=True, stop=True), reads=[b_kT, b_qT], sig=(ab == 1), **kw)
                                et, eb, _ = er.next()
                                p.act(lambda e, et=et, ps=ps: e.activation(out=et[:], in_=ps[:, 0:256], func=AF.Exp, scale=0.125), reads=[psb], writes=[eb])
                                ptt, ptb2, _ = ptr_.next()
                                eng = "dve" if tgl[0] % 2 == 0 else "pool"
                                tgl[0] += 1
                                p.op(eng, lambda e, ptt=ptt, et=et, var=var: e.tensor_tensor(out=ptt[:], in0=et[:], in1=maskd[:, var, :], op=ALU.mult), reads=[eb, b_const], writes=[ptb2])
                                st_[j] = (ptt, ptb2)

                            def s2(st_=st_, base=base, u=u, r=r, j=j, oview=oview):
                                if j == 0:
                                    st_["po"] = pso.next()
                                po, pob, _ = st_["po"]
                                ptt, ptb2 = st_[j]
                                for ab in range(2):
                                    kw = dict(writes=[pob]) if (j == 0 and ab == 0) else dict(pwrites=[pob])
                                    p.pe(lambda e, po=po, ptt=ptt, ab=ab, base=base, u=u, j=j, vb=vb: e.matmul(po[:, j, :], lhsT=ptt[:, 128 * ab:128 * ab + 128], rhs=vb[:, base + u + ab, j, :], start=(ab == 0), stop=(ab == 1)), reads=[ptb2, b_vb], sig=(j == 3 and ab == 1), **kw)
                                if j == 3:
                                    ot, otb, oi = oev.next()
                                    p.act(lambda e, ot=ot, po=po: e.activation(out=ot[:].rearrange("p (a b) -> p a b", b=65), in_=po, func=AF.Copy), reads=[pob], writes=[otb])
                                    p.dma("pool", L(f"bd{oi}"), lambda e, ot=ot, r=r, u=u, oview=oview: e.dma_start(out=oview[r, 128 * u:128 * u + 128, :], in_=ot[:]), reads=[otb], pwrites=[T["b_odil"]])
                            units.append((s1, s2))
            SKEW = 3
            for ui in range(len(units) + SKEW):
                if ui < len(units):
                    units[ui][0]()
                if ui >= SKEW:
                    units[ui - SKEW][1]()
                if ui % 7 == 0:
                    next(pgen, None)
        for _ in pgen:
            pass
        p.drain("sp")
        p.flush()


def load_weight_bf16(p, es, nc, name, src, rows, cols, stage, n0):
    kch = rows // 128
    wt = es.enter_context(nc.sbuf_tensor(name, [128, kch, cols], BF16))
    wb = Buf(name)
    casters = ("dve", "pool", "act")
    n = n0
    for k in range(kch):
        for c0 in range(0, cols, 1024):
            stt, sbf, si = stage.next()
            p.dma("sp", p.lane(f"wst{si}"), lambda e, stt=stt, k=k, c0=c0: e.dma_start(out=stt[:], in_=src[128 * k:128 * k + 128, c0:c0 + 1024]), writes=[sbf])
            eng = casters[n % 3]
            n += 1
            if eng == "act":
                p.act(lambda e, stt=stt, k=k, c0=c0: e.activation(out=wt[:, k, c0:c0 + 1024], in_=stt[:], func=AF.Copy), reads=[sbf], pwrites=[wb])
            else:
                p.op(eng, lambda e, stt=stt, k=k, c0=c0: e.tensor_copy(out=wt[:, k, c0:c0 + 1024], in_=stt[:]), reads=[sbf], pwrites=[wb])
    return wt, wb, n


def transposes(p, pt, ptb, src, srcb, ident, b_id, n, dst_off=0):
    for k in range(n):
        kw = dict(writes=[ptb]) if (k == 0 and dst_off == 0) else dict(pwrites=[ptb])
        p.pe(lambda e, k=k: e.transpose(out=pt[:, dst_off + k, :], in_=src[:, 128 * k:128 * k + 128], identity=ident[:]), reads=[srcb, b_id], sig=(k == n - 1), **kw)


def phase_c(nc, p, T):
    L = p.lane
    with ExitStack() as es:
        def sb(name, shape, dt):
            return es.enter_context(nc.sbuf_tensor(name, shape, dt))

        ident = sb("c_ident", [128, 128], BF16)
        b_id = Buf("c_const")
        p.dma("sp", L("c2"), lambda e: e.dma_start(out=ident[:], in_=T["ident"]), pwrites=[b_id])
        stage = Ring(es, nc, "c_stage", [128, 1024], F32, 3)
        n = 0
        wbn, b_wbn, n = load_weight_bf16(p, es, nc, "c_wbn", T["w_branch_na"], 512, 1024, stage, n)
        wbd, b_wbd, n = load_weight_bf16(p, es, nc, "c_wbd", T["w_branch_dil"], 256, 1024, stage, n)
        wo, b_wo, n = load_weight_bf16(p, es, nc, "c_wo", T["w_out"], 1024, 1024, stage, n)
        xr = Ring(es, nc, "c_x", [128, D], F32, 3)
        onr = Ring(es, nc, "c_on", [128, 512], BF16, 2)
        odr = Ring(es, nc, "c_od", [128, 3, 260], F32, 2)
        sgr = Ring(es, nc, "c_sg", [128, 2048], BF16, 2)
        ods = Ring(es, nc, "c_ods", [128, 260], F32, 2)
        rcr = Ring(es, nc, "c_rc", [128, 4], F32, 2)
        odn = Ring(es, nc, "c_odn", [128, 256], BF16, 2)
        aT = Ring(es, nc, "c_aT", [128, 6, 128], BF16, 2)
        m1 = Ring(es, nc, "c_m1", [128, D], F32, 2)
        m2 = Ring(es, nc, "c_m2", [128, D], F32, 2)
        mg = Ring(es, nc, "c_mg", [128, D], BF16, 2)
        mT = Ring(es, nc, "c_mT", [128, 8, 128], BF16, 2)
        x1r = Ring(es, nc, "c_x1", [128, D], F32, 2)
        ptp = Ring(es, nc, "c_ptp", [128, 8, 128], BF16, 2, psum=True)
        pa = Ring(es, nc, "c_pa", [128, 512], F32, 2, psum=True)
        pd = Ring(es, nc, "c_pd", [128, 512], F32, 2, psum=True)
        py = Ring(es, nc, "c_py", [128, 512], F32, 2, psum=True)

        def loads(i):
            xt, xb, xi = xr.next()
            p.dma("sp", L(f"cx{xi}"), lambda e: e.dma_start(out=xt[:], in_=T["x"][128 * i:128 * i + 128, :]), writes=[xb])
            ot, ob, oi = onr.next()
            p.dma("sp", L(f"con{oi}"), lambda e: e.dma_start(out=ot[:], in_=T["ona_s"][128 * i:128 * i + 128, :]), reads=[T["b_ona"]], writes=[ob])
            dt_, db, di = odr.next()
            p.dma("sp", L(f"cod{di}"), lambda e: e.dma_start(out=dt_[:], in_=T["odil_s"][:, 128 * i:128 * i + 128, :].rearrange("g p c -> p g c")), reads=[T["b_odil"]], writes=[db])
            st_, sb_, si = sgr.next()
            p.dma("sp", L(f"csg{si}"), lambda e: e.dma_start(out=st_[:], in_=T["sg_s"][128 * i:128 * i + 128, :]), reads=[T["b_sg"]], writes=[sb_])
            return (xt, xb, ot, ob, dt_, db, st_, sb_)

        pend = [None]
        nxt = loads(0)
        for i in range(NT):
            xt, xb, ot, ob, dt_, db, sgt, sgb = nxt
            if i + 1 < NT:
                nxt = loads(i + 1)
            odt, odb, _ = ods.next()
            p.dve(lambda e, odt=odt, dt_=dt_: e.tensor_tensor(out=odt[:], in0=dt_[:, 0, :], in1=dt_[:, 1, :], op=ALU.add), reads=[db], writes=[odb])
            p.dve(lambda e, odt=odt, dt_=dt_: e.tensor_tensor(out=odt[:], in0=odt[:], in1=dt_[:, 2, :], op=ALU.add), reads=[db, odb], writes=[odb])
            rc, rcb, _ = rcr.next()
            od3 = odt[:].rearrange("p (a b) -> p a b", b=65)
            p.dve(lambda e, rc=rc, od3=od3: e.reciprocal(out=rc[:], in_=od3[:, :, 64]), reads=[odb], writes=[rcb])
            on_, onb, _ = odn.next()
            p.dve(lambda e, on_=on_, od3=od3, rc=rc: e.tensor_tensor(out=on_[:].rearrange("p (a b) -> p a b", b=64), in0=od3[:, :, 0:64], in1=rc[:].unsqueeze(2).to_broadcast([128, 4, 64]), op=ALU.mult), reads=[odb, rcb], writes=[onb])
            pt, ptb, _ = ptp.next()
            transposes(p, pt, ptb, ot, ob, ident, b_id, 4, 0)
            transposes(p, pt, ptb, on_, onb, ident, b_id, 2, 4)
            at, atb, _ = aT.next()
            p.act(lambda e, at=at, pt=pt: e.activation(out=at[:], in_=pt[:, 0:6, :], func=AF.Copy), reads=[ptb], writes=[atb])
            pas, pds = [], []
            for half in range(2):
                pat, pab, _ = pa.next()
                for k in range(4):
                    kw = dict(writes=[pab]) if k == 0 else dict(pwrites=[pab])
                    p.pe(lambda e, pat=pat, at=at, k=k, half=half: e.matmul(pat[:], lhsT=at[:, k, :], rhs=wbn[:, k, 512 * half:512 * half + 512], start=(k == 0), stop=(k == 3)), reads=[atb, b_wbn], sig=(k == 3), **kw)
                pas.append((pat, pab))
            for half in range(2):
                pdt, pdb, _ = pd.next()
                for k in range(2):
                    kw = dict(writes=[pdb]) if k == 0 else dict(pwrites=[pdb])
                    p.pe(lambda e, pdt=pdt, at=at, k=k, half=half: e.matmul(pdt[:], lhsT=at[:, 4 + k, :], rhs=wbd[:, k, 512 * half:512 * half + 512], start=(k == 0), stop=(k == 1)), reads=[atb, b_wbd], sig=(k == 1), **kw)
                pds.append((pdt, pdb))
            m1t, m1b, _ = m1.next()
            m2t, m2b, _ = m2.next()
            for half in range(2):
                pat, pab = pas[half]
                pdt, pdb = pds[half]
                p.dve(lambda e, m1t=m1t, pat=pat, sgt=sgt, half=half: e.tensor_tensor(out=m1t[:, 512 * half:512 * half + 512], in0=pat[:], in1=sgt[:, 512 * half:512 * half + 512], op=ALU.mult), reads=[pab, sgb], pwrites=[m1b])
                p.dve(lambda e, m2t=m2t, pdt=pdt, sgt=sgt, half=half: e.tensor_tensor(out=m2t[:, 512 * half:512 * half + 512], in0=pdt[:], in1=sgt[:, 1024 + 512 * half:1024 + 512 * half + 512], op=ALU.mult), reads=[pdb, sgb], pwrites=[m2b])
            mgt, mgb, _ = mg.next()
            p.pool(lambda e, mgt=mgt, m1t=m1t, m2t=m2t: e.tensor_tensor(out=mgt[:], in0=m1t[:], in1=m2t[:], op=ALU.add), reads=[m1b, m2b], writes=[mgb])
            def stage2(i=i, xt=xt, xb=xb, mgt=mgt, mgb=mgb):
                pt2, ptb2, _ = ptp.next()
                transposes(p, pt2, ptb2, mgt, mgb, ident, b_id, 8, 0)
                mTt, mTb, _ = mT.next()
                p.act(lambda e, mTt=mTt, pt2=pt2: e.activation(out=mTt[:], in_=pt2[:], func=AF.Copy), reads=[ptb2], writes=[mTb])
                x1t, x1b, x1i = x1r.next()
                for half in range(2):
                    pyt, pyb, _ = py.next()
                    for k in range(8):
                        kw = dict(writes=[pyb]) if k == 0 else dict(pwrites=[pyb])
                        p.pe(lambda e, pyt=pyt, mTt=mTt, k=k, half=half: e.matmul(pyt[:], lhsT=mTt[:, k, :], rhs=wo[:, k, 512 * half:512 * half + 512], start=(k == 0), stop=(k == 7)), reads=[mTb, b_wo], sig=(k == 7), **kw)
                    p.dve(lambda e, x1t=x1t, pyt=pyt, xt=xt, half=half: e.tensor_tensor(out=x1t[:, 512 * half:512 * half + 512], in0=pyt[:], in1=xt[:, 512 * half:512 * half + 512], op=ALU.add), reads=[pyb, xb], pwrites=[x1b])
                p.dma("pool", L(f"cst{x1i}"), lambda e, x1t=x1t, i=i: e.dma_start(out=T["x1_s"][128 * i:128 * i + 128, :], in_=x1t[:]), reads=[x1b], pwrites=[T["b_x1"]])
            if pend[0] is not None:
                pend[0]()
            pend[0] = stage2
        pend[0]()
        p.drain("sp")
        p.flush()


def p_chunks(nc, p, T, es):
    L = p.lane
    sin = Ring(es, nc, "p_in", [128, 2, D], F32, 2)
    sout = Ring(es, nc, "p_out", [128, 2, D], BF16, 2)
    n = 0
    for j in range(64):
        for ti, tbl in enumerate((T["peer_expert_u"], T["peer_expert_v"])):
            it, ib, ii = sin.next()
            ot, ob, oi = sout.next()
            p.dma("sp", L(f"pi{ii}"), lambda e, it=it, tbl=tbl, j=j: e.dma_start(out=it[:], in_=tbl[256 * j:256 * j + 256, :].rearrange("(a q) c -> q a c", q=128)), writes=[ib])
            if n % 2 == 0:
                p.dve(lambda e, it=it, ot=ot: e.tensor_copy(out=ot[:], in_=it[:]), reads=[ib], writes=[ob])
            else:
                p.act(lambda e, it=it, ot=ot: e.activation(out=ot[:], in_=it[:], func=AF.Copy), reads=[ib], writes=[ob])
            n += 1
            p.dma("pool", L(f"po{oi}"), lambda e, ot=ot, ti=ti, j=j: e.dma_start(out=T["uv_s"][256 * j:256 * j + 256, 1024 * ti:1024 * ti + 1024].rearrange("(a q) c -> q a c", q=128), in_=ot[:]), reads=[ob], pwrites=[T["b_uv"]])
            yield


def phase_d(nc, p, T):
    L = p.lane
    U = T["peer_expert_u"]
    V = T["peer_expert_v"]
    ntiles = int(os.environ.get("KD_TILES", NT))
    with ExitStack() as es:
        def sb(name, shape, dt):
            return es.enter_context(nc.sbuf_tensor(name, shape, dt))

        ident = sb("d_ident", [128, 128], BF16)
        gffn = sb("d_gffn", [128, D], F32)
        gple = sb("d_gple", [128, D], F32)
        iota = sb("d_iota", [128, 16], F32)
        sk = sb("d_sk", [128, 2, 128], F32)
        skb = sb("d_skb", [128, 2, 128], BF16)
        kT = sb("d_kT", [128, 2, 128], BF16)
        b_c = Buf("d_const")
        b_sk = Buf("d_sk")
        b_kT = Buf("d_kT")
        p.dma("sp", L("c3"), lambda e: e.dma_start(out=ident[:], in_=T["ident"]), pwrites=[b_c])
        p.dma("sp", L("c3"), lambda e: e.dma_start(out=gffn[:], in_=T["norm_ffn"].partition_broadcast(128)), pwrites=[b_c])
        p.dma("sp", L("c3"), lambda e: e.dma_start(out=gple[:], in_=T["norm_ple"].partition_broadcast(128)), pwrites=[b_c])
        p.dma("sp", L("c3"), lambda e: e.dma_start(out=iota[:], in_=T["iota16"]), pwrites=[b_c])
        p.dma("sp", L("c3"), lambda e: e.dma_start(out=sk[:], in_=T["peer_sub_keys"].rearrange("s n c -> n s c")), pwrites=[b_c])
        NG = int(os.environ.get("KNG", 16))
        uvg = Ring(es, nc, "d_uvg", [128, 2 * D], BF16, NG)
        dgr = Ring(es, nc, "d_dg", [128, 128], BF16, 4)
        class _Stage:
            def __init__(self):
                self.i = -1

            def next(self):
                self.i = (self.i + 1) % 6
                return uvg.t[self.i][:].bitcast(F32), uvg.b[self.i], self.i
        stage = _Stage()
        a_bufs = [Buf(f"a{i}") for i in range(128)]
        w_bufs = [Buf(f"w{i}") for i in range(128)]
        n = 0
        wq, b_wq, n = load_weight_bf16(p, es, nc, "d_wq", T["peer_w_query"], 1024, 2048, stage, n)
        wpg, b_wpg, n = load_weight_bf16(p, es, nc, "d_wpg", T["w_ple_gate"], 1024, 1024, stage, n)
        wpl, b_wpl, n = load_weight_bf16(p, es, nc, "d_wpl", T["w_ple"], 256, 1024, stage, n)

        x1r = Ring(es, nc, "d_x1", [128, D], F32, 2)
        ppr = Ring(es, nc, "d_pp", [128, 256], F32, 2)
        junk = Ring(es, nc, "d_junk", [128, D], BF16, 1)
        st = Ring(es, nc, "d_st", [128, 8], F32, 2)
        h2r = Ring(es, nc, "d_h2", [128, D], F32, 1)
        junka = Ring(es, nc, "d_junka", [128, D], BF16, 1)
        hbr = Ring(es, nc, "d_hb", [128, D], BF16, 2)
        h3r = Ring(es, nc, "d_h3", [128, D], BF16, 1)
        prod = Ring(es, nc, "d_prod", [128, D], BF16, 3)
        junkb = Ring(es, nc, "d_junkb", [128, D], BF16, 1)
        hTr = Ring(es, nc, "d_hT", [128, 8, 128], BF16, 1)
        qpr = Ring(es, nc, "d_qp", [128, 16, 128], BF16, 1)
        scr = Ring(es, nc, "d_sc", [128, 16, 128], F32, 1)
        w1r = Ring(es, nc, "d_w1", [128, 128], F32, 1)
        cdr = Ring(es, nc, "d_cd", [128, 256], F32, 1)
        cd2r = Ring(es, nc, "d_cd2", [128, 256], F32, 1)
        TK = []
        for q_ in range(2):
            TK.append((sb(f"d_tk{q_}", [128, 12, 128], F32), sb(f"d_tku{q_}", [128, 5, 128], U32), Buf(f"d_tk{q_}"), sb(f"d_sm{q_}", [128, 4, 8], F32), Buf(f"d_sm{q_}")))
        oh = scr.t[0][:].rearrange("p (h k) c -> p h k c", k=2).rearrange("p h k (a b) -> p h (k a) b", b=16)
        b_oh = scr.b[0]
        eidx = Ring(es, nc, "d_eidx", [128, 128], I32, 2)
        av = Ring(es, nc, "d_av", [128, 128], F32, 1)
        gav = Ring(es, nc, "d_gav", [128, 128], F32, 1)
        g3r = Ring(es, nc, "d_g3", [128, D], F32, 1)
        pbr = Ring(es, nc, "d_pb", [128, 256], BF16, 1)
        pTr = Ring(es, nc, "d_pT", [128, 2, 128], BF16, 1)
        outr = Ring(es, nc, "d_out", [128, D], F32, 1)
        ptp = Ring(es, nc, "d_ptp", [128, 8, 128], BF16, 1, psum=True)
        pq = Ring(es, nc, "d_pq", [128, 512], F32, 2, psum=True)
        psc = Ring(es, nc, "d_psc", [128, 512], F32, 2, psum=True)
        pyr = Ring(es, nc, "d_py", [128, 512], F32, 2, psum=True)

        if os.environ.get("KDEBUG"):
            print("phase D sbuf remaining", nc.sbuf_bytes_remaining)
        p.dve(lambda e: e.tensor_copy(out=skb[:], in_=sk[:]), reads=[b_c], writes=[b_sk])
        pt, ptb, _ = ptp.next()
        for s_ in range(2):
            kw = dict(writes=[ptb]) if s_ == 0 else dict(pwrites=[ptb])
            p.pe(lambda e, s_=s_, pt=pt: e.transpose(out=pt[:, s_, :], in_=skb[:, s_, :], identity=ident[:]), reads=[b_sk, b_c], sig=(s_ == 1), **kw)
        p.dve(lambda e, pt=pt: e.tensor_copy(out=kT[:], in_=pt[:, 0:2, :]), reads=[ptb], writes=[b_kT])

        def loads(i):
            xt, xb, xi = x1r.next()
            p.dma("sp", L(f"dx{xi}"), lambda e: e.dma_start(out=xt[:], in_=T["x1_s"][128 * i:128 * i + 128, :]), reads=[T["b_x1"]], writes=[xb])
            pp, ppb, pi = ppr.next()
            p.dma("sp", L(f"dp{pi}"), lambda e: e.dma_start(out=pp[:], in_=T["p"][128 * i:128 * i + 128, :]), writes=[ppb])
            return xt, xb, pp, ppb

        def rmsnorm(xt, xb, gain, out_t, out_b):
            jt, jb, _ = junka.next()
            stt, stb, _ = st.next()
            p.act(lambda e: e.activation(out=jt[:], in_=xt[:], func=AF.Square, accum_out=stt[:, 0:1]), reads=[xb], writes=[jb, stb])
            p.act(lambda e: e.activation(out=stt[:, 1:2], in_=stt[:, 0:1], func=AF.Sqrt, scale=1.0 / D, bias=EPS), reads=[stb], pwrites=[stb])
            p.dve(lambda e: e.reciprocal(out=stt[:, 2:3], in_=stt[:, 1:2]), reads=[stb], pwrites=[stb])
            p.dve(lambda e: e.scalar_tensor_tensor(out=out_t[:], in0=xt[:], scalar=stt[:, 2:3], in1=gain[:], op0=ALU.mult, op1=ALU.mult), reads=[xb, stb, b_c], writes=[out_b])

        def front(i):
            xt, xb, pp, ppb = loads(i)
            tk, tku, b_tk, sm, b_sm = TK[i % 2]
            yield
            lset = i % 4
            h2, h2b_, _ = h2r.next()
            rmsnorm(xt, xb, gffn, h2, h2b_)
            yield
            hb, hbb, _ = hbr.next()
            p.act(lambda e, hb=hb, h2=h2: e.activation(out=hb[:], in_=h2[:], func=AF.Copy), reads=[h2b_], writes=[hbb])
            yield
            pt, ptb, _ = ptp.next()
            transposes(p, pt, ptb, hb, hbb, ident, b_c, 8, 0)
            yield
            hT, hTb, _ = hTr.next()
            p.act(lambda e, hT=hT, pt=pt: e.activation(out=hT[:], in_=pt[:], func=AF.Copy), reads=[ptb], writes=[hTb])
            yield
            qp, qpb, _ = qpr.next()
            for rnd in range(4):
                pqt, pqb, _ = pq.next()
                for c4 in range(4):
                    cc = 4 * rnd + c4
                    for k in range(8):
                        kw = dict(writes=[pqb]) if (c4 == 0 and k == 0) else dict(pwrites=[pqb])
                        p.pe(lambda e, pqt=pqt, c4=c4, cc=cc, k=k, hT=hT: e.matmul(pqt[:, 128 * c4:128 * c4 + 128], lhsT=wq[:, k, 128 * cc:128 * cc + 128], rhs=hT[:, k, :], start=(k == 0), stop=(k == 7)), reads=[hTb, b_wq], sig=(c4 == 3 and k == 7), **kw)
                        yield
                if rnd % 2 == 0:
                    p.dve(lambda e, qp=qp, pqt=pqt, rnd=rnd: e.tensor_copy(out=qp[:, 4 * rnd:4 * rnd + 4, :], in_=pqt[:].rearrange("p (a b) -> p a b", b=128)), reads=[pqb], pwrites=[qpb])
                    yield
                else:
                    p.act(lambda e, qp=qp, pqt=pqt, rnd=rnd: e.activation(out=qp[:, 4 * rnd:4 * rnd + 4, :], in_=pqt[:].rearrange("p (a b) -> p a b", b=128), func=AF.Copy), reads=[pqb], pwrites=[qpb])
                    yield
            sc, scb, _ = scr.next()
            for rnd in range(4):
                pst, psb, _ = psc.next()
                for c4 in range(4):
                    cc = 4 * rnd + c4
                    kw = dict(writes=[psb]) if c4 == 0 else dict(pwrites=[psb])
                    p.pe(lambda e, pst=pst, c4=c4, cc=cc, qp=qp: e.matmul(pst[:, 128 * c4:128 * c4 + 128], lhsT=qp[:, cc, :], rhs=kT[:, cc % 2, :], start=True, stop=True), reads=[qpb, b_kT], sig=(c4 == 3), **kw)
                    yield
                if rnd % 2 == 0:
                    p.act(lambda e, sc=sc, pst=pst, rnd=rnd: e.activation(out=sc[:, 4 * rnd:4 * rnd + 4, :], in_=pst[:].rearrange("p (a b) -> p a b", b=128), func=AF.Copy), reads=[psb], pwrites=[scb])
                    yield
                else:
                    p.dve(lambda e, sc=sc, pst=pst, rnd=rnd: e.tensor_copy(out=sc[:, 4 * rnd:4 * rnd + 4, :], in_=pst[:].rearrange("p (a b) -> p a b", b=128)), reads=[psb], pwrites=[scb])
                    yield
            def top16(src_ap, srcb, vals, idxs, wring):
                p.dve(lambda e: e.max(out=vals[:, 0:8], in_=src_ap), reads=[srcb], pwrites=[b_tk])
                p.dve(lambda e: e.max_index(out=idxs[:, 0:8], in_max=vals[:, 0:8], in_values=src_ap), reads=[srcb, b_tk], pwrites=[b_tk])
                wt, wb_, _ = wring.next()
                p.dve(lambda e: e.match_replace(out=wt[:], in_to_replace=vals[:, 0:8], in_values=src_ap, imm_value=-1e30), reads=[srcb, b_tk], writes=[wb_])
                p.dve(lambda e: e.max(out=vals[:, 8:16], in_=wt[:]), reads=[wb_], pwrites=[b_tk])
                p.dve(lambda e: e.max_index(out=idxs[:, 8:16], in_max=vals[:, 8:16], in_values=wt[:]), reads=[wb_, b_tk], pwrites=[b_tk])

            for hh in range(8):
                top16(sc[:, 2 * hh, :], scb, tk[:, 0, 16 * hh:16 * hh + 16], tku[:, 0, 16 * hh:16 * hh + 16], w1r)
                yield "dve"
                top16(sc[:, 2 * hh + 1, :], scb, tk[:, 1, 16 * hh:16 * hh + 16], tku[:, 1, 16 * hh:16 * hh + 16], w1r)
                yield "dve"
                cd, cdb, _ = cdr.next()
                p.dve(lambda e, cd=cd, hh=hh: e.tensor_tensor(out=cd[:].rearrange("p (a b) -> p a b", b=16), in0=tk[:, 0, 16 * hh:16 * hh + 16].unsqueeze(2).to_broadcast([128, 16, 16]), in1=tk[:, 1, 16 * hh:16 * hh + 16].unsqueeze(1).to_broadcast([128, 16, 16]), op=ALU.add), reads=[b_tk], writes=[cdb])
                yield "dve"
                top16(cd[:], cdb, tk[:, 2, 16 * hh:16 * hh + 16], tku[:, 2, 16 * hh:16 * hh + 16], cd2r)
                yield "dve"
            p.dve(lambda e: e.tensor_single_scalar(out=tku[:, 3, :], in_=tku[:, 2, :], scalar=4, op=ALU.logical_shift_right), reads=[b_tk], pwrites=[b_tk])
            yield "dve"
            p.dve(lambda e: e.tensor_single_scalar(out=tku[:, 4, :], in_=tku[:, 2, :], scalar=15, op=ALU.bitwise_and), reads=[b_tk], pwrites=[b_tk])
            yield "dve"
            p.dve(lambda e: e.tensor_copy(out=tk[:, 3:5, :], in_=tku[:, 0:2, :]), reads=[b_tk], pwrites=[b_tk])
            yield "dve"
            p.dve(lambda e: e.tensor_copy(out=tk[:, 5:7, :], in_=tku[:, 3:5, :]), reads=[b_tk], pwrites=[b_tk])
            yield "dve"
            for w in range(2):
                sel = tk[:, 5 + w, :].rearrange("p (h k) -> p h k", k=16).unsqueeze(3).to_broadcast([128, 8, 16, 16])
                tab = tk[:, 3 + w, :].rearrange("p (h a) -> p h a", a=16).unsqueeze(2).to_broadcast([128, 8, 16, 16])
                io = iota[:].unsqueeze(1).unsqueeze(1).to_broadcast([128, 8, 16, 16])
                p.dve(lambda e, sel=sel, io=io: e.tensor_tensor(out=oh, in0=sel, in1=io, op=ALU.is_equal), reads=[b_tk, b_c], writes=[b_oh])
                yield "dve"
                p.dve(lambda e, tab=tab: e.tensor_tensor(out=oh, in0=oh, in1=tab, op=ALU.mult), reads=[b_tk, b_oh], writes=[b_oh])
                yield "dve"
                p.dve(lambda e, w=w: e.tensor_reduce(out=tk[:, 7 + w, :], in_=oh.rearrange("p h k a -> p (h k) a"), axis=AX.X, op=ALU.add), reads=[b_oh], pwrites=[b_tk])
                yield "dve"
            p.dve(lambda e: e.scalar_tensor_tensor(out=tk[:, 11, :], in0=tk[:, 7, :], scalar=128.0, in1=tk[:, 8, :], op0=ALU.mult, op1=ALU.add), reads=[b_tk], pwrites=[b_tk])
            yield "dve"
            ei, eib, _ = eidx.next()
            p.dve(lambda e, ei=ei: e.tensor_copy(out=ei[:], in_=tk[:, 11, :]), reads=[b_tk], writes=[eib])
            yield "dve"
            sc3 = tk[:, 2, :].rearrange("p (h k) -> p h k", k=16)
            p.dve(lambda e, sc3=sc3: e.tensor_tensor(out=tk[:, 9, :].rearrange("p (h k) -> p h k", k=16), in0=sc3, in1=sc3[:, :, 0:1].to_broadcast([128, 8, 16]), op=ALU.subtract), reads=[b_tk], pwrites=[b_tk])
            yield "dve"
            p.act(lambda e: e.activation(out=tk[:, 9, :], in_=tk[:, 9, :], func=AF.Exp), reads=[b_tk], pwrites=[b_tk])
            yield "dve"
            p.dve(lambda e: e.tensor_reduce(out=sm[:, 0, :], in_=tk[:, 9, :].rearrange("p (h k) -> p h k", k=16), axis=AX.X, op=ALU.add), reads=[b_tk], writes=[b_sm])
            yield "dve"
            p.dve(lambda e: e.reciprocal(out=sm[:, 1, :], in_=sm[:, 0, :]), reads=[b_sm], pwrites=[b_sm])
            yield "dve"
            p.dve(lambda e: e.tensor_tensor(out=tk[:, 10, :].rearrange("p (h k) -> p h k", k=16), in0=tk[:, 9, :].rearrange("p (h k) -> p h k", k=16), in1=sm[:, 1, :].unsqueeze(2).to_broadcast([128, 8, 16]), op=ALU.mult), reads=[b_tk, b_sm], pwrites=[b_tk])
            yield "dve"

            F[i] = dict(xt=xt, xb=xb, pp=pp, ppb=ppb, h2=h2, h2b_=h2b_, ei=ei, eib=eib, tk=tk, b_tk=b_tk, hb2=hb, hb2b=hbb)

        F = {}
        for _ in front(0):
            pass
        for i in range(ntiles):
            gen = front(i + 1) if i + 1 < ntiles else iter(())
            gphase = [0]
            fs = F.pop(i)
            xt, xb, pp, ppb, h2, h2b_, ei, eib, tk, b_tk, hb2, hb2b = (fs[k] for k in ("xt", "xb", "pp", "ppb", "h2", "h2b_", "ei", "eib", "tk", "b_tk", "hb2", "hb2b"))
            lset = i % 4
            a_t, _, _ = av.next()
            ga_t, _, _ = gav.next()
            py0, py0b, _ = pyr.next()
            py1, py1b, _ = pyr.next()
            slots = {}
            SK = 2
            for s_ in range(128 + SK):
                if s_ < 128:
                    gt, gb, gi = uvg.next()
                    slots[s_] = (gt, gb)
                    p.dma("pool", L(f"dg{gi}_{lset % 2}"), lambda e, gt=gt, s_=s_, ei=ei: e.indirect_dma_start(out=gt[:], out_offset=None, in_=T["uv_s"], in_offset=bass.IndirectOffsetOnAxis(ap=ei[:, s_:s_ + 1], axis=0)), reads=[eib, T["b_uv"]], writes=[gb])
                    pr_, prb, _ = prod.next()
                    p.dve(lambda e, pr_=pr_, gt=gt, hb2=hb2: e.tensor_tensor(out=pr_[:], in0=gt[:, 0:D], in1=hb2[:], op=ALU.mult), reads=[gb, hb2b], writes=[prb])
                    j2, j2b, _ = junkb.next()
                    p.act(lambda e, j2=j2, pr_=pr_, s_=s_, a_t=a_t: e.activation(out=j2[:], in_=pr_[:], func=AF.Copy, accum_out=a_t[:, s_:s_ + 1]), reads=[prb], writes=[j2b, a_bufs[s_]])
                    p.act(lambda e, ga_t=ga_t, a_t=a_t, s_=s_: e.activation(out=ga_t[:, s_:s_ + 1], in_=a_t[:, s_:s_ + 1], func=AF.Gelu), reads=[a_bufs[s_]], writes=[w_bufs[s_]])
                if s_ >= SK:
                    z = s_ - SK
                    gt, gb = slots.pop(z)
                    dg, dgb, _ = dgr.next()
                    p.dve(lambda e, dg=dg, ga_t=ga_t, z=z, tk=tk: e.tensor_scalar(out=dg[:], in0=ident[:], scalar1=ga_t[:, z:z + 1], scalar2=tk[:, 10, z:z + 1], op0=ALU.mult, op1=ALU.mult), reads=[w_bufs[z], b_tk, b_c], writes=[dgb])
                    for half, (pyt, pyb) in enumerate(((py0, py0b), (py1, py1b))):
                        kw = dict(writes=[pyb]) if z == 0 else dict(pwrites=[pyb])
                        p.pe(lambda e, pyt=pyt, dg=dg, gt=gt, half=half, z=z: e.matmul(pyt[:], lhsT=dg[:], rhs=gt[:, D + 512 * half:D + 512 * half + 512], start=(z == 0), stop=(z == 127)), reads=[dgb, gb], sig=(half == 1), **kw)
                if gphase[0] == 0:
                    for _q in range(6):
                        if next(gen, None) == "dve":
                            gphase[0] = 1
                            break
                elif s_ % 2 == 0:
                    next(gen, None)
            for _ in gen:
                pass
            x2, x2b = xt, xb
            for half, (pyt, pyb) in enumerate(((py0, py0b), (py1, py1b))):
                p.dve(lambda e, x2=x2, pyt=pyt, xt=xt, half=half: e.tensor_tensor(out=x2[:, 512 * half:512 * half + 512], in0=pyt[:], in1=xt[:, 512 * half:512 * half + 512], op=ALU.add), reads=[pyb, xb], writes=[xb])
            h3, h3b, _ = h3r.next()
            rmsnorm(x2, x2b, gple, h3, h3b)
            pt, ptb, _ = ptp.next()
            transposes(p, pt, ptb, h3, h3b, ident, b_c, 8, 0)
            hT3, hT3b, _ = hTr.next()
            p.act(lambda e, hT3=hT3, pt=pt: e.activation(out=hT3[:], in_=pt[:], func=AF.Copy), reads=[ptb], writes=[hT3b])
            g3, g3b, _ = g3r.next()
            for half in range(2):
                pqt, pqb, _ = pq.next()
                for k in range(8):
                    kw = dict(writes=[pqb]) if k == 0 else dict(pwrites=[pqb])
                    p.pe(lambda e, pqt=pqt, hT3=hT3, k=k, half=half: e.matmul(pqt[:], lhsT=hT3[:, k, :], rhs=wpg[:, k, 512 * half:512 * half + 512], start=(k == 0), stop=(k == 7)), reads=[hT3b, b_wpg], sig=(k == 7), **kw)
                p.act(lambda e, g3=g3, pqt=pqt, half=half: e.activation(out=g3[:, 512 * half:512 * half + 512], in_=pqt[:], func=AF.Sigmoid), reads=[pqb], pwrites=[g3b])
            pb, pbb, _ = pbr.next()
            p.dve(lambda e, pb=pb, pp=pp: e.tensor_copy(out=pb[:], in_=pp[:]), reads=[ppb], writes=[pbb])
            pt, ptb, _ = ptp.next()
            transposes(p, pt, ptb, pb, pbb, ident, b_c, 2, 0)
            pT, pTb, _ = pTr.next()
            p.dve(lambda e, pT=pT, pt=pt: e.tensor_copy(out=pT[:], in_=pt[:, 0:2, :]), reads=[ptb], writes=[pTb])
            ot, otb, oi = outr.next()
            t3, t3b = ot, otb
            for half in range(2):
                pst, psb, _ = psc.next()
                for k in range(2):
                    kw = dict(writes=[psb]) if k == 0 else dict(pwrites=[psb])
                    p.pe(lambda e, pst=pst, pT=pT, k=k, half=half: e.matmul(pst[:], lhsT=pT[:, k, :], rhs=wpl[:, k, 512 * half:512 * half + 512], start=(k == 0), stop=(k == 1)), reads=[pTb, b_wpl], sig=(k == 1), **kw)
                p.dve(lambda e, t3=t3, pst=pst, g3=g3, half=half: e.tensor_tensor(out=t3[:, 512 * half:512 * half + 512], in0=pst[:], in1=g3[:, 512 * half:512 * half + 512], op=ALU.mult), reads=[psb, g3b], pwrites=[t3b])
            p.dve(lambda e, ot=ot, t3=t3, x2=x2: e.tensor_tensor(out=ot[:], in0=t3[:], in1=x2[:], op=ALU.add), reads=[t3b, x2b], writes=[otb])
            p.dma("sp", L(f"do{oi}"), lambda e, ot=ot, i=i: e.dma_start(out=T["out"][128 * i:128 * i + 128, :], in_=ot[:]), reads=[otb], pwrites=[T["b_out"]])
        p.drain("sp")
        p.flush()


def _rot_table():
    half = 8
    inv_freq = np.power(np.float32(500000.0), -np.arange(half, dtype=np.float32) * np.float32(2.0) / np.float32(16)).astype(np.float32)
    ang = np.arange(S, dtype=np.float32)[:, None] * inv_freq[None, :]
    return np.concatenate([np.cos(ang), np.sin(ang)], axis=1).astype(np.float32)


def build(debug=False):
    nc = bass.Bass("TRN2", target_bir_lowering=False)
    T = {}

    def din(name, shape, dt):
        T[name] = nc.dram_tensor(name, list(shape), dt, kind="ExternalInput").ap()

    din("x", [S, D], F32)
    din("p", [S, 256], F32)
    din("norm_mix", [1, D], F32)
    din("w_in", [D, INW], F32)
    din("qk_norm_na", [1, 2, 64], F32)
    din("qk_norm_dil", [1, 2, 64], F32)
    din("w_branch_na", [512, D], F32)
    din("w_branch_dil", [256, D], F32)
    din("w_out", [D, D], F32)
    din("norm_ffn", [1, D], F32)
    din("peer_w_query", [D, 2048], F32)
    din("peer_sub_keys", [2, 128, 128], F32)
    din("peer_expert_u", [16384, D], F32)
    din("peer_expert_v", [16384, D], F32)
    din("norm_ple", [1, D], F32)
    din("w_ple_gate", [D, D], F32)
    din("w_ple", [256, D], F32)
    din("ident", [128, 128], BF16)
    din("cs", [S, 16], F32)
    din("maskd", [128, 3, 256], BF16)
    din("cmask", [128, 64], F32)
    din("biasx", [128, 8, 14, 64], F32)
    din("iota16", [128, 16], F32)
    skind = "ExternalOutput" if debug else "Internal"
    T["qkv_s"] = nc.dram_tensor("qkv_s", [S, QKVW], BF16, kind=skind).ap()
    T["sg_s"] = nc.dram_tensor("sg_s", [S, 2048], BF16, kind=skind).ap()
    T["ona_s"] = nc.dram_tensor("ona_s", [S, 512], BF16, kind=skind).ap()
    T["odil_s"] = nc.dram_tensor("odil_s", [3, S, 260], F32, kind=skind).ap()
    T["x1_s"] = nc.dram_tensor("x1_s", [S, D], F32, kind=skind).ap()
    T["uv_s"] = nc.dram_tensor("uv_s", [16384, 2 * D], BF16, kind="Internal").ap()
    T["out"] = nc.dram_tensor("out", [S, D], F32, kind="ExternalOutput").ap()
    for k in ("qkv", "sg", "ona", "odil", "x1", "out", "uv"):
        T["b_" + k] = Buf(k + "_s")
    p = Prog(nc)
    ph = os.environ.get("KPH", "pabcd")
    if "a" in ph:
        phase_a(nc, p, T)
    if "b" in ph:
        phase_b(nc, p, T)
    if "c" in ph:
        phase_c(nc, p, T)
    if "d" in ph:
        phase_d(nc, p, T)
    p.drain("sp")
    p.flush()
    if os.environ.get("KDEBUG"):
        print("ops", p.nops, "sems", len(p.sems))
    return nc


def _masks():
    i = np.arange(128)[:, None]
    j = np.arange(128)[None, :]
    A = (j <= i)
    B = (j >= i)
    m = np.zeros((128, 3, 256), np.float32)
    m[:, 0, :128] = A & (i >= 64)
    m[:, 0, 128:] = B
    m[:, 1, :128] = A
    m[:, 1, 128:] = B
    m[:, 2, :128] = A
    m[:, 2, 128:] = B & (i < 64)
    kc = np.arange(64)[:, None]
    c = np.arange(64)[None, :]
    cs = np.clip(c - 8, 0, 48)
    cm = ((kc >= cs) & (kc < cs + 16)).astype(np.float32)
    cm = np.concatenate([cm, cm], axis=0)
    return m.astype(ml_dtypes.bfloat16), cm


def _biasx(rpb):
    kc = np.arange(64)[:, None]
    c = np.arange(64)[None, :]
    dc = np.clip(kc - c + 15, 0, 30)
    out = np.empty((2, 64, 8, 14, 64), np.float32)
    for a in range(2):
        for dr0 in range(14):
            out[a, :, :, dr0, :] = np.transpose(rpb[:, dr0 + a][:, dc], (1, 0, 2))
    return np.ascontiguousarray(out.reshape(128, 8, 14, 64))


_SHARED = None


def host_shared(inputs):
    f = lambda k: np.ascontiguousarray(np.asarray(inputs[k], np.float32))
    m = {
        "norm_mix": f("norm_mix"),
        "w_in": f("w_in")[0],
        "qk_norm_na": f("qk_norm_na"),
        "qk_norm_dil": f("qk_norm_dil"),
        "w_branch_na": f("w_branch_na")[0],
        "w_branch_dil": f("w_branch_dil")[0],
        "w_out": f("w_out")[0],
        "norm_ffn": f("norm_ffn"),
        "peer_w_query": f("peer_w_query")[0],
        "peer_sub_keys": f("peer_sub_keys")[0],
        "peer_expert_u": f("peer_expert_u")[0],
        "peer_expert_v": f("peer_expert_v")[0],
        "norm_ple": f("norm_ple"),
        "w_ple_gate": f("w_ple_gate")[0],
        "w_ple": f("w_ple")[0],
        "ident": np.eye(128).astype(ml_dtypes.bfloat16),
        "cs": _rot_table(),
        "maskd": _masks()[0],
        "cmask": _masks()[1],
        "biasx": _biasx(np.asarray(inputs["na_rel_bias"], np.float32)[0]),
        "iota16": np.tile(np.arange(16, dtype=np.float32), (128, 1)),
    }
    return m


def host_inputs(inputs, b, shared=None):
    m = dict(shared if shared is not None else host_shared(inputs))
    m["x"] = np.ascontiguousarray(np.asarray(inputs["x"], np.float32)[b])
    m["p"] = np.ascontiguousarray(np.asarray(inputs["p"], np.float32)[0, b])
    return m


def kernel(**inputs):
    nc = build()
    shared = host_shared(inputs)
    nb = np.asarray(inputs["x"]).shape[0]
    in_maps = [host_inputs(inputs, b, shared) for b in range(nb)]
    res = run_bass_kernel_spmd(nc, in_maps, core_ids=list(range(nb)))
    return np.stack([np.asarray(r["out"], np.float32) for r in res.results], axis=0)
```

```python
import os
import numpy as np
import ml_dtypes
from contextlib import ExitStack
import concourse.bass as bass
import concourse.mybir as mybir
from concourse.bass_utils import run_bass_kernel_spmd

F32 = mybir.dt.float32
BF16 = mybir.dt.bfloat16
I32 = mybir.dt.int32
U32 = mybir.dt.uint32
ALU = mybir.AluOpType
AF = mybir.ActivationFunctionType
AX = mybir.AxisListType

ENGS = ("pe", "act", "dve", "pool", "sp")

S = 4096
D = 1024
NT = S // 128
INW = 5888
QKVW = 3840
EPS = 1e-6


class Buf:
    __slots__ = ("name", "writers", "readers")

    def __init__(self, name=""):
        self.name = name
        self.writers = {}
        self.readers = {}


class Lane:
    __slots__ = ("key", "count")

    def __init__(self, key):
        self.key = key
        self.count = 0


def _upd(d, s):
    for k, v in s.items():
        if d.get(k, 0) < v:
            d[k] = v


class Prog:
    def __init__(self, nc):
        self.nc = nc
        self.cnt = {e: 0 for e in ENGS}
        self.seen = {e: {} for e in ENGS}
        self.sems = {}
        self.lanes = {}
        self.ops = {e: [] for e in ENGS}
        self.nops = 0
        for e in ENGS:
            self._sem(e)

    def _sem(self, key):
        if key not in self.sems:
            self.sems[key] = self.nc.alloc_semaphore("s_" + key)
        return self.sems[key]

    def lane(self, name):
        if name not in self.lanes:
            self.lanes[name] = Lane("L_" + name)
            self._sem("L_" + name)
        return self.lanes[name]

    def op(self, eng, fn, reads=(), writes=(), pwrites=(), sig=True, lane=None, after=()):
        deps = {}
        for b in after:
            _upd(deps, b.writers)
        for b in reads:
            _upd(deps, b.writers)
        for b in writes:
            _upd(deps, b.readers)
            _upd(deps, b.writers)
        for b in pwrites:
            _upd(deps, b.readers)
        waits = []
        seen = self.seen[eng]
        for k, v in deps.items():
            if k == "pe" and eng == "pe":
                continue
            if seen.get(k, 0) >= v:
                continue
            seen[k] = v
            waits.append((k, v))
        if lane is not None:
            lane.count += 16
            mykey, myval, inc = lane.key, lane.count, 16
        else:
            if sig:
                self.cnt[eng] += 1
                myval = self.cnt[eng]
                inc = 1
            else:
                myval = self.cnt[eng] + 1
                inc = 0
            mykey = eng
        for b in reads:
            if b.readers.get(mykey, 0) < myval:
                b.readers[mykey] = myval
        for b in writes:
            b.writers = {mykey: myval}
            b.readers = {}
        for b in pwrites:
            if b.writers.get(mykey, 0) < myval:
                b.writers[mykey] = myval
        self.ops[eng].append((waits, fn, mykey, inc))
        self.nops += 1

    def pe(self, fn, **kw):
        self.op("pe", fn, **kw)

    def act(self, fn, **kw):
        self.op("act", fn, **kw)

    def dve(self, fn, **kw):
        self.op("dve", fn, **kw)

    def pool(self, fn, **kw):
        self.op("pool", fn, **kw)

    def dma(self, q, lane, fn, **kw):
        self.op(q, fn, lane=lane, **kw)

    def flush(self):
        nc = self.nc
        ops = self.ops
        sems = self.sems

        def emit(engobj, lst):
            for waits, fn, mykey, inc in lst:
                for k, v in waits:
                    engobj.wait_ge(sems[k], v)
                if fn is None:
                    continue
                inst = fn(engobj)
                if inc:
                    inst.then_inc(sems[mykey], inc)

        with nc.Block() as block:
            @block.tensor
            def _(e):
                emit(e, ops["pe"])

            @block.scalar
            def _(e):
                emit(e, ops["act"])

            @block.vector
            def _(e):
                emit(e, ops["dve"])

            @block.gpsimd
            def _(e):
                emit(e, ops["pool"])

            @block.sync
            def _(e):
                emit(e, ops["sp"])
        self.ops = {e: [] for e in ENGS}

    def wait_all(self, eng, bufs):
        self.op(eng, None, reads=bufs, sig=False)

    def drain(self, eng="sp"):
        b = Buf("drain")
        for ln in self.lanes.values():
            if ln.count:
                b.writers[ln.key] = ln.count
        for e in ENGS:
            if self.cnt[e]:
                b.writers[e] = self.cnt[e]
        self.op(eng, None, reads=[b], sig=False)


class Ring:
    def __init__(self, es, nc, name, shape, dt, n, psum=False):
        self.t = []
        self.b = []
        for i in range(n):
            if psum:
                t = es.enter_context(nc.psum_tensor(f"{name}{i}", shape, dt))
            else:
                t = es.enter_context(nc.sbuf_tensor(f"{name}{i}", shape, dt))
            self.t.append(t)
            self.b.append(Buf(f"{name}{i}"))
        self.i = -1
        self.n = n

    def next(self):
        self.i = (self.i + 1) % self.n
        return self.t[self.i], self.b[self.i], self.i


def phase_a(nc, p, T):
    x, w_in = T["x"], T["w_in"]
    qkv_s, sg_s = T["qkv_s"], T["sg_s"]
    with ExitStack() as es:
        def sb(name, shape, dt):
            return es.enter_context(nc.sbuf_tensor(name, shape, dt))

        wb = sb("a_wb", [128, 8, INW], BF16)
        b_wb = Buf("wb")
        ident = sb("a_ident", [128, 128], BF16)
        gmix = sb("a_gmix", [128, D], F32)
        gfull = sb("a_gfull", [128, 3072], F32)
        cs = sb("a_cs", [128, NT, 16], F32)
        b_id = b_gmix = b_gfull = b_cs = Buf("a_const")
        stage = Ring(es, nc, "a_stage", [128, 1472], F32, 3)
        xr = Ring(es, nc, "a_x", [128, D], F32, 2)
        junk = Ring(es, nc, "a_junk", [128, D], BF16, 1)
        st = Ring(es, nc, "a_st", [128, 8], F32, 2)
        hb = Ring(es, nc, "a_hb", [128, D], BF16, 2)
        hT = Ring(es, nc, "a_hT", [128, 8, 128], BF16, 2)
        sq = Ring(es, nc, "a_sq", [128, 512], F32, 2)
        qst = Ring(es, nc, "a_qst", [128, 24], F32, 3)
        qn = Ring(es, nc, "a_qn", [128, 512], F32, 2)
        qg = Ring(es, nc, "a_qg", [128, 512], F32, 2)
        rt = Ring(es, nc, "a_rt", [128, 4, 8, 8], F32, 2)
        qo = Ring(es, nc, "a_qo", [128, QKVW], BF16, 2)
        so = Ring(es, nc, "a_so", [128, 2048], BF16, 2)
        ptr = Ring(es, nc, "a_ptr", [128, 8, 128], BF16, 2, psum=True)
        pmm = Ring(es, nc, "a_pmm", [128, 512], F32, 6, psum=True)
        L = p.lane

        p.dma("sp", L("c0"), lambda e: e.dma_start(out=ident[:], in_=T["ident"]), pwrites=[b_id])
        p.dma("sp", L("c0"), lambda e: e.dma_start(out=gmix[:], in_=T["norm_mix"].partition_broadcast(128)), pwrites=[b_gmix])
        p.dma("sp", L("c0"), lambda e: e.dma_start(out=cs[:], in_=T["cs"].rearrange("(n p) c -> p n c", p=128)), pwrites=[b_cs])
        first = False
        for (c0, nh, src) in ((0, 8, T["qk_norm_na"][0, 0:1, :]), (512, 8, T["qk_norm_na"][0, 1:2, :]),
                              (1536, 12, T["qk_norm_dil"][0, 0:1, :]), (2304, 12, T["qk_norm_dil"][0, 1:2, :])):
            for j in range(nh):
                cc = c0 + 64 * j
                if first:
                    p.dma("sp", L("c0"), lambda e, cc=cc, src=src: e.dma_start(out=gfull[:, cc:cc + 64], in_=src.partition_broadcast(128)), writes=[b_gfull])
                    first = False
                else:
                    p.dma("sp", L("c0"), lambda e, cc=cc, src=src: e.dma_start(out=gfull[:, cc:cc + 64], in_=src.partition_broadcast(128)), pwrites=[b_gfull])
        casters = ("dve", "pool", "act")
        n = 0
        for k in range(8):
            for q in range(4):
                stt, sbf, si = stage.next()
                p.dma("sp", L(f"stg{si}"), lambda e, stt=stt, k=k, q=q: e.dma_start(out=stt[:], in_=w_in[128 * k:128 * k + 128, 1472 * q:1472 * q + 1472]), writes=[sbf])
                eng = casters[n % 3]
                n += 1
                if eng == "act":
                    p.act(lambda e, stt=stt, k=k, q=q: e.activation(out=wb[:, k, 1472 * q:1472 * q + 1472], in_=stt[:], func=AF.Copy), reads=[sbf], pwrites=[b_wb])
                else:
                    p.op(eng, lambda e, stt=stt, k=k, q=q: e.tensor_copy(out=wb[:, k, 1472 * q:1472 * q + 1472], in_=stt[:]), reads=[sbf], pwrites=[b_wb])

        def load_x(i):
            xt, xb, xi = xr.next()
            p.dma("sp", L(f"ax{xi}"), lambda e: e.dma_start(out=xt[:], in_=x[128 * i:128 * i + 128, :]), writes=[xb])
            return xt, xb

        nxt = load_x(0)
        for i in range(NT):
            xt, xb = nxt
            if i + 1 < NT:
                nxt = load_x(i + 1)
            jt, jb, _ = junk.next()
            stt, stb, _ = st.next()
            p.act(lambda e, jt=jt, xt=xt, stt=stt: e.activation(out=jt[:], in_=xt[:], func=AF.Square, accum_out=stt[:, 0:1]), reads=[xb], writes=[jb, stb])
            p.act(lambda e, stt=stt: e.activation(out=stt[:, 1:2], in_=stt[:, 0:1], func=AF.Sqrt, scale=1.0 / D, bias=EPS), reads=[stb], pwrites=[stb])
            p.dve(lambda e, stt=stt: e.reciprocal(out=stt[:, 2:3], in_=stt[:, 1:2]), reads=[stb], pwrites=[stb])
            hbt, hbb, _ = hb.next()
            p.dve(lambda e, hbt=hbt, xt=xt, stt=stt: e.scalar_tensor_tensor(out=hbt[:], in0=xt[:], scalar=stt[:, 2:3], in1=gmix[:], op0=ALU.mult, op1=ALU.mult), reads=[xb, stb, b_gmix], writes=[hbb])
            pt, ptb, _ = ptr.next()
            for k in range(8):
                if k == 0:
                    p.pe(lambda e, pt=pt, hbt=hbt, k=k: e.transpose(out=pt[:, k, :], in_=hbt[:, 128 * k:128 * k + 128], identity=ident[:]), reads=[hbb, b_id], writes=[ptb], sig=False)
                else:
                    p.pe(lambda e, pt=pt, hbt=hbt, k=k: e.transpose(out=pt[:, k, :], in_=hbt[:, 128 * k:128 * k + 128], identity=ident[:]), reads=[hbb, b_id], pwrites=[ptb], sig=(k == 7))
            hTt, hTb, _ = hT.next()
            p.act(lambda e, hTt=hTt, pt=pt: e.activation(out=hTt[:], in_=pt[:], func=AF.Copy), reads=[ptb], writes=[hTb])
            qot, qob, qoi = qo.next()
            sot, sob, soi = so.next()
            first_q = True
            first_s = True
            for blk in range(12):
                c0 = 512 * blk
                w = 256 if blk == 7 else 512
                if blk >= 8:
                    c0 = QKVW + 512 * (blk - 8)
                pm, pmb, _ = pmm.next()
                for k in range(8):
                    if k == 0:
                        p.pe(lambda e, pm=pm, hTt=hTt, k=k, c0=c0, w=w: e.matmul(pm[:, 0:w], lhsT=hTt[:, k, :], rhs=wb[:, k, c0:c0 + w], start=True, stop=False), reads=[hTb, b_wb], writes=[pmb], sig=False)
                    else:
                        p.pe(lambda e, pm=pm, hTt=hTt, k=k, c0=c0, w=w: e.matmul(pm[:, 0:w], lhsT=hTt[:, k, :], rhs=wb[:, k, c0:c0 + w], start=False, stop=(k == 7)), reads=[hTb, b_wb], pwrites=[pmb], sig=(k == 7))
                qkw = dict(pwrites=[qob])
                if blk in (0, 1, 3, 4, 5):
                    sqt, sqb, _ = sq.next()
                    qs, qsb, _ = qst.next()
                    p.act(lambda e, sqt=sqt, pm=pm: e.activation(out=sqt[:], in_=pm[:], func=AF.Square), reads=[pmb], writes=[sqb])
                    p.dve(lambda e, qs=qs, sqt=sqt: e.tensor_reduce(out=qs[:, 0:8], in_=sqt[:].rearrange("p (a b) -> p a b", b=64), axis=AX.X, op=ALU.add), reads=[sqb], writes=[qsb])
                    p.act(lambda e, qs=qs: e.activation(out=qs[:, 8:16], in_=qs[:, 0:8], func=AF.Sqrt, scale=1.0 / 64, bias=EPS), reads=[qsb], pwrites=[qsb])
                    p.dve(lambda e, qs=qs: e.reciprocal(out=qs[:, 16:24], in_=qs[:, 8:16]), reads=[qsb], pwrites=[qsb])
                    qnt, qnb, _ = qn.next()
                    p.dve(lambda e, qnt=qnt, pm=pm, qs=qs: e.tensor_tensor(out=qnt[:].rearrange("p (a b) -> p a b", b=64), in0=pm[:].rearrange("p (a b) -> p a b", b=64), in1=qs[:, 16:24].unsqueeze(2).to_broadcast([128, 8, 64]), op=ALU.mult), reads=[pmb, qsb], writes=[qnb])
                    if blk < 2:
                        p.dve(lambda e, qot=qot, qnt=qnt, c0=c0: e.tensor_tensor(out=qot[:, c0:c0 + 512], in0=qnt[:], in1=gfull[:, c0:c0 + 512], op=ALU.mult), reads=[qnb, b_gfull], **qkw)
                    else:
                        qgt, qgb, _ = qg.next()
                        p.dve(lambda e, qgt=qgt, qnt=qnt, c0=c0: e.tensor_tensor(out=qgt[:], in0=qnt[:], in1=gfull[:, c0:c0 + 512], op=ALU.mult), reads=[qnb, b_gfull], writes=[qgb])
                        q3 = qgt[:].rearrange("p (a b) -> p a b", b=64)
                        o3 = qot[:, c0:c0 + 512].rearrange("p (a b) -> p a b", b=64)
                        rtt, rtb, _ = rt.next()
                        cosb = cs[:, i, 0:8].unsqueeze(1).to_broadcast([128, 8, 8])
                        sinb = cs[:, i, 8:16].unsqueeze(1).to_broadcast([128, 8, 8])
                        p.pool(lambda e, rtt=rtt, q3=q3, cosb=cosb: e.tensor_tensor(out=rtt[:, 0], in0=q3[:, :, 0:8], in1=cosb, op=ALU.mult), reads=[qgb, b_cs], writes=[rtb])
                        p.pool(lambda e, rtt=rtt, q3=q3, sinb=sinb: e.tensor_tensor(out=rtt[:, 1], in0=q3[:, :, 8:16], in1=sinb, op=ALU.mult), reads=[qgb, b_cs], pwrites=[rtb])
                        p.pool(lambda e, rtt=rtt, q3=q3, cosb=cosb: e.tensor_tensor(out=rtt[:, 2], in0=q3[:, :, 8:16], in1=cosb, op=ALU.mult), reads=[qgb, b_cs], pwrites=[rtb])
                        p.pool(lambda e, rtt=rtt, q3=q3, sinb=sinb: e.tensor_tensor(out=rtt[:, 3], in0=q3[:, :, 0:8], in1=sinb, op=ALU.mult), reads=[qgb, b_cs], pwrites=[rtb])
                        p.pool(lambda e, rtt=rtt, o3=o3: e.tensor_tensor(out=o3[:, :, 0:8], in0=rtt[:, 0], in1=rtt[:, 1], op=ALU.subtract), reads=[rtb], **qkw)
                        p.pool(lambda e, rtt=rtt, o3=o3: e.tensor_tensor(out=o3[:, :, 8:16], in0=rtt[:, 2], in1=rtt[:, 3], op=ALU.add), reads=[rtb], pwrites=[qob])
                        p.pool(lambda e, q3=q3, o3=o3: e.tensor_copy(out=o3[:, :, 16:64], in_=q3[:, :, 16:64]), reads=[qgb], pwrites=[qob])
                    first_q = False
                elif blk in (2, 6, 7):
                    p.act(lambda e, qot=qot, pm=pm, c0=c0, w=w: e.activation(out=qot[:, c0:c0 + w], in_=pm[:, 0:w], func=AF.Copy), reads=[pmb], **qkw)
                    first_q = False
                else:
                    g0 = 512 * (blk - 8)
                    skw = dict(pwrites=[sob])
                    first_s = False
                    p.act(lambda e, sot=sot, pm=pm, g0=g0: e.activation(out=sot[:, g0:g0 + 512], in_=pm[:], func=AF.Sigmoid), reads=[pmb], **skw)
            p.dma("pool", L(f"aq{qoi}"), lambda e, qot=qot, i=i: e.dma_start(out=qkv_s[128 * i:128 * i + 128, :], in_=qot[:]), reads=[qob], pwrites=[T["b_qkv"]])
            p.dma("pool", L(f"as{soi}"), lambda e, sot=sot, i=i: e.dma_start(out=sg_s[128 * i:128 * i + 128, :], in_=sot[:]), reads=[sob], pwrites=[T["b_sg"]])
        p.drain("sp")
        p.flush()


def phase_b(nc, p, T):
    qkv_s = T["qkv_s"]
    L = p.lane
    with ExitStack() as es:
        def sb(name, shape, dt):
            return es.enter_context(nc.sbuf_tensor(name, shape, dt))

        ident = sb("b_ident", [128, 128], BF16)
        maskd = sb("b_maskd", [128, 3, 256], BF16)
        cmask = sb("b_cmask", [128, 64], F32)
        eb2 = sb("b_eb2", [128, 8, 14, 64], BF16)
        b_const = Buf("b_const")
        b_eb2 = Buf("eb2")
        qtok = sb("b_qtok", [128, NT, 256], BF16)
        ktok = sb("b_ktok", [128, NT, 256], BF16)
        b_qtok = Buf("qtok")
        b_ktok = Buf("ktok")
        qT = sb("b_qT", [128, 2, S], BF16)
        kT = sb("b_kT", [128, 2, S + 128], BF16)
        b_qT = Buf("qT")
        b_kT = Buf("kT")
        va = sb("b_va", [128, NT, 4, 65], BF16)
        vbr = [(sb("b_vb0", [128, NT + 16, 4, 65], BF16), Buf("vb0")), (sb("b_vb1", [128, NT + 16, 4, 65], BF16), Buf("vb1"))]
        b_va = Buf("va")
        bstage = Ring(es, nc, "b_bst", [128, 14, 64], F32, 2)
        er = Ring(es, nc, "b_e", [128, 256], BF16, 3)
        ptr_ = Ring(es, nc, "b_pt", [128, 256], BF16, 6)
        oev = Ring(es, nc, "b_oev", [128, 260], F32, 2)
        orec = Ring(es, nc, "b_orec", [64, 4], F32, 2)
        ona = Ring(es, nc, "b_ona", [64, 4, 64], BF16, 2)
        ptp = Ring(es, nc, "b_ptp", [128, 8, 128], BF16, 2, psum=True)
        pss = Ring(es, nc, "b_pss", [128, 512], F32, 4, psum=True)
        pso_ = Ring(es, nc, "b_pso", [128, 512], F32, 2, psum=True)

        class _PSO:
            def next(self):
                t, b, i = pso_.next()
                return t[:, 0:260].rearrange("p (a b) -> p a b", b=65), b, i
        pso = _PSO()

        p.dma("sp", L("c1"), lambda e: e.dma_start(out=ident[:], in_=T["ident"]), pwrites=[b_const])
        p.dma("sp", L("c1"), lambda e: e.dma_start(out=maskd[:], in_=T["maskd"]), pwrites=[b_const])
        p.dma("sp", L("c1"), lambda e: e.dma_start(out=cmask[:], in_=T["cmask"]), pwrites=[b_const])
        for h in range(8):
            bt, bb, bi = bstage.next()
            p.dma("sp", L(f"bst{bi}"), lambda e, bt=bt, h=h: e.dma_start(out=bt[:], in_=T["biasx"][:, h]), writes=[bb])
            p.act(lambda e, bt=bt: e.activation(out=bt[:], in_=bt[:], func=AF.Exp), reads=[bb], writes=[bb])
            p.dve(lambda e, bt=bt, h=h: e.tensor_tensor(out=eb2[:, h], in0=bt[:], in1=cmask[:].unsqueeze(1).to_broadcast([128, 14, 64]), op=ALU.mult), reads=[bb, b_const], pwrites=[b_eb2])
        p.pool(lambda e: e.memset(kT[:, :, 0:64], 0.0), pwrites=[b_kT])
        p.pool(lambda e: e.memset(kT[:, :, S + 64:S + 128], 0.0), pwrites=[b_kT])

        groups = [("na", 0), ("dil", 0), ("na", 1), ("dil", 1), ("dil", 2)]
        if os.environ.get("KB_STOP") == "const":
            groups = []
        if os.environ.get("KB_GROUPS"):
            groups = [groups[int(c)] for c in os.environ["KB_GROUPS"]]
        tgl = [0]
        pgen = p_chunks(nc, p, T, es)
        def gparams(gi):
            kind, gx = groups[gi]
            if kind == "na":
                d = 1
                qc0, kc0, vc0 = 256 * gx, 512 + 256 * gx, 1024 + 256 * gx
            else:
                d = (1, 4, 16)[gx]
                qc0, kc0, vc0 = 1536 + 256 * gx, 2304 + 256 * gx, 3072 + 256 * gx
            Lr = S // d
            nseg = Lr // 128
            vb, b_vb = vbr[gi % 2]
            lvb = L(f"bvb{gi % 2}")
            view = qkv_s.rearrange("(m r) c -> r m c", r=d)
            return kind, gx, d, qc0, kc0, vc0, Lr, nseg, vb, b_vb, lvb, view

        def load_v(gi):
            kind, gx, d, qc0, kc0, vc0, Lr, nseg, vb, b_vb, lvb, view = gparams(gi)
            p.pool(lambda e, vb=vb: e.memset(vb[:], 0.0), writes=[b_vb])
            p.pool(lambda e, vb=vb: e.memset(vb[:, :, :, 64:65], 1.0), after=[b_vb], pwrites=[b_vb])
            if kind == "na":
                p.pool(lambda e: e.memset(va[:, :, :, 64:65], 1.0), writes=[b_va])
                for hh in range(4):
                    p.dma("sp", lvb, lambda e, hh=hh, vc0=vc0, vb=vb: e.dma_start(out=vb[:, 0:NT - 1, hh, 0:64], in_=qkv_s[64:S - 64, vc0 + 64 * hh:vc0 + 64 * hh + 64].rearrange("(n p) c -> p n c", p=128)), reads=[T["b_qkv"]], after=[b_vb], pwrites=[b_vb])
                for hh in range(4):
                    p.dma("sp", L("bva"), lambda e, hh=hh, vc0=vc0: e.dma_start(out=va[:, :, hh, 0:64], in_=qkv_s[:, vc0 + 64 * hh:vc0 + 64 * hh + 64].rearrange("(n p) c -> p n c", p=128)), reads=[T["b_qkv"]], after=[b_va], pwrites=[b_va])
            else:
                for r in range(d):
                    base = r * (nseg + 1)
                    c = vc0
                    if nseg - 1 >= 4:
                        for hh in range(4):
                            p.dma("sp", lvb, lambda e, r=r, base=base, c=c, hh=hh, view=view, Lr=Lr, nseg=nseg, vb=vb: e.dma_start(out=vb[:, base + 1:base + nseg, hh, 0:64], in_=view[r, 64:Lr - 64, c + 64 * hh:c + 64 * hh + 64].rearrange("(n p) c -> p n c", p=128)), reads=[T["b_qkv"]], after=[b_vb], pwrites=[b_vb])
                    else:
                        for n_ in range(nseg - 1):
                            p.dma("sp", lvb, lambda e, r=r, base=base, c=c, n_=n_, view=view, vb=vb: e.dma_start(out=vb[:, base + 1 + n_, :, 0:64], in_=view[r, 64 + 128 * n_:64 + 128 * n_ + 128, c:c + 256].rearrange("p (h c) -> p h c", h=4)), reads=[T["b_qkv"]], after=[b_vb], pwrites=[b_vb])
                    p.dma("sp", lvb, lambda e, r=r, base=base, c=c, view=view, vb=vb: e.dma_start(out=vb[64:128, base, :, 0:64], in_=view[r, 0:64, c:c + 256].rearrange("p (h c) -> p h c", h=4)), reads=[T["b_qkv"]], after=[b_vb], pwrites=[b_vb])
                    p.dma("sp", lvb, lambda e, r=r, base=base, c=c, view=view, Lr=Lr, nseg=nseg, vb=vb: e.dma_start(out=vb[0:64, base + nseg, :, 0:64], in_=view[r, Lr - 64:Lr, c:c + 256].rearrange("p (h c) -> p h c", h=4)), reads=[T["b_qkv"]], after=[b_vb], pwrites=[b_vb])

        for gi, (kind, gx) in enumerate(groups):
            if kind == "na":
                d = 1
                qc0, kc0, vc0 = 256 * gx, 512 + 256 * gx, 1024 + 256 * gx
            else:
                d = (1, 4, 16)[gx]
                qc0, kc0, vc0 = 1536 + 256 * gx, 2304 + 256 * gx, 3072 + 256 * gx
            Lr = S // d
            nseg = Lr // 128
            vb, b_vb = vbr[gi % 2]
            lvb = L(f"bvb{gi % 2}")
            view = qkv_s.rearrange("(m r) c -> r m c", r=d)
            for r in range(d):
                kwq = dict(writes=[b_qtok]) if r == 0 else dict(pwrites=[b_qtok])
                kwk = dict(writes=[b_ktok]) if r == 0 else dict(pwrites=[b_ktok])
                p.dma("sp", L("bq"), lambda e, r=r, view=view, qc0=qc0, nseg=nseg: e.dma_start(out=qtok[:, r * nseg:(r + 1) * nseg, :], in_=view[r, :, qc0:qc0 + 256].rearrange("(n p) c -> p n c", p=128)), reads=[T["b_qkv"]], **kwq)
                p.dma("sp", L("bk"), lambda e, r=r, view=view, kc0=kc0, nseg=nseg: e.dma_start(out=ktok[:, r * nseg:(r + 1) * nseg, :], in_=view[r, :, kc0:kc0 + 256].rearrange("(n p) c -> p n c", p=128)), reads=[T["b_qkv"]], **kwk)
            if gi == 0:
                load_v(0)
            if gi + 1 < len(groups):
                load_v(gi + 1)
            if os.environ.get("KB_STOP") == "loads":
                continue
            for n in range(NT):
                pt, ptb, _ = ptp.next()
                for pr in range(2):
                    kw = dict(writes=[ptb]) if pr == 0 else dict(pwrites=[ptb])
                    p.pe(lambda e, pt=pt, n=n, pr=pr: e.transpose(out=pt[:, pr, :], in_=qtok[:, n, 128 * pr:128 * pr + 128], identity=ident[:]), reads=[b_qtok, b_const], sig=False, **kw)
                for pr in range(2):
                    p.pe(lambda e, pt=pt, n=n, pr=pr: e.transpose(out=pt[:, 2 + pr, :], in_=ktok[:, n, 128 * pr:128 * pr + 128], identity=ident[:]), reads=[b_ktok, b_const], pwrites=[ptb], sig=(pr == 1))
                if n % 2 == 0:
                    p.dve(lambda e, pt=pt, n=n: e.tensor_copy(out=qT[:, :, 128 * n:128 * n + 128], in_=pt[:, 0:2, :]), reads=[ptb], pwrites=[b_qT])
                    p.dve(lambda e, pt=pt, n=n: e.tensor_copy(out=kT[:, :, 64 + 128 * n:64 + 128 * n + 128], in_=pt[:, 2:4, :]), reads=[ptb], pwrites=[b_kT])
                else:
                    p.act(lambda e, pt=pt, n=n: e.activation(out=qT[:, :, 128 * n:128 * n + 128], in_=pt[:, 0:2, :], func=AF.Copy), reads=[ptb], pwrites=[b_qT])
                    p.act(lambda e, pt=pt, n=n: e.activation(out=kT[:, :, 64 + 128 * n:64 + 128 * n + 128], in_=pt[:, 2:4, :], func=AF.Copy), reads=[ptb], pwrites=[b_kT])
            units = []
            if kind == "na":
                for r in range(64):
                    rs = min(max(r - 4, 0), 56)
                    rel = r - rs
                    st_ = {}
                    for j in range(4):
                        def s1(st_=st_, r=r, rs=rs, rel=rel, j=j, gx=gx):
                            pr, hh = j // 2, j % 2
                            hg = 4 * gx + j
                            ps, psb, _ = pss.next()
                            for jj in range(4):
                                k0 = 64 + 64 * (rs + 2 * jj)
                                kw = dict(writes=[psb]) if jj == 0 else dict(pwrites=[psb])
                                p.pe(lambda e, ps=ps, jj=jj, k0=k0, pr=pr, hh=hh, r=r: e.matmul(ps[:, 64 * jj:64 * jj + 64], lhsT=kT[64 * hh:64 * hh + 64, pr, k0:k0 + 128], rhs=qT[64 * hh:64 * hh + 64, pr, 64 * r:64 * r + 64], start=True, stop=True), reads=[b_kT, b_qT], sig=(jj == 3), **kw)
                            et, eb, _ = er.next()
                            p.act(lambda e, et=et, ps=ps: e.activation(out=et[:], in_=ps[:, 0:256], func=AF.Exp, scale=0.125), reads=[psb], writes=[eb])
                            ptt, ptb2, _ = ptr_.next()
                            eng = "dve" if tgl[0] % 2 == 0 else "pool"
                            tgl[0] += 1
                            p.op(eng, lambda e, ptt=ptt, et=et, hg=hg, rel=rel: e.tensor_tensor(out=ptt[:].rearrange("p (a b) -> p a b", b=64), in0=et[:].rearrange("p (a b) -> p a b", b=64), in1=eb2[:, hg, 7 - rel:14 - rel:2, :], op=ALU.mult), reads=[eb, b_eb2], writes=[ptb2])
                            st_[j] = (ptt, ptb2)

                        def s2(st_=st_, r=r, rs=rs, j=j, gx=gx):
                            if j == 0:
                                st_["po"] = pso.next()
                            po, pob, _ = st_["po"]
                            ptt, ptb2 = st_[j]
                            for jj in range(4):
                                row = rs + 2 * jj
                                if row % 2 == 0:
                                    vt, vbuf, ti = va, b_va, row // 2
                                else:
                                    vt, vbuf, ti = vb, b_vb, (row - 1) // 2
                                kw = dict(writes=[pob]) if (j == 0 and jj == 0) else dict(pwrites=[pob])
                                p.pe(lambda e, po=po, ptt=ptt, jj=jj, vt=vt, ti=ti, j=j: e.matmul(po[0:64, j, :], lhsT=ptt[:, 64 * jj:64 * jj + 64], rhs=vt[:, ti, j, :], start=(jj == 0), stop=(jj == 3)), reads=[ptb2, vbuf], sig=(j == 3 and jj == 3), **kw)
                            if j == 3:
                                rc, rcb, _ = orec.next()
                                p.dve(lambda e, rc=rc, po=po: e.reciprocal(out=rc[:], in_=po[0:64, :, 64]), reads=[pob], writes=[rcb])
                                ot, otb, oi = ona.next()
                                p.dve(lambda e, ot=ot, po=po, rc=rc: e.tensor_tensor(out=ot[:], in0=po[0:64, :, 0:64], in1=rc[:].unsqueeze(2).to_broadcast([64, 4, 64]), op=ALU.mult), reads=[pob, rcb], writes=[otb])
                                p.dma("pool", L(f"bo{oi}"), lambda e, ot=ot, r=r, gx=gx: e.dma_start(out=T["ona_s"][64 * r:64 * r + 64, 256 * gx:256 * gx + 256], in_=ot[:].rearrange("p a b -> p (a b)")), reads=[otb], pwrites=[T["b_ona"]])
                        units.append((s1, s2))
            else:
                oview = T["odil_s"][gx].rearrange("(m r) c -> r m c", r=d)
                for r in range(d):
                    base = r * (nseg + 1)
                    for u in range(nseg):
                        var = 0 if u == 0 else (2 if u == nseg - 1 else 1)
                        t0 = r * Lr + 128 * u
                        st_ = {}
                        for j in range(4):
                            def s1(st_=st_, t0=t0, var=var, j=j):
                                pr, hh = j // 2, j % 2
                                ps, psb, _ = pss.next()
                                for ab in range(2):
                                    kw = dict(writes=[psb]) if ab == 0 else dict(pwrites=[psb])
                                    p.pe(lambda e, ps=ps, ab=ab, t0=t0, pr=pr, hh=hh: e.matmul(ps[:, 128 * ab:128 * ab + 128], lhsT=kT[64 * hh:64 * hh + 64, pr, t0 + 128 * ab:t0 + 128 * ab + 128], rhs=qT[64 * hh:64 * hh + 64, pr, t0:t0 + 128], start=True, stop=True), reads=[b_kT, b_qT], sig=(ab == 1), **kw)
                                et, eb, _ = er.next()
                                p.act(lambda e, et=et, ps=ps: e.activation(out=et[:], in_=ps[:, 0:256], func=AF.Exp, scale=0.125), reads=[psb], writes=[eb])
                                ptt, ptb2, _ = ptr_.next()
                                eng = "dve" if tgl[0] % 2 == 0 else "pool"
                                tgl[0] += 1
                                p.op(eng, lambda e, ptt=ptt, et=et, var=var: e.tensor_tensor(out=ptt[:], in0=et[:], in1=maskd[:, var, :], op=ALU.mult), reads=[eb, b_const], writes=[ptb2])
                                st_[j] = (ptt, ptb2)

                            def s2(st_=st_, base=base, u=u, r=r, j=j, oview=oview):
                                if j == 0:
                                    st_["po"] = pso.next()
                                po, pob, _ = st_["po"]
                                ptt, ptb2 = st_[j]
                                for ab in range(2):
                                    kw = dict(writes=[pob]) if (j == 0 and ab == 0) else dict(pwrites=[pob])
                                    p.pe(lambda e, po=po, ptt=ptt, ab=ab, base=base, u=u, j=j, vb=vb: e.matmul(po[:, j, :], lhsT=ptt[:, 128 * ab:128 * ab + 128], rhs=vb[:, base + u + ab, j, :], start=(ab == 0), stop=(ab == 1)), reads=[ptb2, b_vb], sig=(j == 3 and ab == 1), **kw)
                                if j == 3:
                                    ot, otb, oi = oev.next()
                                    p.act(lambda e, ot=ot, po=po: e.activation(out=ot[:].rearrange("p (a b) -> p a b", b=65), in_=po, func=AF.Copy), reads=[pob], writes=[otb])
                                    p.dma("pool", L(f"bd{oi}"), lambda e, ot=ot, r=r, u=u, oview=oview: e.dma_start(out=oview[r, 128 * u:128 * u + 128, :], in_=ot[:]), reads=[otb], pwrites=[T["b_odil"]])
                            units.append((s1, s2))
            SKEW = 3
            for ui in range(len(units) + SKEW):
                if ui < len(units):
                    units[ui][0]()
                if ui >= SKEW:
                    units[ui - SKEW][1]()
                if ui % 7 == 0:
                    next(pgen, None)
        for _ in pgen:
            pass
        p.drain("sp")
        p.flush()


def load_weight_bf16(p, es, nc, name, src, rows, cols, stage, n0):
    kch = rows // 128
    wt = es.enter_context(nc.sbuf_tensor(name, [128, kch, cols], BF16))
    wb = Buf(name)
    casters = ("dve", "pool", "act")
    n = n0
    for k in range(kch):
        for c0 in range(0, cols, 1024):
            stt, sbf, si = stage.next()
            p.dma("sp", p.lane(f"wst{si}"), lambda e, stt=stt, k=k, c0=c0: e.dma_start(out=stt[:], in_=src[128 * k:128 * k + 128, c0:c0 + 1024]), writes=[sbf])
            eng = casters[n % 3]
            n += 1
            if eng == "act":
                p.act(lambda e, stt=stt, k=k, c0=c0: e.activation(out=wt[:, k, c0:c0 + 1024], in_=stt[:], func=AF.Copy), reads=[sbf], pwrites=[wb])
            else:
                p.op(eng, lambda e, stt=stt, k=k, c0=c0: e.tensor_copy(out=wt[:, k, c0:c0 + 1024], in_=stt[:]), reads=[sbf], pwrites=[wb])
    return wt, wb, n


def transposes(p, pt, ptb, src, srcb, ident, b_id, n, dst_off=0):
    for k in range(n):
        kw = dict(writes=[ptb]) if (k == 0 and dst_off == 0) else dict(pwrites=[ptb])
        p.pe(lambda e, k=k: e.transpose(out=pt[:, dst_off + k, :], in_=src[:, 128 * k:128 * k + 128], identity=ident[:]), reads=[srcb, b_id], sig=(k == n - 1), **kw)


def phase_c(nc, p, T):
    L = p.lane
    with ExitStack() as es:
        def sb(name, shape, dt):
            return es.enter_context(nc.sbuf_tensor(name, shape, dt))

        ident = sb("c_ident", [128, 128], BF16)
        b_id = Buf("c_const")
        p.dma("sp", L("c2"), lambda e: e.dma_start(out=ident[:], in_=T["ident"]), pwrites=[b_id])
        stage = Ring(es, nc, "c_stage", [128, 1024], F32, 3)
        n = 0
        wbn, b_wbn, n = load_weight_bf16(p, es, nc, "c_wbn", T["w_branch_na"], 512, 1024, stage, n)
        wbd, b_wbd, n = load_weight_bf16(p, es, nc, "c_wbd", T["w_branch_dil"], 256, 1024, stage, n)
        wo, b_wo, n = load_weight_bf16(p, es, nc, "c_wo", T["w_out"], 1024, 1024, stage, n)
        xr = Ring(es, nc, "c_x", [128, D], F32, 3)
        onr = Ring(es, nc, "c_on", [128, 512], BF16, 2)
        odr = Ring(es, nc, "c_od", [128, 3, 260], F32, 2)
        sgr = Ring(es, nc, "c_sg", [128, 2048], BF16, 2)
        ods = Ring(es, nc, "c_ods", [128, 260], F32, 2)
        rcr = Ring(es, nc, "c_rc", [128, 4], F32, 2)
        odn = Ring(es, nc, "c_odn", [128, 256], BF16, 2)
        aT = Ring(es, nc, "c_aT", [128, 6, 128], BF16, 2)
        m1 = Ring(es, nc, "c_m1", [128, D], F32, 2)
        m2 = Ring(es, nc, "c_m2", [128, D], F32, 2)
        mg = Ring(es, nc, "c_mg", [128, D], BF16, 2)
        mT = Ring(es, nc, "c_mT", [128, 8, 128], BF16, 2)
        x1r = Ring(es, nc, "c_x1", [128, D], F32, 2)
        ptp = Ring(es, nc, "c_ptp", [128, 8, 128], BF16, 2, psum=True)
        pa = Ring(es, nc, "c_pa", [128, 512], F32, 2, psum=True)
        pd = Ring(es, nc, "c_pd", [128, 512], F32, 2, psum=True)
        py = Ring(es, nc, "c_py", [128, 512], F32, 2, psum=True)

        def loads(i):
            xt, xb, xi = xr.next()
            p.dma("sp", L(f"cx{xi}"), lambda e: e.dma_start(out=xt[:], in_=T["x"][128 * i:128 * i + 128, :]), writes=[xb])
            ot, ob, oi = onr.next()
            p.dma("sp", L(f"con{oi}"), lambda e: e.dma_start(out=ot[:], in_=T["ona_s"][128 * i:128 * i + 128, :]), reads=[T["b_ona"]], writes=[ob])
            dt_, db, di = odr.next()
            p.dma("sp", L(f"cod{di}"), lambda e: e.dma_start(out=dt_[:], in_=T["odil_s"][:, 128 * i:128 * i + 128, :].rearrange("g p c -> p g c")), reads=[T["b_odil"]], writes=[db])
            st_, sb_, si = sgr.next()
            p.dma("sp", L(f"csg{si}"), lambda e: e.dma_start(out=st_[:], in_=T["sg_s"][128 * i:128 * i + 128, :]), reads=[T["b_sg"]], writes=[sb_])
            return (xt, xb, ot, ob, dt_, db, st_, sb_)

        pend = [None]
        nxt = loads(0)
        for i in range(NT):
            xt, xb, ot, ob, dt_, db, sgt, sgb = nxt
            if i + 1 < NT:
                nxt = loads(i + 1)
            odt, odb, _ = ods.next()
            p.dve(lambda e, odt=odt, dt_=dt_: e.tensor_tensor(out=odt[:], in0=dt_[:, 0, :], in1=dt_[:, 1, :], op=ALU.add), reads=[db], writes=[odb])
            p.dve(lambda e, odt=odt, dt_=dt_: e.tensor_tensor(out=odt[:], in0=odt[:], in1=dt_[:, 2, :], op=ALU.add), reads=[db, odb], writes=[odb])
            rc, rcb, _ = rcr.next()
            od3 = odt[:].rearrange("p (a b) -> p a b", b=65)
            p.dve(lambda e, rc=rc, od3=od3: e.reciprocal(out=rc[:], in_=od3[:, :, 64]), reads=[odb], writes=[rcb])
            on_, onb, _ = odn.next()
            p.dve(lambda e, on_=on_, od3=od3, rc=rc: e.tensor_tensor(out=on_[:].rearrange("p (a b) -> p a b", b=64), in0=od3[:, :, 0:64], in1=rc[:].unsqueeze(2).to_broadcast([128, 4, 64]), op=ALU.mult), reads=[odb, rcb], writes=[onb])
            pt, ptb, _ = ptp.next()
            transposes(p, pt, ptb, ot, ob, ident, b_id, 4, 0)
            transposes(p, pt, ptb, on_, onb, ident, b_id, 2, 4)
            at, atb, _ = aT.next()
            p.act(lambda e, at=at, pt=pt: e.activation(out=at[:], in_=pt[:, 0:6, :], func=AF.Copy), reads=[ptb], writes=[atb])
            pas, pds = [], []
            for half in range(2):
                pat, pab, _ = pa.next()
                for k in range(4):
                    kw = dict(writes=[pab]) if k == 0 else dict(pwrites=[pab])
                    p.pe(lambda e, pat=pat, at=at, k=k, half=half: e.matmul(pat[:], lhsT=at[:, k, :], rhs=wbn[:, k, 512 * half:512 * half + 512], start=(k == 0), stop=(k == 3)), reads=[atb, b_wbn], sig=(k == 3), **kw)
                pas.append((pat, pab))
            for half in range(2):
                pdt, pdb, _ = pd.next()
                for k in range(2):
                    kw = dict(writes=[pdb]) if k == 0 else dict(pwrites=[pdb])
                    p.pe(lambda e, pdt=pdt, at=at, k=k, half=half: e.matmul(pdt[:], lhsT=at[:, 4 + k, :], rhs=wbd[:, k, 512 * half:512 * half + 512], start=(k == 0), stop=(k == 1)), reads=[atb, b_wbd], sig=(k == 1), **kw)
                pds.append((pdt, pdb))
            m1t, m1b, _ = m1.next()
            m2t, m2b, _ = m2.next()
            for half in range(2):
                pat, pab = pas[half]
                pdt, pdb = pds[half]
                p.dve(lambda e, m1t=m1t, pat=pat, sgt=sgt, half=half: e.tensor_tensor(out=m1t[:, 512 * half:512 * half + 512], in0=pat[:], in1=sgt[:, 512 * half:512 * half + 512], op=ALU.mult), reads=[pab, sgb], pwrites=[m1b])
                p.dve(lambda e, m2t=m2t, pdt=pdt, sgt=sgt, half=half: e.tensor_tensor(out=m2t[:, 512 * half:512 * half + 512], in0=pdt[:], in1=sgt[:, 1024 + 512 * half:1024 + 512 * half + 512], op=ALU.mult), reads=[pdb, sgb], pwrites=[m2b])
            mgt, mgb, _ = mg.next()
            p.pool(lambda e, mgt=mgt, m1t=m1t, m2t=m2t: e.tensor_tensor(out=mgt[:], in0=m1t[:], in1=m2t[:], op=ALU.add), reads=[m1b, m2b], writes=[mgb])
            def stage2(i=i, xt=xt, xb=xb, mgt=mgt, mgb=mgb):
                pt2, ptb2, _ = ptp.next()
                transposes(p, pt2, ptb2, mgt, mgb, ident, b_id, 8, 0)
                mTt, mTb, _ = mT.next()
                p.act(lambda e, mTt=mTt, pt2=pt2: e.activation(out=mTt[:], in_=pt2[:], func=AF.Copy), reads=[ptb2], writes=[mTb])
                x1t, x1b, x1i = x1r.next()
                for half in range(2):
                    pyt, pyb, _ = py.next()
                    for k in range(8):
                        kw = dict(writes=[pyb]) if k == 0 else dict(pwrites=[pyb])
                        p.pe(lambda e, pyt=pyt, mTt=mTt, k=k, half=half: e.matmul(pyt[:], lhsT=mTt[:, k, :], rhs=wo[:, k, 512 * half:512 * half + 512], start=(k == 0), stop=(k == 7)), reads=[mTb, b_wo], sig=(k == 7), **kw)
                    p.dve(lambda e, x1t=x1t, pyt=pyt, xt=xt, half=half: e.tensor_tensor(out=x1t[:, 512 * half:512 * half + 512], in0=pyt[:], in1=xt[:, 512 * half:512 * half + 512], op=ALU.add), reads=[pyb, xb], pwrites=[x1b])
                p.dma("pool", L(f"cst{x1i}"), lambda e, x1t=x1t, i=i: e.dma_start(out=T["x1_s"][128 * i:128 * i + 128, :], in_=x1t[:]), reads=[x1b], pwrites=[T["b_x1"]])
            if pend[0] is not None:
                pend[0]()
            pend[0] = stage2
        pend[0]()
        p.drain("sp")
        p.flush()


def p_chunks(nc, p, T, es):
    L = p.lane
    sin = Ring(es, nc, "p_in", [128, 2, D], F32, 2)
    sout = Ring(es, nc, "p_out", [128, 2, D], BF16, 2)
    n = 0
    for j in range(64):
        for ti, tbl in enumerate((T["peer_expert_u"], T["peer_expert_v"])):
            it, ib, ii = sin.next()
            ot, ob, oi = sout.next()
            p.dma("sp", L(f"pi{ii}"), lambda e, it=it, tbl=tbl, j=j: e.dma_start(out=it[:], in_=tbl[256 * j:256 * j + 256, :].rearrange("(a q) c -> q a c", q=128)), writes=[ib])
            if n % 2 == 0:
                p.dve(lambda e, it=it, ot=ot: e.tensor_copy(out=ot[:], in_=it[:]), reads=[ib], writes=[ob])
            else:
                p.act(lambda e, it=it, ot=ot: e.activation(out=ot[:], in_=it[:], func=AF.Copy), reads=[ib], writes=[ob])
            n += 1
            p.dma("pool", L(f"po{oi}"), lambda e, ot=ot, ti=ti, j=j: e.dma_start(out=T["uv_s"][256 * j:256 * j + 256, 1024 * ti:1024 * ti + 1024].rearrange("(a q) c -> q a c", q=128), in_=ot[:]), reads=[ob], pwrites=[T["b_uv"]])
            yield


def phase_d(nc, p, T):
    L = p.lane
    U = T["peer_expert_u"]
    V = T["peer_expert_v"]
    ntiles = int(os.environ.get("KD_TILES", NT))
    with ExitStack() as es:
        def sb(name, shape, dt):
            return es.enter_context(nc.sbuf_tensor(name, shape, dt))

        ident = sb("d_ident", [128, 128], BF16)
        gffn = sb("d_gffn", [128, D], F32)
        gple = sb("d_gple", [128, D], F32)
        iota = sb("d_iota", [128, 16], F32)
        sk = sb("d_sk", [128, 2, 128], F32)
        skb = sb("d_skb", [128, 2, 128], BF16)
        kT = sb("d_kT", [128, 2, 128], BF16)
        b_c = Buf("d_const")
        b_sk = Buf("d_sk")
        b_kT = Buf("d_kT")
        p.dma("sp", L("c3"), lambda e: e.dma_start(out=ident[:], in_=T["ident"]), pwrites=[b_c])
        p.dma("sp", L("c3"), lambda e: e.dma_start(out=gffn[:], in_=T["norm_ffn"].partition_broadcast(128)), pwrites=[b_c])
        p.dma("sp", L("c3"), lambda e: e.dma_start(out=gple[:], in_=T["norm_ple"].partition_broadcast(128)), pwrites=[b_c])
        p.dma("sp", L("c3"), lambda e: e.dma_start(out=iota[:], in_=T["iota16"]), pwrites=[b_c])
        p.dma("sp", L("c3"), lambda e: e.dma_start(out=sk[:], in_=T["peer_sub_keys"].rearrange("s n c -> n s c")), pwrites=[b_c])
        NG = int(os.environ.get("KNG", 14))
        uvg = Ring(es, nc, "d_uvg", [128, 2 * D], BF16, NG)
        dgr = Ring(es, nc, "d_dg", [128, 128], BF16, 4)
        class _Stage:
            def __init__(self):
                self.i = -1

            def next(self):
                self.i = (self.i + 1) % 6
                return uvg.t[self.i][:].bitcast(F32), uvg.b[self.i], self.i
        stage = _Stage()
        a_bufs = [Buf(f"a{i}") for i in range(128)]
        w_bufs = [Buf(f"w{i}") for i in range(128)]
        n = 0
        wq, b_wq, n = load_weight_bf16(p, es, nc, "d_wq", T["peer_w_query"], 1024, 2048, stage, n)
        wpg, b_wpg, n = load_weight_bf16(p, es, nc, "d_wpg", T["w_ple_gate"], 1024, 1024, stage, n)
        wpl, b_wpl, n = load_weight_bf16(p, es, nc, "d_wpl", T["w_ple"], 256, 1024, stage, n)

        x1r = Ring(es, nc, "d_x1", [128, D], F32, 2)
        ppr = Ring(es, nc, "d_pp", [128, 256], F32, 2)
        junk = Ring(es, nc, "d_junk", [128, D], BF16, 1)
        st = Ring(es, nc, "d_st", [128, 8], F32, 2)
        h2r = Ring(es, nc, "d_h2", [128, D], F32, 1)
        junka = Ring(es, nc, "d_junka", [128, D], BF16, 1)
        hbr = Ring(es, nc, "d_hb", [128, D], BF16, 2)
        h3r = Ring(es, nc, "d_h3", [128, D], BF16, 1)
        prod = Ring(es, nc, "d_prod", [128, D], BF16, 3)
        junkb = Ring(es, nc, "d_junkb", [128, D], BF16, 1)
        hTr = Ring(es, nc, "d_hT", [128, 8, 128], BF16, 1)
        qpr = Ring(es, nc, "d_qp", [128, 16, 128], BF16, 1)
        scr = Ring(es, nc, "d_sc", [128, 16, 128], F32, 1)
        w1r = Ring(es, nc, "d_w1", [128, 128], F32, 4)
        cdr = Ring(es, nc, "d_cd", [128, 256], F32, 2)
        cd2r = Ring(es, nc, "d_cd2", [128, 256], F32, 2)
        dummy = sb("d_dummy", [128, 8], F32)
        CB = [[[Buf(f"cb{q_}_{h_}_{w_}") for w_ in range(3)] for h_ in range(8)] for q_ in range(2)]
        TK = []
        for q_ in range(2):
            TK.append((sb(f"d_tk{q_}", [128, 12, 128], F32), sb(f"d_tku{q_}", [128, 5, 128], U32), Buf(f"d_tk{q_}"), sb(f"d_sm{q_}", [128, 4, 8], F32), Buf(f"d_sm{q_}")))
        oh = scr.t[0][:].rearrange("p (h k) c -> p h k c", k=2).rearrange("p h k (a b) -> p h (k a) b", b=16)
        b_oh = scr.b[0]
        eidx = Ring(es, nc, "d_eidx", [128, 128], I32, 2)
        av = Ring(es, nc, "d_av", [128, 128], F32, 1)
        gav = Ring(es, nc, "d_gav", [128, 128], F32, 1)
        g3r = Ring(es, nc, "d_g3", [128, D], F32, 1)
        pbr = Ring(es, nc, "d_pb", [128, 256], BF16, 1)
        pTr = Ring(es, nc, "d_pT", [128, 2, 128], BF16, 1)
        outr = Ring(es, nc, "d_out", [128, D], F32, 1)
        ptp = Ring(es, nc, "d_ptp", [128, 8, 128], BF16, 1, psum=True)
        pq = Ring(es, nc, "d_pq", [128, 512], F32, 2, psum=True)
        psc = Ring(es, nc, "d_psc", [128, 512], F32, 2, psum=True)
        pyr = Ring(es, nc, "d_py", [128, 512], F32, 2, psum=True)

        if os.environ.get("KDEBUG"):
            print("phase D sbuf remaining", nc.sbuf_bytes_remaining)
        p.dve(lambda e: e.tensor_copy(out=skb[:], in_=sk[:]), reads=[b_c], writes=[b_sk])
        pt, ptb, _ = ptp.next()
        for s_ in range(2):
            kw = dict(writes=[ptb]) if s_ == 0 else dict(pwrites=[ptb])
            p.pe(lambda e, s_=s_, pt=pt: e.transpose(out=pt[:, s_, :], in_=skb[:, s_, :], identity=ident[:]), reads=[b_sk, b_c], sig=(s_ == 1), **kw)
        p.dve(lambda e, pt=pt: e.tensor_copy(out=kT[:], in_=pt[:, 0:2, :]), reads=[ptb], writes=[b_kT])

        def loads(i):
            xt, xb, xi = x1r.next()
            p.dma("sp", L(f"dx{xi}"), lambda e: e.dma_start(out=xt[:], in_=T["x1_s"][128 * i:128 * i + 128, :]), reads=[T["b_x1"]], writes=[xb])
            pp, ppb, pi = ppr.next()
            p.dma("sp", L(f"dp{pi}"), lambda e: e.dma_start(out=pp[:], in_=T["p"][128 * i:128 * i + 128, :]), writes=[ppb])
            return xt, xb, pp, ppb

        def rmsnorm(xt, xb, gain, out_t, out_b):
            jt, jb, _ = junka.next()
            stt, stb, _ = st.next()
            p.act(lambda e: e.activation(out=jt[:], in_=xt[:], func=AF.Square, accum_out=stt[:, 0:1]), reads=[xb], writes=[jb, stb])
            p.act(lambda e: e.activation(out=stt[:, 1:2], in_=stt[:, 0:1], func=AF.Sqrt, scale=1.0 / D, bias=EPS), reads=[stb], pwrites=[stb])
            p.dve(lambda e: e.reciprocal(out=stt[:, 2:3], in_=stt[:, 1:2]), reads=[stb], pwrites=[stb])
            p.dve(lambda e: e.scalar_tensor_tensor(out=out_t[:], in0=xt[:], scalar=stt[:, 2:3], in1=gain[:], op0=ALU.mult, op1=ALU.mult), reads=[xb, stb, b_c], writes=[out_b])

        def front(i):
            xt, xb, pp, ppb = loads(i)
            tk, tku, b_tk, sm, b_sm = TK[i % 2]
            yield
            lset = i % 4
            h2, h2b_, _ = h2r.next()
            rmsnorm(xt, xb, gffn, h2, h2b_)
            yield
            hb, hbb, _ = hbr.next()
            p.act(lambda e, hb=hb, h2=h2: e.activation(out=hb[:], in_=h2[:], func=AF.Copy), reads=[h2b_], writes=[hbb])
            yield
            pt, ptb, _ = ptp.next()
            transposes(p, pt, ptb, hb, hbb, ident, b_c, 8, 0)
            yield
            hT, hTb, _ = hTr.next()
            p.act(lambda e, hT=hT, pt=pt: e.activation(out=hT[:], in_=pt[:], func=AF.Copy), reads=[ptb], writes=[hTb])
            yield
            qp, qpb, _ = qpr.next()
            for rnd in range(4):
                pqt, pqb, _ = pq.next()
                for c4 in range(4):
                    cc = 4 * rnd + c4
                    for k in range(8):
                        kw = dict(writes=[pqb]) if (c4 == 0 and k == 0) else dict(pwrites=[pqb])
                        p.pe(lambda e, pqt=pqt, c4=c4, cc=cc, k=k, hT=hT: e.matmul(pqt[:, 128 * c4:128 * c4 + 128], lhsT=wq[:, k, 128 * cc:128 * cc + 128], rhs=hT[:, k, :], start=(k == 0), stop=(k == 7)), reads=[hTb, b_wq], sig=(c4 == 3 and k == 7), **kw)
                        yield
                if rnd % 2 == 0:
                    p.dve(lambda e, qp=qp, pqt=pqt, rnd=rnd: e.tensor_copy(out=qp[:, 4 * rnd:4 * rnd + 4, :], in_=pqt[:].rearrange("p (a b) -> p a b", b=128)), reads=[pqb], pwrites=[qpb])
                    yield
                else:
                    p.act(lambda e, qp=qp, pqt=pqt, rnd=rnd: e.activation(out=qp[:, 4 * rnd:4 * rnd + 4, :], in_=pqt[:].rearrange("p (a b) -> p a b", b=128), func=AF.Copy), reads=[pqb], pwrites=[qpb])
                    yield
            sc, scb, _ = scr.next()
            for rnd in range(4):
                pst, psb, _ = psc.next()
                for c4 in range(4):
                    cc = 4 * rnd + c4
                    kw = dict(writes=[psb]) if c4 == 0 else dict(pwrites=[psb])
                    p.pe(lambda e, pst=pst, c4=c4, cc=cc, qp=qp: e.matmul(pst[:, 128 * c4:128 * c4 + 128], lhsT=qp[:, cc, :], rhs=kT[:, cc % 2, :], start=True, stop=True), reads=[qpb, b_kT], sig=(c4 == 3), **kw)
                    yield
                if rnd % 2 == 0:
                    p.act(lambda e, sc=sc, pst=pst, rnd=rnd: e.activation(out=sc[:, 4 * rnd:4 * rnd + 4, :], in_=pst[:].rearrange("p (a b) -> p a b", b=128), func=AF.Copy), reads=[psb], pwrites=[scb])
                    yield
                else:
                    p.dve(lambda e, sc=sc, pst=pst, rnd=rnd: e.tensor_copy(out=sc[:, 4 * rnd:4 * rnd + 4, :], in_=pst[:].rearrange("p (a b) -> p a b", b=128)), reads=[psb], pwrites=[scb])
                    yield
            def top16_ops(src_ap, srcb, vals, idxs, wring, cb):
                wt, wb_, _ = wring.next()
                return [
                    lambda: p.dve(lambda e: e.max(out=vals[:, 0:8], in_=src_ap), reads=[srcb], pwrites=[cb]),
                    lambda: p.dve(lambda e: e.max_index(out=idxs[:, 0:8], in_max=vals[:, 0:8], in_values=src_ap), reads=[srcb, cb], pwrites=[cb]),
                    lambda: p.dve(lambda e: e.match_replace(out=wt[:], in_to_replace=vals[:, 0:8], in_values=src_ap, imm_value=-1e30), reads=[srcb, cb], writes=[wb_]),
                    lambda: p.dve(lambda e: e.max(out=vals[:, 8:16], in_=wt[:]), reads=[wb_], pwrites=[cb]),
                    lambda: p.dve(lambda e: e.max_index(out=idxs[:, 8:16], in_max=vals[:, 8:16], in_values=wt[:]), reads=[wb_, cb], pwrites=[cb]),
                ]

            chain_bufs = []
            prev2 = None
            for hh in range(9):
                chains = []
                if hh < 8:
                    cb1, cb2 = CB[i % 2][hh][0], CB[i % 2][hh][1]
                    chains.append(top16_ops(sc[:, 2 * hh, :], scb, tk[:, 0, 16 * hh:16 * hh + 16], tku[:, 0, 16 * hh:16 * hh + 16], w1r, cb1))
                    chains.append(top16_ops(sc[:, 2 * hh + 1, :], scb, tk[:, 1, 16 * hh:16 * hh + 16], tku[:, 1, 16 * hh:16 * hh + 16], w1r, cb2))
                if prev2 is not None:
                    chains.append(prev2)
                for k_ in range(5):
                    for ch in chains:
                        ch[k_]()
                    yield "dve"
                if hh < 8:
                    cd, cdb, _ = cdr.next()
                    p.dve(lambda e, cd=cd, hh=hh: e.tensor_tensor(out=cd[:].rearrange("p (a b) -> p a b", b=16), in0=tk[:, 0, 16 * hh:16 * hh + 16].unsqueeze(2).to_broadcast([128, 16, 16]), in1=tk[:, 1, 16 * hh:16 * hh + 16].unsqueeze(1).to_broadcast([128, 16, 16]), op=ALU.add), reads=[cb1, cb2], writes=[cdb])
                    cb3 = CB[i % 2][hh][2]
                    prev2 = top16_ops(cd[:], cdb, tk[:, 2, 16 * hh:16 * hh + 16], tku[:, 2, 16 * hh:16 * hh + 16], cd2r, cb3)
                    chain_bufs += [cb1, cb2, cb3]
                    yield "dve"
                else:
                    prev2 = None
            p.dve(lambda e: e.memset(dummy[:, 0:1], 0.0), reads=chain_bufs, pwrites=[b_tk])
            yield "dve"
            p.dve(lambda e: e.tensor_single_scalar(out=tku[:, 3, :], in_=tku[:, 2, :], scalar=4, op=ALU.logical_shift_right), reads=[b_tk], pwrites=[b_tk])
            yield "dve"
            p.dve(lambda e: e.tensor_single_scalar(out=tku[:, 4, :], in_=tku[:, 2, :], scalar=15, op=ALU.bitwise_and), reads=[b_tk], pwrites=[b_tk])
            yield "dve"
            p.dve(lambda e: e.tensor_copy(out=tk[:, 3:5, :], in_=tku[:, 0:2, :]), reads=[b_tk], pwrites=[b_tk])
            yield "dve"
            p.dve(lambda e: e.tensor_copy(out=tk[:, 5:7, :], in_=tku[:, 3:5, :]), reads=[b_tk], pwrites=[b_tk])
            yield "dve"
            for w in range(2):
                sel = tk[:, 5 + w, :].rearrange("p (h k) -> p h k", k=16).unsqueeze(3).to_broadcast([128, 8, 16, 16])
                tab = tk[:, 3 + w, :].rearrange("p (h a) -> p h a", a=16).unsqueeze(2).to_broadcast([128, 8, 16, 16])
                io = iota[:].unsqueeze(1).unsqueeze(1).to_broadcast([128, 8, 16, 16])
                p.dve(lambda e, sel=sel, io=io: e.tensor_tensor(out=oh, in0=sel, in1=io, op=ALU.is_equal), reads=[b_tk, b_c], writes=[b_oh])
                yield "dve"
                p.dve(lambda e, tab=tab: e.tensor_tensor(out=oh, in0=oh, in1=tab, op=ALU.mult), reads=[b_tk, b_oh], writes=[b_oh])
                yield "dve"
                p.dve(lambda e, w=w: e.tensor_reduce(out=tk[:, 7 + w, :], in_=oh.rearrange("p h k a -> p (h k) a"), axis=AX.X, op=ALU.add), reads=[b_oh], pwrites=[b_tk])
                yield "dve"
            p.dve(lambda e: e.scalar_tensor_tensor(out=tk[:, 11, :], in0=tk[:, 7, :], scalar=128.0, in1=tk[:, 8, :], op0=ALU.mult, op1=ALU.add), reads=[b_tk], pwrites=[b_tk])
            yield "dve"
            ei, eib, _ = eidx.next()
            p.dve(lambda e, ei=ei: e.tensor_copy(out=ei[:], in_=tk[:, 11, :]), reads=[b_tk], writes=[eib])
            yield "dve"
            sc3 = tk[:, 2, :].rearrange("p (h k) -> p h k", k=16)
            p.dve(lambda e, sc3=sc3: e.tensor_tensor(out=tk[:, 9, :].rearrange("p (h k) -> p h k", k=16), in0=sc3, in1=sc3[:, :, 0:1].to_broadcast([128, 8, 16]), op=ALU.subtract), reads=[b_tk], pwrites=[b_tk])
            yield "dve"
            p.act(lambda e: e.activation(out=tk[:, 9, :], in_=tk[:, 9, :], func=AF.Exp), reads=[b_tk], pwrites=[b_tk])
            yield "dve"
            p.dve(lambda e: e.tensor_reduce(out=sm[:, 0, :], in_=tk[:, 9, :].rearrange("p (h k) -> p h k", k=16), axis=AX.X, op=ALU.add), reads=[b_tk], writes=[b_sm])
            yield "dve"
            p.dve(lambda e: e.reciprocal(out=sm[:, 1, :], in_=sm[:, 0, :]), reads=[b_sm], pwrites=[b_sm])
            yield "dve"
            p.dve(lambda e: e.tensor_tensor(out=tk[:, 10, :].rearrange("p (h k) -> p h k", k=16), in0=tk[:, 9, :].rearrange("p (h k) -> p h k", k=16), in1=sm[:, 1, :].unsqueeze(2).to_broadcast([128, 8, 16]), op=ALU.mult), reads=[b_tk, b_sm], pwrites=[b_tk])
            yield "dve"

            F[i] = dict(xt=xt, xb=xb, pp=pp, ppb=ppb, h2=h2, h2b_=h2b_, ei=ei, eib=eib, tk=tk, b_tk=b_tk, hb2=hb, hb2b=hbb)

        F = {}
        for _ in front(0):
            pass
        for i in range(ntiles):
            gen = front(i + 1) if i + 1 < ntiles else iter(())
            gphase = [0]
            fs = F.pop(i)
            xt, xb, pp, ppb, h2, h2b_, ei, eib, tk, b_tk, hb2, hb2b = (fs[k] for k in ("xt", "xb", "pp", "ppb", "h2", "h2b_", "ei", "eib", "tk", "b_tk", "hb2", "hb2b"))
            lset = i % 4
            a_t, _, _ = av.next()
            ga_t, _, _ = gav.next()
            py0, py0b, _ = pyr.next()
            py1, py1b, _ = pyr.next()
            slots = {}
            SK = 2
            for s_ in range(128 + SK):
                if s_ < 128:
                    gt, gb, gi = uvg.next()
                    slots[s_] = (gt, gb)
                    p.dma("pool", L(f"dg{gi}_{lset % 2}"), lambda e, gt=gt, s_=s_, ei=ei: e.indirect_dma_start(out=gt[:], out_offset=None, in_=T["uv_s"], in_offset=bass.IndirectOffsetOnAxis(ap=ei[:, s_:s_ + 1], axis=0)), reads=[eib, T["b_uv"]], writes=[gb])
                    pr_, prb, _ = prod.next()
                    p.dve(lambda e, pr_=pr_, gt=gt, hb2=hb2: e.tensor_tensor(out=pr_[:], in0=gt[:, 0:D], in1=hb2[:], op=ALU.mult), reads=[gb, hb2b], writes=[prb])
                    j2, j2b, _ = junkb.next()
                    p.act(lambda e, j2=j2, pr_=pr_, s_=s_, a_t=a_t: e.activation(out=j2[:], in_=pr_[:], func=AF.Copy, accum_out=a_t[:, s_:s_ + 1]), reads=[prb], writes=[j2b, a_bufs[s_]])
                    p.act(lambda e, ga_t=ga_t, a_t=a_t, s_=s_: e.activation(out=ga_t[:, s_:s_ + 1], in_=a_t[:, s_:s_ + 1], func=AF.Gelu), reads=[a_bufs[s_]], writes=[w_bufs[s_]])
                if s_ >= SK:
                    z = s_ - SK
                    gt, gb = slots.pop(z)
                    dg, dgb, _ = dgr.next()
                    p.dve(lambda e, dg=dg, ga_t=ga_t, z=z, tk=tk: e.tensor_scalar(out=dg[:], in0=ident[:], scalar1=ga_t[:, z:z + 1], scalar2=tk[:, 10, z:z + 1], op0=ALU.mult, op1=ALU.mult), reads=[w_bufs[z], b_tk, b_c], writes=[dgb])
                    for half, (pyt, pyb) in enumerate(((py0, py0b), (py1, py1b))):
                        kw = dict(writes=[pyb]) if z == 0 else dict(pwrites=[pyb])
                        p.pe(lambda e, pyt=pyt, dg=dg, gt=gt, half=half, z=z: e.matmul(pyt[:], lhsT=dg[:], rhs=gt[:, D + 512 * half:D + 512 * half + 512], start=(z == 0), stop=(z == 127)), reads=[dgb, gb], sig=(half == 1), **kw)
                if gphase[0] == 0:
                    for _q in range(6):
                        if next(gen, None) == "dve":
                            gphase[0] = 1
                            break
                elif s_ % 4 != 3:
                    next(gen, None)
            for _ in gen:
                pass
            x2, x2b = xt, xb
            for half, (pyt, pyb) in enumerate(((py0, py0b), (py1, py1b))):
                p.dve(lambda e, x2=x2, pyt=pyt, xt=xt, half=half: e.tensor_tensor(out=x2[:, 512 * half:512 * half + 512], in0=pyt[:], in1=xt[:, 512 * half:512 * half + 512], op=ALU.add), reads=[pyb, xb], writes=[xb])
            h3, h3b, _ = h3r.next()
            rmsnorm(x2, x2b, gple, h3, h3b)
            pt, ptb, _ = ptp.next()
            transposes(p, pt, ptb, h3, h3b, ident, b_c, 8, 0)
            hT3, hT3b, _ = hTr.next()
            p.act(lambda e, hT3=hT3, pt=pt: e.activation(out=hT3[:], in_=pt[:], func=AF.Copy), reads=[ptb], writes=[hT3b])
            g3, g3b, _ = g3r.next()
            for half in range(2):
                pqt, pqb, _ = pq.next()
                for k in range(8):
                    kw = dict(writes=[pqb]) if k == 0 else dict(pwrites=[pqb])
                    p.pe(lambda e, pqt=pqt, hT3=hT3, k=k, half=half: e.matmul(pqt[:], lhsT=hT3[:, k, :], rhs=wpg[:, k, 512 * half:512 * half + 512], start=(k == 0), stop=(k == 7)), reads=[hT3b, b_wpg], sig=(k == 7), **kw)
                p.act(lambda e, g3=g3, pqt=pqt, half=half: e.activation(out=g3[:, 512 * half:512 * half + 512], in_=pqt[:], func=AF.Sigmoid), reads=[pqb], pwrites=[g3b])
            pb, pbb, _ = pbr.next()
            p.dve(lambda e, pb=pb, pp=pp: e.tensor_copy(out=pb[:], in_=pp[:]), reads=[ppb], writes=[pbb])
            pt, ptb, _ = ptp.next()
            transposes(p, pt, ptb, pb, pbb, ident, b_c, 2, 0)
            pT, pTb, _ = pTr.next()
            p.dve(lambda e, pT=pT, pt=pt: e.tensor_copy(out=pT[:], in_=pt[:, 0:2, :]), reads=[ptb], writes=[pTb])
            ot, otb, oi = outr.next()
            t3, t3b = ot, otb
            for half in range(2):
                pst, psb, _ = psc.next()
                for k in range(2):
                    kw = dict(writes=[psb]) if k == 0 else dict(pwrites=[psb])
                    p.pe(lambda e, pst=pst, pT=pT, k=k, half=half: e.matmul(pst[:], lhsT=pT[:, k, :], rhs=wpl[:, k, 512 * half:512 * half + 512], start=(k == 0), stop=(k == 1)), reads=[pTb, b_wpl], sig=(k == 1), **kw)
                p.dve(lambda e, t3=t3, pst=pst, g3=g3, half=half: e.tensor_tensor(out=t3[:, 512 * half:512 * half + 512], in0=pst[:], in1=g3[:, 512 * half:512 * half + 512], op=ALU.mult), reads=[psb, g3b], pwrites=[t3b])
            p.dve(lambda e, ot=ot, t3=t3, x2=x2: e.tensor_tensor(out=ot[:], in0=t3[:], in1=x2[:], op=ALU.add), reads=[t3b, x2b], writes=[otb])
            p.dma("sp", L(f"do{oi}"), lambda e, ot=ot, i=i: e.dma_start(out=T["out"][128 * i:128 * i + 128, :], in_=ot[:]), reads=[otb], pwrites=[T["b_out"]])
        p.drain("sp")
        p.flush()


def _rot_table():
    half = 8
    inv_freq = np.power(np.float32(500000.0), -np.arange(half, dtype=np.float32) * np.float32(2.0) / np.float32(16)).astype(np.float32)
    ang = np.arange(S, dtype=np.float32)[:, None] * inv_freq[None, :]
    return np.concatenate([np.cos(ang), np.sin(ang)], axis=1).astype(np.float32)


def build(debug=False):
    nc = bass.Bass("TRN2", target_bir_lowering=False)
    T = {}

    def din(name, shape, dt):
        T[name] = nc.dram_tensor(name, list(shape), dt, kind="ExternalInput").ap()

    din("x", [S, D], F32)
    din("p", [S, 256], F32)
    din("norm_mix", [1, D], F32)
    din("w_in", [D, INW], F32)
    din("qk_norm_na", [1, 2, 64], F32)
    din("qk_norm_dil", [1, 2, 64], F32)
    din("w_branch_na", [512, D], F32)
    din("w_branch_dil", [256, D], F32)
    din("w_out", [D, D], F32)
    din("norm_ffn", [1, D], F32)
    din("peer_w_query", [D, 2048], F32)
    din("peer_sub_keys", [2, 128, 128], F32)
    din("peer_expert_u", [16384, D], F32)
    din("peer_expert_v", [16384, D], F32)
    din("norm_ple", [1, D], F32)
    din("w_ple_gate", [D, D], F32)
    din("w_ple", [256, D], F32)
    din("ident", [128, 128], BF16)
    din("cs", [S, 16], F32)
    din("maskd", [128, 3, 256], BF16)
    din("cmask", [128, 64], F32)
    din("biasx", [128, 8, 14, 64], F32)
    din("iota16", [128, 16], F32)
    skind = "ExternalOutput" if debug else "Internal"
    T["qkv_s"] = nc.dram_tensor("qkv_s", [S, QKVW], BF16, kind=skind).ap()
    T["sg_s"] = nc.dram_tensor("sg_s", [S, 2048], BF16, kind=skind).ap()
    T["ona_s"] = nc.dram_tensor("ona_s", [S, 512], BF16, kind=skind).ap()
    T["odil_s"] = nc.dram_tensor("odil_s", [3, S, 260], F32, kind=skind).ap()
    T["x1_s"] = nc.dram_tensor("x1_s", [S, D], F32, kind=skind).ap()
    T["uv_s"] = nc.dram_tensor("uv_s", [16384, 2 * D], BF16, kind="Internal").ap()
    T["out"] = nc.dram_tensor("out", [S, D], F32, kind="ExternalOutput").ap()
    for k in ("qkv", "sg", "ona", "odil", "x1", "out", "uv"):
        T["b_" + k] = Buf(k + "_s")
    p = Prog(nc)
    ph = os.environ.get("KPH", "pabcd")
    if "a" in ph:
        phase_a(nc, p, T)
    if "b" in ph:
        phase_b(nc, p, T)
    if "c" in ph:
        phase_c(nc, p, T)
    if "d" in ph:
        phase_d(nc, p, T)
    p.drain("sp")
    p.flush()
    if os.environ.get("KDEBUG"):
        print("ops", p.nops, "sems", len(p.sems))
    return nc


def _masks():
    i = np.arange(128)[:, None]
    j = np.arange(128)[None, :]
    A = (j <= i)
    B = (j >= i)
    m = np.zeros((128, 3, 256), np.float32)
    m[:, 0, :128] = A & (i >= 64)
    m[:, 0, 128:] = B
    m[:, 1, :128] = A
    m[:, 1, 128:] = B
    m[:, 2, :128] = A
    m[:, 2, 128:] = B & (i < 64)
    kc = np.arange(64)[:, None]
    c = np.arange(64)[None, :]
    cs = np.clip(c - 8, 0, 48)
    cm = ((kc >= cs) & (kc < cs + 16)).astype(np.float32)
    cm = np.concatenate([cm, cm], axis=0)
    return m.astype(ml_dtypes.bfloat16), cm


def _biasx(rpb):
    kc = np.arange(64)[:, None]
    c = np.arange(64)[None, :]
    dc = np.clip(kc - c + 15, 0, 30)
    out = np.empty((2, 64, 8, 14, 64), np.float32)
    for a in range(2):
        for dr0 in range(14):
            out[a, :, :, dr0, :] = np.transpose(rpb[:, dr0 + a][:, dc], (1, 0, 2))
    return np.ascontiguousarray(out.reshape(128, 8, 14, 64))


_SHARED = None


def host_shared(inputs):
    f = lambda k: np.ascontiguousarray(np.asarray(inputs[k], np.float32))
    m = {
        "norm_mix": f("norm_mix"),
        "w_in": f("w_in")[0],
        "qk_norm_na": f("qk_norm_na"),
        "qk_norm_dil": f("qk_norm_dil"),
        "w_branch_na": f("w_branch_na")[0],
        "w_branch_dil": f("w_branch_dil")[0],
        "w_out": f("w_out")[0],
        "norm_ffn": f("norm_ffn"),
        "peer_w_query": f("peer_w_query")[0],
        "peer_sub_keys": f("peer_sub_keys")[0],
        "peer_expert_u": f("peer_expert_u")[0],
        "peer_expert_v": f("peer_expert_v")[0],
        "norm_ple": f("norm_ple"),
        "w_ple_gate": f("w_ple_gate")[0],
        "w_ple": f("w_ple")[0],
        "ident": np.eye(128).astype(ml_dtypes.bfloat16),
        "cs": _rot_table(),
        "maskd": _masks()[0],
        "cmask": _masks()[1],
        "biasx": _biasx(np.asarray(inputs["na_rel_bias"], np.float32)[0]),
        "iota16": np.tile(np.arange(16, dtype=np.float32), (128, 1)),
    }
    return m


def host_inputs(inputs, b, shared=None):
    m = dict(shared if shared is not None else host_shared(inputs))
    m["x"] = np.ascontiguousarray(np.asarray(inputs["x"], np.float32)[b])
    m["p"] = np.ascontiguousarray(np.asarray(inputs["p"], np.float32)[0, b])
    return m


def kernel(**inputs):
    nc = build()
    shared = host_shared(inputs)
    nb = np.asarray(inputs["x"]).shape[0]
    in_maps = [host_inputs(inputs, b, shared) for b in range(nb)]
    res = run_bass_kernel_spmd(nc, in_maps, core_ids=list(range(nb)))
    return np.stack([np.asarray(r["out"], np.float32) for r in res.results], axis=0)
```

```python
import os
import numpy as np
import ml_dtypes
from contextlib import ExitStack
import concourse.bass as bass
import concourse.mybir as mybir
from concourse.bass_utils import run_bass_kernel_spmd

F32 = mybir.dt.float32
BF16 = mybir.dt.bfloat16
I32 = mybir.dt.int32
U32 = mybir.dt.uint32
ALU = mybir.AluOpType
AF = mybir.ActivationFunctionType
AX = mybir.AxisListType

ENGS = ("pe", "act", "dve", "pool", "sp")

S = 4096
D = 1024
NT = S // 128
INW = 5888
QKVW = 3840
EPS = 1e-6


class Buf:
    __slots__ = ("name", "writers", "readers")

    def __init__(self, name=""):
        self.name = name
        self.writers = {}
        self.readers = {}


class Lane:
    __slots__ = ("key", "count")

    def __init__(self, key):
        self.key = key
        self.count = 0


def _upd(d, s):
    for k, v in s.items():
        if d.get(k, 0) < v:
            d[k] = v


class Prog:
    def __init__(self, nc):
        self.nc = nc
        self.cnt = {e: 0 for e in ENGS}
        self.seen = {e: {} for e in ENGS}
        self.sems = {}
        self.lanes = {}
        self.ops = {e: [] for e in ENGS}
        self.nops = 0
        for e in ENGS:
            self._sem(e)

    def _sem(self, key):
        if key not in self.sems:
            self.sems[key] = self.nc.alloc_semaphore("s_" + key)
        return self.sems[key]

    def lane(self, name):
        if name not in self.lanes:
            self.lanes[name] = Lane("L_" + name)
            self._sem("L_" + name)
        return self.lanes[name]

    def op(self, eng, fn, reads=(), writes=(), pwrites=(), sig=True, lane=None, after=()):
        deps = {}
        for b in after:
            _upd(deps, b.writers)
        for b in reads:
            _upd(deps, b.writers)
        for b in writes:
            _upd(deps, b.readers)
            _upd(deps, b.writers)
        for b in pwrites:
            _upd(deps, b.readers)
        waits = []
        seen = self.seen[eng]
        for k, v in deps.items():
            if k == "pe" and eng == "pe":
                continue
            if seen.get(k, 0) >= v:
                continue
            seen[k] = v
            waits.append((k, v))
        if lane is not None:
            lane.count += 16
            mykey, myval, inc = lane.key, lane.count, 16
        else:
            if sig:
                self.cnt[eng] += 1
                myval = self.cnt[eng]
                inc = 1
            else:
                myval = self.cnt[eng] + 1
                inc = 0
            mykey = eng
        for b in reads:
            if b.readers.get(mykey, 0) < myval:
                b.readers[mykey] = myval
        for b in writes:
            b.writers = {mykey: myval}
            b.readers = {}
        for b in pwrites:
            if b.writers.get(mykey, 0) < myval:
                b.writers[mykey] = myval
        self.ops[eng].append((waits, fn, mykey, inc))
        self.nops += 1

    def pe(self, fn, **kw):
        self.op("pe", fn, **kw)

    def act(self, fn, **kw):
        self.op("act", fn, **kw)

    def dve(self, fn, **kw):
        self.op("dve", fn, **kw)

    def pool(self, fn, **kw):
        self.op("pool", fn, **kw)

    def dma(self, q, lane, fn, **kw):
        self.op(q, fn, lane=lane, **kw)

    def flush(self):
        nc = self.nc
        ops = self.ops
        sems = self.sems

        def emit(engobj, lst):
            for waits, fn, mykey, inc in lst:
                for k, v in waits:
                    engobj.wait_ge(sems[k], v)
                if fn is None:
                    continue
                inst = fn(engobj)
                if inc:
                    inst.then_inc(sems[mykey], inc)

        with nc.Block() as block:
            @block.tensor
            def _(e):
                emit(e, ops["pe"])

            @block.scalar
            def _(e):
                emit(e, ops["act"])

            @block.vector
            def _(e):
                emit(e, ops["dve"])

            @block.gpsimd
            def _(e):
                emit(e, ops["pool"])

            @block.sync
            def _(e):
                emit(e, ops["sp"])
        self.ops = {e: [] for e in ENGS}

    def wait_all(self, eng, bufs):
        self.op(eng, None, reads=bufs, sig=False)

    def drain(self, eng="sp"):
        b = Buf("drain")
        for ln in self.lanes.values():
            if ln.count:
                b.writers[ln.key] = ln.count
        for e in ENGS:
            if self.cnt[e]:
                b.writers[e] = self.cnt[e]
        self.op(eng, None, reads=[b], sig=False)


class Ring:
    def __init__(self, es, nc, name, shape, dt, n, psum=False):
        self.t = []
        self.b = []
        for i in range(n):
            if psum:
                t = es.enter_context(nc.psum_tensor(f"{name}{i}", shape, dt))
            else:
                t = es.enter_context(nc.sbuf_tensor(f"{name}{i}", shape, dt))
            self.t.append(t)
            self.b.append(Buf(f"{name}{i}"))
        self.i = -1
        self.n = n

    def next(self):
        self.i = (self.i + 1) % self.n
        return self.t[self.i], self.b[self.i], self.i


def phase_a(nc, p, T):
    x, w_in = T["x"], T["w_in"]
    qkv_s, sg_s = T["qkv_s"], T["sg_s"]
    with ExitStack() as es:
        def sb(name, shape, dt):
            return es.enter_context(nc.sbuf_tensor(name, shape, dt))

        wb = sb("a_wb", [128, 8, INW], BF16)
        b_wb = Buf("wb")
        ident = sb("a_ident", [128, 128], BF16)
        gmix = sb("a_gmix", [128, D], F32)
        gfull = sb("a_gfull", [128, 3072], F32)
        cs = sb("a_cs", [128, NT, 16], F32)
        b_id = b_gmix = b_gfull = b_cs = Buf("a_const")
        stage = Ring(es, nc, "a_stage", [128, 1472], F32, 3)
        xr = Ring(es, nc, "a_x", [128, D], F32, 2)
        junk = Ring(es, nc, "a_junk", [128, D], BF16, 1)
        st = Ring(es, nc, "a_st", [128, 8], F32, 2)
        hb = Ring(es, nc, "a_hb", [128, D], BF16, 2)
        hT = Ring(es, nc, "a_hT", [128, 8, 128], BF16, 2)
        sq = Ring(es, nc, "a_sq", [128, 512], F32, 2)
        qst = Ring(es, nc, "a_qst", [128, 24], F32, 3)
        qn = Ring(es, nc, "a_qn", [128, 512], F32, 2)
        qg = Ring(es, nc, "a_qg", [128, 512], F32, 2)
        rt = Ring(es, nc, "a_rt", [128, 4, 8, 8], F32, 2)
        qo = Ring(es, nc, "a_qo", [128, QKVW], BF16, 2)
        so = Ring(es, nc, "a_so", [128, 2048], BF16, 2)
        ptr = Ring(es, nc, "a_ptr", [128, 8, 128], BF16, 2, psum=True)
        pmm = Ring(es, nc, "a_pmm", [128, 512], F32, 6, psum=True)
        L = p.lane

        p.dma("sp", L("c0"), lambda e: e.dma_start(out=ident[:], in_=T["ident"]), pwrites=[b_id])
        p.dma("sp", L("c0"), lambda e: e.dma_start(out=gmix[:], in_=T["norm_mix"].partition_broadcast(128)), pwrites=[b_gmix])
        p.dma("sp", L("c0"), lambda e: e.dma_start(out=cs[:], in_=T["cs"].rearrange("(n p) c -> p n c", p=128)), pwrites=[b_cs])
        first = False
        for (c0, nh, src) in ((0, 8, T["qk_norm_na"][0, 0:1, :]), (512, 8, T["qk_norm_na"][0, 1:2, :]),
                              (1536, 12, T["qk_norm_dil"][0, 0:1, :]), (2304, 12, T["qk_norm_dil"][0, 1:2, :])):
            for j in range(nh):
                cc = c0 + 64 * j
                if first:
                    p.dma("sp", L("c0"), lambda e, cc=cc, src=src: e.dma_start(out=gfull[:, cc:cc + 64], in_=src.partition_broadcast(128)), writes=[b_gfull])
                    first = False
                else:
                    p.dma("sp", L("c0"), lambda e, cc=cc, src=src: e.dma_start(out=gfull[:, cc:cc + 64], in_=src.partition_broadcast(128)), pwrites=[b_gfull])
        casters = ("dve", "pool", "act")
        n = 0
        for k in range(8):
            for q in range(4):
                stt, sbf, si = stage.next()
                p.dma("sp", L(f"stg{si}"), lambda e, stt=stt, k=k, q=q: e.dma_start(out=stt[:], in_=w_in[128 * k:128 * k + 128, 1472 * q:1472 * q + 1472]), writes=[sbf])
                eng = casters[n % 3]
                n += 1
                if eng == "act":
                    p.act(lambda e, stt=stt, k=k, q=q: e.activation(out=wb[:, k, 1472 * q:1472 * q + 1472], in_=stt[:], func=AF.Copy), reads=[sbf], pwrites=[b_wb])
                else:
                    p.op(eng, lambda e, stt=stt, k=k, q=q: e.tensor_copy(out=wb[:, k, 1472 * q:1472 * q + 1472], in_=stt[:]), reads=[sbf], pwrites=[b_wb])

        def load_x(i):
            xt, xb, xi = xr.next()
            p.dma("sp", L(f"ax{xi}"), lambda e: e.dma_start(out=xt[:], in_=x[128 * i:128 * i + 128, :]), writes=[xb])
            return xt, xb

        nxt = load_x(0)
        for i in range(NT):
            xt, xb = nxt
            if i + 1 < NT:
                nxt = load_x(i + 1)
            jt, jb, _ = junk.next()
            stt, stb, _ = st.next()
            p.act(lambda e, jt=jt, xt=xt, stt=stt: e.activation(out=jt[:], in_=xt[:], func=AF.Square, accum_out=stt[:, 0:1]), reads=[xb], writes=[jb, stb])
            p.act(lambda e, stt=stt: e.activation(out=stt[:, 1:2], in_=stt[:, 0:1], func=AF.Sqrt, scale=1.0 / D, bias=EPS), reads=[stb], pwrites=[stb])
            p.dve(lambda e, stt=stt: e.reciprocal(out=stt[:, 2:3], in_=stt[:, 1:2]), reads=[stb], pwrites=[stb])
            hbt, hbb, _ = hb.next()
            p.dve(lambda e, hbt=hbt, xt=xt, stt=stt: e.scalar_tensor_tensor(out=hbt[:], in0=xt[:], scalar=stt[:, 2:3], in1=gmix[:], op0=ALU.mult, op1=ALU.mult), reads=[xb, stb, b_gmix], writes=[hbb])
            pt, ptb, _ = ptr.next()
            for k in range(8):
                if k == 0:
                    p.pe(lambda e, pt=pt, hbt=hbt, k=k: e.transpose(out=pt[:, k, :], in_=hbt[:, 128 * k:128 * k + 128], identity=ident[:]), reads=[hbb, b_id], writes=[ptb], sig=False)
                else:
                    p.pe(lambda e, pt=pt, hbt=hbt, k=k: e.transpose(out=pt[:, k, :], in_=hbt[:, 128 * k:128 * k + 128], identity=ident[:]), reads=[hbb, b_id], pwrites=[ptb], sig=(k == 7))
            hTt, hTb, _ = hT.next()
            p.act(lambda e, hTt=hTt, pt=pt: e.activation(out=hTt[:], in_=pt[:], func=AF.Copy), reads=[ptb], writes=[hTb])
            qot, qob, qoi = qo.next()
            sot, sob, soi = so.next()
            first_q = True
            first_s = True
            for blk in range(12):
                c0 = 512 * blk
                w = 256 if blk == 7 else 512
                if blk >= 8:
                    c0 = QKVW + 512 * (blk - 8)
                pm, pmb, _ = pmm.next()
                for k in range(8):
                    if k == 0:
                        p.pe(lambda e, pm=pm, hTt=hTt, k=k, c0=c0, w=w: e.matmul(pm[:, 0:w], lhsT=hTt[:, k, :], rhs=wb[:, k, c0:c0 + w], start=True, stop=False), reads=[hTb, b_wb], writes=[pmb], sig=False)
                    else:
                        p.pe(lambda e, pm=pm, hTt=hTt, k=k, c0=c0, w=w: e.matmul(pm[:, 0:w], lhsT=hTt[:, k, :], rhs=wb[:, k, c0:c0 + w], start=False, stop=(k == 7)), reads=[hTb, b_wb], pwrites=[pmb], sig=(k == 7))
                qkw = dict(pwrites=[qob])
                if blk in (0, 1, 3, 4, 5):
                    sqt, sqb, _ = sq.next()
                    qs, qsb, _ = qst.next()
                    p.act(lambda e, sqt=sqt, pm=pm: e.activation(out=sqt[:], in_=pm[:], func=AF.Square), reads=[pmb], writes=[sqb])
                    p.dve(lambda e, qs=qs, sqt=sqt: e.tensor_reduce(out=qs[:, 0:8], in_=sqt[:].rearrange("p (a b) -> p a b", b=64), axis=AX.X, op=ALU.add), reads=[sqb], writes=[qsb])
                    p.act(lambda e, qs=qs: e.activation(out=qs[:, 8:16], in_=qs[:, 0:8], func=AF.Sqrt, scale=1.0 / 64, bias=EPS), reads=[qsb], pwrites=[qsb])
                    p.dve(lambda e, qs=qs: e.reciprocal(out=qs[:, 16:24], in_=qs[:, 8:16]), reads=[qsb], pwrites=[qsb])
                    qnt, qnb, _ = qn.next()
                    p.dve(lambda e, qnt=qnt, pm=pm, qs=qs: e.tensor_tensor(out=qnt[:].rearrange("p (a b) -> p a b", b=64), in0=pm[:].rearrange("p (a b) -> p a b", b=64), in1=qs[:, 16:24].unsqueeze(2).to_broadcast([128, 8, 64]), op=ALU.mult), reads=[pmb, qsb], writes=[qnb])
                    if blk < 2:
                        p.dve(lambda e, qot=qot, qnt=qnt, c0=c0: e.tensor_tensor(out=qot[:, c0:c0 + 512], in0=qnt[:], in1=gfull[:, c0:c0 + 512], op=ALU.mult), reads=[qnb, b_gfull], **qkw)
                    else:
                        qgt, qgb, _ = qg.next()
                        p.dve(lambda e, qgt=qgt, qnt=qnt, c0=c0: e.tensor_tensor(out=qgt[:], in0=qnt[:], in1=gfull[:, c0:c0 + 512], op=ALU.mult), reads=[qnb, b_gfull], writes=[qgb])
                        q3 = qgt[:].rearrange("p (a b) -> p a b", b=64)
                        o3 = qot[:, c0:c0 + 512].rearrange("p (a b) -> p a b", b=64)
                        rtt, rtb, _ = rt.next()
                        cosb = cs[:, i, 0:8].unsqueeze(1).to_broadcast([128, 8, 8])
                        sinb = cs[:, i, 8:16].unsqueeze(1).to_broadcast([128, 8, 8])
                        p.pool(lambda e, rtt=rtt, q3=q3, cosb=cosb: e.tensor_tensor(out=rtt[:, 0], in0=q3[:, :, 0:8], in1=cosb, op=ALU.mult), reads=[qgb, b_cs], writes=[rtb])
                        p.pool(lambda e, rtt=rtt, q3=q3, sinb=sinb: e.tensor_tensor(out=rtt[:, 1], in0=q3[:, :, 8:16], in1=sinb, op=ALU.mult), reads=[qgb, b_cs], pwrites=[rtb])
                        p.pool(lambda e, rtt=rtt, q3=q3, cosb=cosb: e.tensor_tensor(out=rtt[:, 2], in0=q3[:, :, 8:16], in1=cosb, op=ALU.mult), reads=[qgb, b_cs], pwrites=[rtb])
                        p.pool(lambda e, rtt=rtt, q3=q3, sinb=sinb: e.tensor_tensor(out=rtt[:, 3], in0=q3[:, :, 0:8], in1=sinb, op=ALU.mult), reads=[qgb, b_cs], pwrites=[rtb])
                        p.pool(lambda e, rtt=rtt, o3=o3: e.tensor_tensor(out=o3[:, :, 0:8], in0=rtt[:, 0], in1=rtt[:, 1], op=ALU.subtract), reads=[rtb], **qkw)
                        p.pool(lambda e, rtt=rtt, o3=o3: e.tensor_tensor(out=o3[:, :, 8:16], in0=rtt[:, 2], in1=rtt[:, 3], op=ALU.add), reads=[rtb], pwrites=[qob])
                        p.pool(lambda e, q3=q3, o3=o3: e.tensor_copy(out=o3[:, :, 16:64], in_=q3[:, :, 16:64]), reads=[qgb], pwrites=[qob])
                    first_q = False
                elif blk in (2, 6, 7):
                    p.act(lambda e, qot=qot, pm=pm, c0=c0, w=w: e.activation(out=qot[:, c0:c0 + w], in_=pm[:, 0:w], func=AF.Copy), reads=[pmb], **qkw)
                    first_q = False
                else:
                    g0 = 512 * (blk - 8)
                    skw = dict(pwrites=[sob])
                    first_s = False
                    p.act(lambda e, sot=sot, pm=pm, g0=g0: e.activation(out=sot[:, g0:g0 + 512], in_=pm[:], func=AF.Sigmoid), reads=[pmb], **skw)
            p.dma("pool", L(f"aq{qoi}"), lambda e, qot=qot, i=i: e.dma_start(out=qkv_s[128 * i:128 * i + 128, :], in_=qot[:]), reads=[qob], pwrites=[T["b_qkv"]])
            p.dma("pool", L(f"as{soi}"), lambda e, sot=sot, i=i: e.dma_start(out=sg_s[128 * i:128 * i + 128, :], in_=sot[:]), reads=[sob], pwrites=[T["b_sg"]])
        p.drain("sp")
        p.flush()


def phase_b(nc, p, T):
    qkv_s = T["qkv_s"]
    L = p.lane
    with ExitStack() as es:
        def sb(name, shape, dt):
            return es.enter_context(nc.sbuf_tensor(name, shape, dt))

        ident = sb("b_ident", [128, 128], BF16)
        maskd = sb("b_maskd", [128, 3, 256], BF16)
        cmask = sb("b_cmask", [128, 64], F32)
        eb2 = sb("b_eb2", [128, 8, 14, 64], BF16)
        b_const = Buf("b_const")
        b_eb2 = Buf("eb2")
        qtok = sb("b_qtok", [128, NT, 256], BF16)
        ktok = sb("b_ktok", [128, NT, 256], BF16)
        b_qtok = Buf("qtok")
        b_ktok = Buf("ktok")
        qT = sb("b_qT", [128, 2, S], BF16)
        kT = sb("b_kT", [128, 2, S + 128], BF16)
        b_qT = Buf("qT")
        b_kT = Buf("kT")
        va = sb("b_va", [128, NT, 4, 65], BF16)
        vbr = [(sb("b_vb0", [128, NT + 16, 4, 65], BF16), Buf("vb0")), (sb("b_vb1", [128, NT + 16, 4, 65], BF16), Buf("vb1"))]
        b_va = Buf("va")
        bstage = Ring(es, nc, "b_bst", [128, 14, 64], F32, 2)
        er = Ring(es, nc, "b_e", [128, 256], BF16, 3)
        ptr_ = Ring(es, nc, "b_pt", [128, 256], BF16, 6)
        oev = Ring(es, nc, "b_oev", [128, 260], F32, 2)
        orec = Ring(es, nc, "b_orec", [64, 4], F32, 2)
        ona = Ring(es, nc, "b_ona", [64, 4, 64], BF16, 2)
        ptp = Ring(es, nc, "b_ptp", [128, 8, 128], BF16, 2, psum=True)
        pss = Ring(es, nc, "b_pss", [128, 512], F32, 4, psum=True)
        pso_ = Ring(es, nc, "b_pso", [128, 512], F32, 2, psum=True)

        class _PSO:
            def next(self):
                t, b, i = pso_.next()
                return t[:, 0:260].rearrange("p (a b) -> p a b", b=65), b, i
        pso = _PSO()

        p.dma("sp", L("c1"), lambda e: e.dma_start(out=ident[:], in_=T["ident"]), pwrites=[b_const])
        p.dma("sp", L("c1"), lambda e: e.dma_start(out=maskd[:], in_=T["maskd"]), pwrites=[b_const])
        p.dma("sp", L("c1"), lambda e: e.dma_start(out=cmask[:], in_=T["cmask"]), pwrites=[b_const])
        for h in range(8):
            bt, bb, bi = bstage.next()
            p.dma("sp", L(f"bst{bi}"), lambda e, bt=bt, h=h: e.dma_start(out=bt[:], in_=T["biasx"][:, h]), writes=[bb])
            p.act(lambda e, bt=bt: e.activation(out=bt[:], in_=bt[:], func=AF.Exp), reads=[bb], writes=[bb])
            p.dve(lambda e, bt=bt, h=h: e.tensor_tensor(out=eb2[:, h], in0=bt[:], in1=cmask[:].unsqueeze(1).to_broadcast([128, 14, 64]), op=ALU.mult), reads=[bb, b_const], pwrites=[b_eb2])
        p.pool(lambda e: e.memset(kT[:, :, 0:64], 0.0), pwrites=[b_kT])
        p.pool(lambda e: e.memset(kT[:, :, S + 64:S + 128], 0.0), pwrites=[b_kT])

        groups = [("na", 0), ("dil", 0), ("na", 1), ("dil", 1), ("dil", 2)]
        if os.environ.get("KB_STOP") == "const":
            groups = []
        if os.environ.get("KB_GROUPS"):
            groups = [groups[int(c)] for c in os.environ["KB_GROUPS"]]
        tgl = [0]
        pgen = p_chunks(nc, p, T, es)
        def gparams(gi):
            kind, gx = groups[gi]
            if kind == "na":
                d = 1
                qc0, kc0, vc0 = 256 * gx, 512 + 256 * gx, 1024 + 256 * gx
            else:
                d = (1, 4, 16)[gx]
                qc0, kc0, vc0 = 1536 + 256 * gx, 2304 + 256 * gx, 3072 + 256 * gx
            Lr = S // d
            nseg = Lr // 128
            vb, b_vb = vbr[gi % 2]
            lvb = L(f"bvb{gi % 2}")
            view = qkv_s.rearrange("(m r) c -> r m c", r=d)
            return kind, gx, d, qc0, kc0, vc0, Lr, nseg, vb, b_vb, lvb, view

        def load_v(gi):
            kind, gx, d, qc0, kc0, vc0, Lr, nseg, vb, b_vb, lvb, view = gparams(gi)
            p.pool(lambda e, vb=vb: e.memset(vb[:], 0.0), writes=[b_vb])
            p.pool(lambda e, vb=vb: e.memset(vb[:, :, :, 64:65], 1.0), after=[b_vb], pwrites=[b_vb])
            if kind == "na":
                p.pool(lambda e: e.memset(va[:, :, :, 64:65], 1.0), writes=[b_va])
                for hh in range(4):
                    p.dma("sp", lvb, lambda e, hh=hh, vc0=vc0, vb=vb: e.dma_start(out=vb[:, 0:NT - 1, hh, 0:64], in_=qkv_s[64:S - 64, vc0 + 64 * hh:vc0 + 64 * hh + 64].rearrange("(n p) c -> p n c", p=128)), reads=[T["b_qkv"]], after=[b_vb], pwrites=[b_vb])
                for hh in range(4):
                    p.dma("sp", L("bva"), lambda e, hh=hh, vc0=vc0: e.dma_start(out=va[:, :, hh, 0:64], in_=qkv_s[:, vc0 + 64 * hh:vc0 + 64 * hh + 64].rearrange("(n p) c -> p n c", p=128)), reads=[T["b_qkv"]], after=[b_va], pwrites=[b_va])
            else:
                for r in range(d):
                    base = r * (nseg + 1)
                    c = vc0
                    if nseg - 1 >= 4:
                        for hh in range(4):
                            p.dma("sp", lvb, lambda e, r=r, base=base, c=c, hh=hh, view=view, Lr=Lr, nseg=nseg, vb=vb: e.dma_start(out=vb[:, base + 1:base + nseg, hh, 0:64], in_=view[r, 64:Lr - 64, c + 64 * hh:c + 64 * hh + 64].rearrange("(n p) c -> p n c", p=128)), reads=[T["b_qkv"]], after=[b_vb], pwrites=[b_vb])
                    else:
                        for n_ in range(nseg - 1):
                            p.dma("sp", lvb, lambda e, r=r, base=base, c=c, n_=n_, view=view, vb=vb: e.dma_start(out=vb[:, base + 1 + n_, :, 0:64], in_=view[r, 64 + 128 * n_:64 + 128 * n_ + 128, c:c + 256].rearrange("p (h c) -> p h c", h=4)), reads=[T["b_qkv"]], after=[b_vb], pwrites=[b_vb])
                    p.dma("sp", lvb, lambda e, r=r, base=base, c=c, view=view, vb=vb: e.dma_start(out=vb[64:128, base, :, 0:64], in_=view[r, 0:64, c:c + 256].rearrange("p (h c) -> p h c", h=4)), reads=[T["b_qkv"]], after=[b_vb], pwrites=[b_vb])
                    p.dma("sp", lvb, lambda e, r=r, base=base, c=c, view=view, Lr=Lr, nseg=nseg, vb=vb: e.dma_start(out=vb[0:64, base + nseg, :, 0:64], in_=view[r, Lr - 64:Lr, c:c + 256].rearrange("p (h c) -> p h c", h=4)), reads=[T["b_qkv"]], after=[b_vb], pwrites=[b_vb])

        for gi, (kind, gx) in enumerate(groups):
            if kind == "na":
                d = 1
                qc0, kc0, vc0 = 256 * gx, 512 + 256 * gx, 1024 + 256 * gx
            else:
                d = (1, 4, 16)[gx]
                qc0, kc0, vc0 = 1536 + 256 * gx, 2304 + 256 * gx, 3072 + 256 * gx
            Lr = S // d
            nseg = Lr // 128
            vb, b_vb = vbr[gi % 2]
            lvb = L(f"bvb{gi % 2}")
            view = qkv_s.rearrange("(m r) c -> r m c", r=d)
            for r in range(d):
                kwq = dict(writes=[b_qtok]) if r == 0 else dict(pwrites=[b_qtok])
                kwk = dict(writes=[b_ktok]) if r == 0 else dict(pwrites=[b_ktok])
                p.dma("sp", L("bq"), lambda e, r=r, view=view, qc0=qc0, nseg=nseg: e.dma_start(out=qtok[:, r * nseg:(r + 1) * nseg, :], in_=view[r, :, qc0:qc0 + 256].rearrange("(n p) c -> p n c", p=128)), reads=[T["b_qkv"]], **kwq)
                p.dma("sp", L("bk"), lambda e, r=r, view=view, kc0=kc0, nseg=nseg: e.dma_start(out=ktok[:, r * nseg:(r + 1) * nseg, :], in_=view[r, :, kc0:kc0 + 256].rearrange("(n p) c -> p n c", p=128)), reads=[T["b_qkv"]], **kwk)
            if gi == 0:
                load_v(0)
            if gi + 1 < len(groups):
                load_v(gi + 1)
            if os.environ.get("KB_STOP") == "loads":
                continue
            for n in range(NT):
                pt, ptb, _ = ptp.next()
                for pr in range(2):
                    kw = dict(writes=[ptb]) if pr == 0 else dict(pwrites=[ptb])
                    p.pe(lambda e, pt=pt, n=n, pr=pr: e.transpose(out=pt[:, pr, :], in_=qtok[:, n, 128 * pr:128 * pr + 128], identity=ident[:]), reads=[b_qtok, b_const], sig=False, **kw)
                for pr in range(2):
                    p.pe(lambda e, pt=pt, n=n, pr=pr: e.transpose(out=pt[:, 2 + pr, :], in_=ktok[:, n, 128 * pr:128 * pr + 128], identity=ident[:]), reads=[b_ktok, b_const], pwrites=[ptb], sig=(pr == 1))
                if n % 2 == 0:
                    p.dve(lambda e, pt=pt, n=n: e.tensor_copy(out=qT[:, :, 128 * n:128 * n + 128], in_=pt[:, 0:2, :]), reads=[ptb], pwrites=[b_qT])
                    p.dve(lambda e, pt=pt, n=n: e.tensor_copy(out=kT[:, :, 64 + 128 * n:64 + 128 * n + 128], in_=pt[:, 2:4, :]), reads=[ptb], pwrites=[b_kT])
                else:
                    p.act(lambda e, pt=pt, n=n: e.activation(out=qT[:, :, 128 * n:128 * n + 128], in_=pt[:, 0:2, :], func=AF.Copy), reads=[ptb], pwrites=[b_qT])
                    p.act(lambda e, pt=pt, n=n: e.activation(out=kT[:, :, 64 + 128 * n:64 + 128 * n + 128], in_=pt[:, 2:4, :], func=AF.Copy), reads=[ptb], pwrites=[b_kT])
            units = []
            if kind == "na":
                for r in range(64):
                    rs = min(max(r - 4, 0), 56)
                    rel = r - rs
                    st_ = {}
                    for j in range(4):
                        def s1(st_=st_, r=r, rs=rs, rel=rel, j=j, gx=gx):
                            pr, hh = j // 2, j % 2
                            hg = 4 * gx + j
                            ps, psb, _ = pss.next()
                            for jj in range(4):
                                k0 = 64 + 64 * (rs + 2 * jj)
                                kw = dict(writes=[psb]) if jj == 0 else dict(pwrites=[psb])
                                p.pe(lambda e, ps=ps, jj=jj, k0=k0, pr=pr, hh=hh, r=r: e.matmul(ps[:, 64 * jj:64 * jj + 64], lhsT=kT[64 * hh:64 * hh + 64, pr, k0:k0 + 128], rhs=qT[64 * hh:64 * hh + 64, pr, 64 * r:64 * r + 64], start=True, stop=True), reads=[b_kT, b_qT], sig=(jj == 3), **kw)
                            et, eb, _ = er.next()
                            p.act(lambda e, et=et, ps=ps: e.activation(out=et[:], in_=ps[:, 0:256], func=AF.Exp, scale=0.125), reads=[psb], writes=[eb])
                            ptt, ptb2, _ = ptr_.next()
                            eng = "dve" if tgl[0] % 2 == 0 else "pool"
                            tgl[0] += 1
                            p.op(eng, lambda e, ptt=ptt, et=et, hg=hg, rel=rel: e.tensor_tensor(out=ptt[:].rearrange("p (a b) -> p a b", b=64), in0=et[:].rearrange("p (a b) -> p a b", b=64), in1=eb2[:, hg, 7 - rel:14 - rel:2, :], op=ALU.mult), reads=[eb, b_eb2], writes=[ptb2])
                            st_[j] = (ptt, ptb2)

                        def s2(st_=st_, r=r, rs=rs, j=j, gx=gx):
                            if j == 0:
                                st_["po"] = pso.next()
                            po, pob, _ = st_["po"]
                            ptt, ptb2 = st_[j]
                            for jj in range(4):
                                row = rs + 2 * jj
                                if row % 2 == 0:
                                    vt, vbuf, ti = va, b_va, row // 2
                                else:
                                    vt, vbuf, ti = vb, b_vb, (row - 1) // 2
                                kw = dict(writes=[pob]) if (j == 0 and jj == 0) else dict(pwrites=[pob])
                                p.pe(lambda e, po=po, ptt=ptt, jj=jj, vt=vt, ti=ti, j=j: e.matmul(po[0:64, j, :], lhsT=ptt[:, 64 * jj:64 * jj + 64], rhs=vt[:, ti, j, :], start=(jj == 0), stop=(jj == 3)), reads=[ptb2, vbuf], sig=(j == 3 and jj == 3), **kw)
                            if j == 3:
                                rc, rcb, _ = orec.next()
                                p.dve(lambda e, rc=rc, po=po: e.reciprocal(out=rc[:], in_=po[0:64, :, 64]), reads=[pob], writes=[rcb])
                                ot, otb, oi = ona.next()
                                p.dve(lambda e, ot=ot, po=po, rc=rc: e.tensor_tensor(out=ot[:], in0=po[0:64, :, 0:64], in1=rc[:].unsqueeze(2).to_broadcast([64, 4, 64]), op=ALU.mult), reads=[pob, rcb], writes=[otb])
                                p.dma("pool", L(f"bo{oi}"), lambda e, ot=ot, r=r, gx=gx: e.dma_start(out=T["ona_s"][64 * r:64 * r + 64, 256 * gx:256 * gx + 256], in_=ot[:].rearrange("p a b -> p (a b)")), reads=[otb], pwrites=[T["b_ona"]])
                        units.append((s1, s2))
            else:
                oview = T["odil_s"][gx].rearrange("(m r) c -> r m c", r=d)
                for r in range(d):
                    base = r * (nseg + 1)
                    for u in range(nseg):
                        var = 0 if u == 0 else (2 if u == nseg - 1 else 1)
                        t0 = r * Lr + 128 * u
                        st_ = {}
                        for j in range(4):
                            def s1(st_=st_, t0=t0, var=var, j=j):
                                pr, hh = j // 2, j % 2
                                ps, psb, _ = pss.next()
                                for ab in range(2):
                                    kw = dict(writes=[psb]) if ab == 0 else dict(pwrites=[psb])
                                    p.pe(lambda e, ps=ps, ab=ab, t0=t0, pr=pr, hh=hh: e.matmul(ps[:, 128 * ab:128 * ab + 128], lhsT=kT[64 * hh:64 * hh + 64, pr, t0 + 128 * ab:t0 + 128 * ab + 128], rhs=qT[64 * hh:64 * hh + 64, pr, t0:t0 + 128], start=True, stop=True), reads=[b_kT, b_qT], sig=(ab == 1), **kw)
                                et, eb, _ = er.next()
                                p.act(lambda e, et=et, ps=ps: e.activation(out=et[:], in_=ps[:, 0:256], func=AF.Exp, scale=0.125), reads=[psb], writes=[eb])
                                ptt, ptb2, _ = ptr_.next()
                                eng = "dve" if tgl[0] % 2 == 0 else "pool"
                                tgl[0] += 1
                                p.op(eng, lambda e, ptt=ptt, et=et, var=var: e.tensor_tensor(out=ptt[:], in0=et[:], in1=maskd[:, var, :], op=ALU.mult), reads=[eb, b_const], writes=[ptb2])
                                st_[j] = (ptt, ptb2)

                            def s2(st_=st_, base=base, u=u, r=r, j=j, oview=oview):
                                if j == 0:
                                    st_["po"] = pso.next()
                                po, pob, _ = st_["po"]
                                ptt, ptb2 = st_[j]
                                for ab in range(2):
                                    kw = dict(writes=[pob]) if (j == 0 and ab == 0) else dict(pwrites=[pob])
                                    p.pe(lambda e, po=po, ptt=ptt, ab=ab, base=base, u=u, j=j, vb=vb: e.matmul(po[:, j, :], lhsT=ptt[:, 128 * ab:128 * ab + 128], rhs=vb[:, base + u + ab, j, :], start=(ab == 0), stop=(ab == 1)), reads=[ptb2, b_vb], sig=(j == 3 and ab == 1), **kw)
                                if j == 3:
                                    ot, otb, oi = oev.next()
                                    p.act(lambda e, ot=ot, po=po: e.activation(out=ot[:].rearrange("p (a b) -> p a b", b=65), in_=po, func=AF.Copy), reads=[pob], writes=[otb])
                                    p.dma("pool", L(f"bd{oi}"), lambda e, ot=ot, r=r, u=u, oview=oview: e.dma_start(out=oview[r, 128 * u:128 * u + 128, :], in_=ot[:]), reads=[otb], pwrites=[T["b_odil"]])
                            units.append((s1, s2))
            SKEW = 3
            for ui in range(len(units) + SKEW):
                if ui < len(units):
                    units[ui][0]()
                if ui >= SKEW:
                    units[ui - SKEW][1]()
                if ui % 7 == 0:
                    next(pgen, None)
        for _ in pgen:
            pass
        p.drain("sp")
        p.flush()


def load_weight_bf16(p, es, nc, name, src, rows, cols, stage, n0):
    kch = rows // 128
    wt = es.enter_context(nc.sbuf_tensor(name, [128, kch, cols], BF16))
    wb = Buf(name)
    casters = ("dve", "pool", "act")
    n = n0
    for k in range(kch):
        for c0 in range(0, cols, 1024):
            stt, sbf, si = stage.next()
            p.dma("sp", p.lane(f"wst{si}"), lambda e, stt=stt, k=k, c0=c0: e.dma_start(out=stt[:], in_=src[128 * k:128 * k + 128, c0:c0 + 1024]), writes=[sbf])
            eng = casters[n % 3]
            n += 1
            if eng == "act":
                p.act(lambda e, stt=stt, k=k, c0=c0: e.activation(out=wt[:, k, c0:c0 + 1024], in_=stt[:], func=AF.Copy), reads=[sbf], pwrites=[wb])
            else:
                p.op(eng, lambda e, stt=stt, k=k, c0=c0: e.tensor_copy(out=wt[:, k, c0:c0 + 1024], in_=stt[:]), reads=[sbf], pwrites=[wb])
    return wt, wb, n


def transposes(p, pt, ptb, src, srcb, ident, b_id, n, dst_off=0):
    for k in range(n):
        kw = dict(writes=[ptb]) if (k == 0 and dst_off == 0) else dict(pwrites=[ptb])
        p.pe(lambda e, k=k: e.transpose(out=pt[:, dst_off + k, :], in_=src[:, 128 * k:128 * k + 128], identity=ident[:]), reads=[srcb, b_id], sig=(k == n - 1), **kw)


def phase_c(nc, p, T):
    L = p.lane
    with ExitStack() as es:
        def sb(name, shape, dt):
            return es.enter_context(nc.sbuf_tensor(name, shape, dt))

        ident = sb("c_ident", [128, 128], BF16)
        b_id = Buf("c_const")
        p.dma("sp", L("c2"), lambda e: e.dma_start(out=ident[:], in_=T["ident"]), pwrites=[b_id])
        stage = Ring(es, nc, "c_stage", [128, 1024], F32, 3)
        n = 0
        wbn, b_wbn, n = load_weight_bf16(p, es, nc, "c_wbn", T["w_branch_na"], 512, 1024, stage, n)
        wbd, b_wbd, n = load_weight_bf16(p, es, nc, "c_wbd", T["w_branch_dil"], 256, 1024, stage, n)
        wo, b_wo, n = load_weight_bf16(p, es, nc, "c_wo", T["w_out"], 1024, 1024, stage, n)
        xr = Ring(es, nc, "c_x", [128, D], F32, 4)
        onr = Ring(es, nc, "c_on", [128, 512], BF16, 2)
        odr = Ring(es, nc, "c_od", [128, 3, 260], F32, 2)
        sgr = Ring(es, nc, "c_sg", [128, 2048], BF16, 3)
        ods = Ring(es, nc, "c_ods", [128, 260], F32, 2)
        rcr = Ring(es, nc, "c_rc", [128, 4], F32, 2)
        odn = Ring(es, nc, "c_odn", [128, 256], BF16, 2)
        aT = Ring(es, nc, "c_aT", [128, 6, 128], BF16, 2)
        m1 = Ring(es, nc, "c_m1", [128, D], F32, 2)
        m2 = Ring(es, nc, "c_m2", [128, D], F32, 2)
        mg = Ring(es, nc, "c_mg", [128, D], BF16, 2)
        mT = Ring(es, nc, "c_mT", [128, 8, 128], BF16, 2)
        x1r = Ring(es, nc, "c_x1", [128, D], F32, 2)
        ptp = Ring(es, nc, "c_ptp", [128, 8, 128], BF16, 2, psum=True)
        pa = Ring(es, nc, "c_pa", [128, 512], F32, 2, psum=True)
        pd = Ring(es, nc, "c_pd", [128, 512], F32, 2, psum=True)
        py = Ring(es, nc, "c_py", [128, 512], F32, 2, psum=True)

        def loads(i):
            xt, xb, xi = xr.next()
            p.dma("sp", L(f"cx{xi}"), lambda e: e.dma_start(out=xt[:], in_=T["x"][128 * i:128 * i + 128, :]), writes=[xb])
            ot, ob, oi = onr.next()
            p.dma("sp", L(f"con{oi}"), lambda e: e.dma_start(out=ot[:], in_=T["ona_s"][128 * i:128 * i + 128, :]), reads=[T["b_ona"]], writes=[ob])
            dt_, db, di = odr.next()
            p.dma("sp", L(f"cod{di}"), lambda e: e.dma_start(out=dt_[:], in_=T["odil_s"][:, 128 * i:128 * i + 128, :].rearrange("g p c -> p g c")), reads=[T["b_odil"]], writes=[db])
            st_, sb_, si = sgr.next()
            p.dma("sp", L(f"csg{si}"), lambda e: e.dma_start(out=st_[:], in_=T["sg_s"][128 * i:128 * i + 128, :]), reads=[T["b_sg"]], writes=[sb_])
            return (xt, xb, ot, ob, dt_, db, st_, sb_)

        q1b, q2 = [], []
        nxt = loads(0)
        for i in range(NT):
            xt, xb, ot, ob, dt_, db, sgt, sgb = nxt
            if i + 1 < NT:
                nxt = loads(i + 1)
            odt, odb, _ = ods.next()
            p.dve(lambda e, odt=odt, dt_=dt_: e.tensor_tensor(out=odt[:], in0=dt_[:, 0, :], in1=dt_[:, 1, :], op=ALU.add), reads=[db], writes=[odb])
            p.dve(lambda e, odt=odt, dt_=dt_: e.tensor_tensor(out=odt[:], in0=odt[:], in1=dt_[:, 2, :], op=ALU.add), reads=[db, odb], writes=[odb])
            rc, rcb, _ = rcr.next()
            od3 = odt[:].rearrange("p (a b) -> p a b", b=65)
            p.dve(lambda e, rc=rc, od3=od3: e.reciprocal(out=rc[:], in_=od3[:, :, 64]), reads=[odb], writes=[rcb])
            on_, onb, _ = odn.next()
            p.dve(lambda e, on_=on_, od3=od3, rc=rc: e.tensor_tensor(out=on_[:].rearrange("p (a b) -> p a b", b=64), in0=od3[:, :, 0:64], in1=rc[:].unsqueeze(2).to_broadcast([128, 4, 64]), op=ALU.mult), reads=[odb, rcb], writes=[onb])
            pt, ptb, _ = ptp.next()
            transposes(p, pt, ptb, ot, ob, ident, b_id, 4, 0)
            transposes(p, pt, ptb, on_, onb, ident, b_id, 2, 4)
            at, atb, _ = aT.next()
            p.act(lambda e, at=at, pt=pt: e.activation(out=at[:], in_=pt[:, 0:6, :], func=AF.Copy), reads=[ptb], writes=[atb])
            def s1b(i=i, xt=xt, xb=xb, at=at, atb=atb, sgt=sgt, sgb=sgb):
                pas, pds = [], []
                for half in range(2):
                    pat, pab, _ = pa.next()
                    for k in range(4):
                        kw = dict(writes=[pab]) if k == 0 else dict(pwrites=[pab])
                        p.pe(lambda e, pat=pat, at=at, k=k, half=half: e.matmul(pat[:], lhsT=at[:, k, :], rhs=wbn[:, k, 512 * half:512 * half + 512], start=(k == 0), stop=(k == 3)), reads=[atb, b_wbn], sig=(k == 3), **kw)
                    pas.append((pat, pab))
                for half in range(2):
                    pdt, pdb, _ = pd.next()
                    for k in range(2):
                        kw = dict(writes=[pdb]) if k == 0 else dict(pwrites=[pdb])
                        p.pe(lambda e, pdt=pdt, at=at, k=k, half=half: e.matmul(pdt[:], lhsT=at[:, 4 + k, :], rhs=wbd[:, k, 512 * half:512 * half + 512], start=(k == 0), stop=(k == 1)), reads=[atb, b_wbd], sig=(k == 1), **kw)
                    pds.append((pdt, pdb))
                m1t, m1b, _ = m1.next()
                m2t, m2b, _ = m2.next()
                for half in range(2):
                    pat, pab = pas[half]
                    pdt, pdb = pds[half]
                    p.dve(lambda e, m1t=m1t, pat=pat, sgt=sgt, half=half: e.tensor_tensor(out=m1t[:, 512 * half:512 * half + 512], in0=pat[:], in1=sgt[:, 512 * half:512 * half + 512], op=ALU.mult), reads=[pab, sgb], pwrites=[m1b])
                    p.dve(lambda e, m2t=m2t, pdt=pdt, sgt=sgt, half=half: e.tensor_tensor(out=m2t[:, 512 * half:512 * half + 512], in0=pdt[:], in1=sgt[:, 1024 + 512 * half:1024 + 512 * half + 512], op=ALU.mult), reads=[pdb, sgb], pwrites=[m2b])
                mgt, mgb, _ = mg.next()
                p.pool(lambda e, mgt=mgt, m1t=m1t, m2t=m2t: e.tensor_tensor(out=mgt[:], in0=m1t[:], in1=m2t[:], op=ALU.add), reads=[m1b, m2b], writes=[mgb])
                def stage2(i=i, xt=xt, xb=xb, mgt=mgt, mgb=mgb):
                    pt2, ptb2, _ = ptp.next()
                    transposes(p, pt2, ptb2, mgt, mgb, ident, b_id, 8, 0)
                    mTt, mTb, _ = mT.next()
                    p.act(lambda e, mTt=mTt, pt2=pt2: e.activation(out=mTt[:], in_=pt2[:], func=AF.Copy), reads=[ptb2], writes=[mTb])
                    x1t, x1b, x1i = x1r.next()
                    for half in range(2):
                        pyt, pyb, _ = py.next()
                        for k in range(8):
                            kw = dict(writes=[pyb]) if k == 0 else dict(pwrites=[pyb])
                            p.pe(lambda e, pyt=pyt, mTt=mTt, k=k, half=half: e.matmul(pyt[:], lhsT=mTt[:, k, :], rhs=wo[:, k, 512 * half:512 * half + 512], start=(k == 0), stop=(k == 7)), reads=[mTb, b_wo], sig=(k == 7), **kw)
                        p.dve(lambda e, x1t=x1t, pyt=pyt, xt=xt, half=half: e.tensor_tensor(out=x1t[:, 512 * half:512 * half + 512], in0=pyt[:], in1=xt[:, 512 * half:512 * half + 512], op=ALU.add), reads=[pyb, xb], pwrites=[x1b])
                    p.dma("pool", L(f"cst{x1i}"), lambda e, x1t=x1t, i=i: e.dma_start(out=T["x1_s"][128 * i:128 * i + 128, :], in_=x1t[:]), reads=[x1b], pwrites=[T["b_x1"]])
                return stage2
            q1b.append(s1b)
            if len(q1b) > 1:
                q2.append(q1b.pop(0)())
            if len(q2) > 1:
                q2.pop(0)()
        while q1b:
            q2.append(q1b.pop(0)())
        while q2:
            q2.pop(0)()
        p.drain("sp")
        p.flush()


def p_chunks(nc, p, T, es):
    L = p.lane
    sin = Ring(es, nc, "p_in", [128, 2, D], F32, 2)
    sout = Ring(es, nc, "p_out", [128, 2, D], BF16, 2)
    n = 0
    for j in range(64):
        for ti, tbl in enumerate((T["peer_expert_u"], T["peer_expert_v"])):
            it, ib, ii = sin.next()
            ot, ob, oi = sout.next()
            p.dma("sp", L(f"pi{ii}"), lambda e, it=it, tbl=tbl, j=j: e.dma_start(out=it[:], in_=tbl[256 * j:256 * j + 256, :].rearrange("(a q) c -> q a c", q=128)), writes=[ib])
            if n % 2 == 0:
                p.dve(lambda e, it=it, ot=ot: e.tensor_copy(out=ot[:], in_=it[:]), reads=[ib], writes=[ob])
            else:
                p.act(lambda e, it=it, ot=ot: e.activation(out=ot[:], in_=it[:], func=AF.Copy), reads=[ib], writes=[ob])
            n += 1
            p.dma("pool", L(f"po{oi}"), lambda e, ot=ot, ti=ti, j=j: e.dma_start(out=T["uv_s"][256 * j:256 * j + 256, 1024 * ti:1024 * ti + 1024].rearrange("(a q) c -> q a c", q=128), in_=ot[:]), reads=[ob], pwrites=[T["b_uv"]])
            yield


def phase_d(nc, p, T):
    L = p.lane
    U = T["peer_expert_u"]
    V = T["peer_expert_v"]
    ntiles = int(os.environ.get("KD_TILES", NT))
    with ExitStack() as es:
        def sb(name, shape, dt):
            return es.enter_context(nc.sbuf_tensor(name, shape, dt))

        ident = sb("d_ident", [128, 128], BF16)
        gffn = sb("d_gffn", [128, D], F32)
        gple = sb("d_gple", [128, D], F32)
        iota = sb("d_iota", [128, 16], F32)
        sk = sb("d_sk", [128, 2, 128], F32)
        skb = sb("d_skb", [128, 2, 128], BF16)
        kT = sb("d_kT", [128, 2, 128], BF16)
        b_c = Buf("d_const")
        b_sk = Buf("d_sk")
        b_kT = Buf("d_kT")
        p.dma("sp", L("c3"), lambda e: e.dma_start(out=ident[:], in_=T["ident"]), pwrites=[b_c])
        p.dma("sp", L("c3"), lambda e: e.dma_start(out=gffn[:], in_=T["norm_ffn"].partition_broadcast(128)), pwrites=[b_c])
        p.dma("sp", L("c3"), lambda e: e.dma_start(out=gple[:], in_=T["norm_ple"].partition_broadcast(128)), pwrites=[b_c])
        p.dma("sp", L("c3"), lambda e: e.dma_start(out=iota[:], in_=T["iota16"]), pwrites=[b_c])
        p.dma("sp", L("c3"), lambda e: e.dma_start(out=sk[:], in_=T["peer_sub_keys"].rearrange("s n c -> n s c")), pwrites=[b_c])
        NG = int(os.environ.get("KNG", 14))
        uvg = Ring(es, nc, "d_uvg", [128, 2 * D], BF16, NG)
        dgr = Ring(es, nc, "d_dg", [128, 128], BF16, 4)
        class _Stage:
            def __init__(self):
                self.i = -1

            def next(self):
                self.i = (self.i + 1) % 6
                return uvg.t[self.i][:].bitcast(F32), uvg.b[self.i], self.i
        stage = _Stage()
        a_bufs = [Buf(f"a{i}") for i in range(128)]
        w_bufs = [Buf(f"w{i}") for i in range(128)]
        n = 0
        wq, b_wq, n = load_weight_bf16(p, es, nc, "d_wq", T["peer_w_query"], 1024, 2048, stage, n)
        wpg, b_wpg, n = load_weight_bf16(p, es, nc, "d_wpg", T["w_ple_gate"], 1024, 1024, stage, n)
        wpl, b_wpl, n = load_weight_bf16(p, es, nc, "d_wpl", T["w_ple"], 256, 1024, stage, n)

        x1r = Ring(es, nc, "d_x1", [128, D], F32, 2)
        ppr = Ring(es, nc, "d_pp", [128, 256], F32, 2)
        junk = Ring(es, nc, "d_junk", [128, D], BF16, 1)
        st = Ring(es, nc, "d_st", [128, 8], F32, 2)
        h2r = Ring(es, nc, "d_h2", [128, D], F32, 1)
        junka = Ring(es, nc, "d_junka", [128, D], BF16, 1)
        hbr = Ring(es, nc, "d_hb", [128, D], BF16, 2)
        h3r = Ring(es, nc, "d_h3", [128, D], BF16, 1)
        prod = Ring(es, nc, "d_prod", [128, D], BF16, 3)
        junkb = Ring(es, nc, "d_junkb", [128, D], BF16, 1)
        hTr = Ring(es, nc, "d_hT", [128, 8, 128], BF16, 1)
        qpr = Ring(es, nc, "d_qp", [128, 16, 128], BF16, 1)
        scr = Ring(es, nc, "d_sc", [128, 16, 128], F32, 1)
        w1r = Ring(es, nc, "d_w1", [128, 128], F32, 4)
        cdr = Ring(es, nc, "d_cd", [128, 256], F32, 2)
        cd2r = Ring(es, nc, "d_cd2", [128, 256], F32, 2)
        dummy = sb("d_dummy", [128, 8], F32)
        CB = [[[Buf(f"cb{q_}_{h_}_{w_}") for w_ in range(3)] for h_ in range(8)] for q_ in range(2)]
        TK = []
        for q_ in range(2):
            TK.append((sb(f"d_tk{q_}", [128, 12, 128], F32), sb(f"d_tku{q_}", [128, 5, 128], U32), Buf(f"d_tk{q_}"), sb(f"d_sm{q_}", [128, 4, 8], F32), Buf(f"d_sm{q_}")))
        oh = scr.t[0][:].rearrange("p (h k) c -> p h k c", k=2).rearrange("p h k (a b) -> p h (k a) b", b=16)
        b_oh = scr.b[0]
        eidx = Ring(es, nc, "d_eidx", [128, 128], I32, 2)
        av = Ring(es, nc, "d_av", [128, 128], F32, 1)
        gav = Ring(es, nc, "d_gav", [128, 128], F32, 1)
        g3r = Ring(es, nc, "d_g3", [128, D], F32, 1)
        pbr = Ring(es, nc, "d_pb", [128, 256], BF16, 1)
        pTr = Ring(es, nc, "d_pT", [128, 2, 128], BF16, 1)
        outr = Ring(es, nc, "d_out", [128, D], F32, 1)
        ptp = Ring(es, nc, "d_ptp", [128, 8, 128], BF16, 1, psum=True)
        pq = Ring(es, nc, "d_pq", [128, 512], F32, 2, psum=True)
        psc = Ring(es, nc, "d_psc", [128, 512], F32, 2, psum=True)
        pyr = Ring(es, nc, "d_py", [128, 512], F32, 2, psum=True)

        if os.environ.get("KDEBUG"):
            print("phase D sbuf remaining", nc.sbuf_bytes_remaining)
        p.dve(lambda e: e.tensor_copy(out=skb[:], in_=sk[:]), reads=[b_c], writes=[b_sk])
        pt, ptb, _ = ptp.next()
        for s_ in range(2):
            kw = dict(writes=[ptb]) if s_ == 0 else dict(pwrites=[ptb])
            p.pe(lambda e, s_=s_, pt=pt: e.transpose(out=pt[:, s_, :], in_=skb[:, s_, :], identity=ident[:]), reads=[b_sk, b_c], sig=(s_ == 1), **kw)
        p.dve(lambda e, pt=pt: e.tensor_copy(out=kT[:], in_=pt[:, 0:2, :]), reads=[ptb], writes=[b_kT])

        def loads(i):
            xt, xb, xi = x1r.next()
            p.dma("sp", L(f"dx{xi}"), lambda e: e.dma_start(out=xt[:], in_=T["x1_s"][128 * i:128 * i + 128, :]), reads=[T["b_x1"]], writes=[xb])
            pp, ppb, pi = ppr.next()
            p.dma("sp", L(f"dp{pi}"), lambda e: e.dma_start(out=pp[:], in_=T["p"][128 * i:128 * i + 128, :]), writes=[ppb])
            return xt, xb, pp, ppb

        def rmsnorm(xt, xb, gain, out_t, out_b):
            jt, jb, _ = junka.next()
            stt, stb, _ = st.next()
            p.act(lambda e: e.activation(out=jt[:], in_=xt[:], func=AF.Square, accum_out=stt[:, 0:1]), reads=[xb], writes=[jb, stb])
            p.act(lambda e: e.activation(out=stt[:, 1:2], in_=stt[:, 0:1], func=AF.Sqrt, scale=1.0 / D, bias=EPS), reads=[stb], pwrites=[stb])
            p.dve(lambda e: e.reciprocal(out=stt[:, 2:3], in_=stt[:, 1:2]), reads=[stb], pwrites=[stb])
            p.dve(lambda e: e.scalar_tensor_tensor(out=out_t[:], in0=xt[:], scalar=stt[:, 2:3], in1=gain[:], op0=ALU.mult, op1=ALU.mult), reads=[xb, stb, b_c], writes=[out_b])

        def front(i):
            xt, xb, pp, ppb = loads(i)
            tk, tku, b_tk, sm, b_sm = TK[i % 2]
            yield
            lset = i % 4
            h2, h2b_, _ = h2r.next()
            rmsnorm(xt, xb, gffn, h2, h2b_)
            yield
            hb, hbb, _ = hbr.next()
            p.act(lambda e, hb=hb, h2=h2: e.activation(out=hb[:], in_=h2[:], func=AF.Copy), reads=[h2b_], writes=[hbb])
            yield
            pt, ptb, _ = ptp.next()
            transposes(p, pt, ptb, hb, hbb, ident, b_c, 8, 0)
            yield
            hT, hTb, _ = hTr.next()
            p.act(lambda e, hT=hT, pt=pt: e.activation(out=hT[:], in_=pt[:], func=AF.Copy), reads=[ptb], writes=[hTb])
            yield
            qp, qpb, _ = qpr.next()
            for rnd in range(4):
                pqt, pqb, _ = pq.next()
                for c4 in range(4):
                    cc = 4 * rnd + c4
                    for k in range(8):
                        kw = dict(writes=[pqb]) if (c4 == 0 and k == 0) else dict(pwrites=[pqb])
                        p.pe(lambda e, pqt=pqt, c4=c4, cc=cc, k=k, hT=hT: e.matmul(pqt[:, 128 * c4:128 * c4 + 128], lhsT=wq[:, k, 128 * cc:128 * cc + 128], rhs=hT[:, k, :], start=(k == 0), stop=(k == 7)), reads=[hTb, b_wq], sig=(c4 == 3 and k == 7), **kw)
                        yield
                if rnd % 2 == 0:
                    p.dve(lambda e, qp=qp, pqt=pqt, rnd=rnd: e.tensor_copy(out=qp[:, 4 * rnd:4 * rnd + 4, :], in_=pqt[:].rearrange("p (a b) -> p a b", b=128)), reads=[pqb], pwrites=[qpb])
                    yield
                else:
                    p.act(lambda e, qp=qp, pqt=pqt, rnd=rnd: e.activation(out=qp[:, 4 * rnd:4 * rnd + 4, :], in_=pqt[:].rearrange("p (a b) -> p a b", b=128), func=AF.Copy), reads=[pqb], pwrites=[qpb])
                    yield
            sc, scb, _ = scr.next()
            for rnd in range(4):
                pst, psb, _ = psc.next()
                for c4 in range(4):
                    cc = 4 * rnd + c4
                    kw = dict(writes=[psb]) if c4 == 0 else dict(pwrites=[psb])
                    p.pe(lambda e, pst=pst, c4=c4, cc=cc, qp=qp: e.matmul(pst[:, 128 * c4:128 * c4 + 128], lhsT=qp[:, cc, :], rhs=kT[:, cc % 2, :], start=True, stop=True), reads=[qpb, b_kT], sig=(c4 == 3), **kw)
                    yield
                if rnd % 2 == 0:
                    p.act(lambda e, sc=sc, pst=pst, rnd=rnd: e.activation(out=sc[:, 4 * rnd:4 * rnd + 4, :], in_=pst[:].rearrange("p (a b) -> p a b", b=128), func=AF.Copy), reads=[psb], pwrites=[scb])
                    yield
                else:
                    p.dve(lambda e, sc=sc, pst=pst, rnd=rnd: e.tensor_copy(out=sc[:, 4 * rnd:4 * rnd + 4, :], in_=pst[:].rearrange("p (a b) -> p a b", b=128)), reads=[psb], pwrites=[scb])
                    yield
            def top16_ops(src_ap, srcb, vals, idxs, wring, cb):
                wt, wb_, _ = wring.next()
                return [
                    lambda: p.dve(lambda e: e.max(out=vals[:, 0:8], in_=src_ap), reads=[srcb], pwrites=[cb]),
                    lambda: p.dve(lambda e: e.max_index(out=idxs[:, 0:8], in_max=vals[:, 0:8], in_values=src_ap), reads=[srcb, cb], pwrites=[cb]),
                    lambda: p.dve(lambda e: e.match_replace(out=wt[:], in_to_replace=vals[:, 0:8], in_values=src_ap, imm_value=-1e30), reads=[srcb, cb], writes=[wb_]),
                    lambda: p.dve(lambda e: e.max(out=vals[:, 8:16], in_=wt[:]), reads=[wb_], pwrites=[cb]),
                    lambda: p.dve(lambda e: e.max_index(out=idxs[:, 8:16], in_max=vals[:, 8:16], in_values=wt[:]), reads=[wb_, cb], pwrites=[cb]),
                ]

            chain_bufs = []
            prev2 = None
            for hh in range(9):
                chains = []
                if hh < 8:
                    cb1, cb2 = CB[i % 2][hh][0], CB[i % 2][hh][1]
                    chains.append(top16_ops(sc[:, 2 * hh, :], scb, tk[:, 0, 16 * hh:16 * hh + 16], tku[:, 0, 16 * hh:16 * hh + 16], w1r, cb1))
                    chains.append(top16_ops(sc[:, 2 * hh + 1, :], scb, tk[:, 1, 16 * hh:16 * hh + 16], tku[:, 1, 16 * hh:16 * hh + 16], w1r, cb2))
                if prev2 is not None:
                    chains.append(prev2)
                for k_ in range(5):
                    for ch in chains:
                        ch[k_]()
                    yield "dve"
                if hh < 8:
                    cd, cdb, _ = cdr.next()
                    p.dve(lambda e, cd=cd, hh=hh: e.tensor_tensor(out=cd[:].rearrange("p (a b) -> p a b", b=16), in0=tk[:, 0, 16 * hh:16 * hh + 16].unsqueeze(2).to_broadcast([128, 16, 16]), in1=tk[:, 1, 16 * hh:16 * hh + 16].unsqueeze(1).to_broadcast([128, 16, 16]), op=ALU.add), reads=[cb1, cb2], writes=[cdb])
                    cb3 = CB[i % 2][hh][2]
                    prev2 = top16_ops(cd[:], cdb, tk[:, 2, 16 * hh:16 * hh + 16], tku[:, 2, 16 * hh:16 * hh + 16], cd2r, cb3)
                    chain_bufs += [cb1, cb2, cb3]
                    yield "dve"
                else:
                    prev2 = None
            p.dve(lambda e: e.memset(dummy[:, 0:1], 0.0), reads=chain_bufs, pwrites=[b_tk])
            yield "dve"
            p.dve(lambda e: e.tensor_single_scalar(out=tku[:, 3, :], in_=tku[:, 2, :], scalar=4, op=ALU.logical_shift_right), reads=[b_tk], pwrites=[b_tk])
            yield "dve"
            p.dve(lambda e: e.tensor_single_scalar(out=tku[:, 4, :], in_=tku[:, 2, :], scalar=15, op=ALU.bitwise_and), reads=[b_tk], pwrites=[b_tk])
            yield "dve"
            p.dve(lambda e: e.tensor_copy(out=tk[:, 3:5, :], in_=tku[:, 0:2, :]), reads=[b_tk], pwrites=[b_tk])
            yield "dve"
            p.dve(lambda e: e.tensor_copy(out=tk[:, 5:7, :], in_=tku[:, 3:5, :]), reads=[b_tk], pwrites=[b_tk])
            yield "dve"
            for w in range(2):
                sel = tk[:, 5 + w, :].rearrange("p (h k) -> p h k", k=16).unsqueeze(3).to_broadcast([128, 8, 16, 16])
                tab = tk[:, 3 + w, :].rearrange("p (h a) -> p h a", a=16).unsqueeze(2).to_broadcast([128, 8, 16, 16])
                io = iota[:].unsqueeze(1).unsqueeze(1).to_broadcast([128, 8, 16, 16])
                p.dve(lambda e, sel=sel, io=io: e.tensor_tensor(out=oh, in0=sel, in1=io, op=ALU.is_equal), reads=[b_tk, b_c], writes=[b_oh])
                yield "dve"
                p.dve(lambda e, tab=tab: e.tensor_tensor(out=oh, in0=oh, in1=tab, op=ALU.mult), reads=[b_tk, b_oh], writes=[b_oh])
                yield "dve"
                p.dve(lambda e, w=w: e.tensor_reduce(out=tk[:, 7 + w, :], in_=oh.rearrange("p h k a -> p (h k) a"), axis=AX.X, op=ALU.add), reads=[b_oh], pwrites=[b_tk])
                yield "dve"
            p.dve(lambda e: e.scalar_tensor_tensor(out=tk[:, 11, :], in0=tk[:, 7, :], scalar=128.0, in1=tk[:, 8, :], op0=ALU.mult, op1=ALU.add), reads=[b_tk], pwrites=[b_tk])
            yield "dve"
            ei, eib, _ = eidx.next()
            p.dve(lambda e, ei=ei: e.tensor_copy(out=ei[:], in_=tk[:, 11, :]), reads=[b_tk], writes=[eib])
            yield "dve"
            sc3 = tk[:, 2, :].rearrange("p (h k) -> p h k", k=16)
            p.dve(lambda e, sc3=sc3: e.tensor_tensor(out=tk[:, 9, :].rearrange("p (h k) -> p h k", k=16), in0=sc3, in1=sc3[:, :, 0:1].to_broadcast([128, 8, 16]), op=ALU.subtract), reads=[b_tk], pwrites=[b_tk])
            yield "dve"
            p.act(lambda e: e.activation(out=tk[:, 9, :], in_=tk[:, 9, :], func=AF.Exp), reads=[b_tk], pwrites=[b_tk])
            yield "dve"
            p.dve(lambda e: e.tensor_reduce(out=sm[:, 0, :], in_=tk[:, 9, :].rearrange("p (h k) -> p h k", k=16), axis=AX.X, op=ALU.add), reads=[b_tk], writes=[b_sm])
            yield "dve"
            p.dve(lambda e: e.reciprocal(out=sm[:, 1, :], in_=sm[:, 0, :]), reads=[b_sm], pwrites=[b_sm])
            yield "dve"
            p.dve(lambda e: e.tensor_tensor(out=tk[:, 10, :].rearrange("p (h k) -> p h k", k=16), in0=tk[:, 9, :].rearrange("p (h k) -> p h k", k=16), in1=sm[:, 1, :].unsqueeze(2).to_broadcast([128, 8, 16]), op=ALU.mult), reads=[b_tk, b_sm], pwrites=[b_tk])
            yield "dve"

            F[i] = dict(xt=xt, xb=xb, pp=pp, ppb=ppb, h2=h2, h2b_=h2b_, ei=ei, eib=eib, tk=tk, b_tk=b_tk, hb2=hb, hb2b=hbb)

        F = {}
        for _ in front(0):
            pass
        for i in range(ntiles):
            gen = front(i + 1) if i + 1 < ntiles else iter(())
            gphase = [0]
            fs = F.pop(i)
            xt, xb, pp, ppb, h2, h2b_, ei, eib, tk, b_tk, hb2, hb2b = (fs[k] for k in ("xt", "xb", "pp", "ppb", "h2", "h2b_", "ei", "eib", "tk", "b_tk", "hb2", "hb2b"))
            lset = i % 4
            a_t, _, _ = av.next()
            ga_t, _, _ = gav.next()
            py0, py0b, _ = pyr.next()
            py1, py1b, _ = pyr.next()
            slots = {}
            SK = 2
            for s_ in range(128 + SK):
                if s_ < 128:
                    gt, gb, gi = uvg.next()
                    slots[s_] = (gt, gb)
                    p.dma("pool", L(f"dg{gi}_{lset % 2}"), lambda e, gt=gt, s_=s_, ei=ei: e.indirect_dma_start(out=gt[:], out_offset=None, in_=T["uv_s"], in_offset=bass.IndirectOffsetOnAxis(ap=ei[:, s_:s_ + 1], axis=0)), reads=[eib, T["b_uv"]], writes=[gb])
                    pr_, prb, _ = prod.next()
                    p.dve(lambda e, pr_=pr_, gt=gt, hb2=hb2: e.tensor_tensor(out=pr_[:], in0=gt[:, 0:D], in1=hb2[:], op=ALU.mult), reads=[gb, hb2b], writes=[prb])
                    j2, j2b, _ = junkb.next()
                    p.act(lambda e, j2=j2, pr_=pr_, s_=s_, a_t=a_t: e.activation(out=j2[:], in_=pr_[:], func=AF.Copy, accum_out=a_t[:, s_:s_ + 1]), reads=[prb], writes=[j2b, a_bufs[s_]])
                    p.act(lambda e, ga_t=ga_t, a_t=a_t, s_=s_: e.activation(out=ga_t[:, s_:s_ + 1], in_=a_t[:, s_:s_ + 1], func=AF.Gelu), reads=[a_bufs[s_]], writes=[w_bufs[s_]])
                if s_ >= SK:
                    z = s_ - SK
                    gt, gb = slots.pop(z)
                    dg, dgb, _ = dgr.next()
                    p.dve(lambda e, dg=dg, ga_t=ga_t, z=z, tk=tk: e.tensor_scalar(out=dg[:], in0=ident[:], scalar1=ga_t[:, z:z + 1], scalar2=tk[:, 10, z:z + 1], op0=ALU.mult, op1=ALU.mult), reads=[w_bufs[z], b_tk, b_c], writes=[dgb])
                    for half, (pyt, pyb) in enumerate(((py0, py0b), (py1, py1b))):
                        kw = dict(writes=[pyb]) if z == 0 else dict(pwrites=[pyb])
                        p.pe(lambda e, pyt=pyt, dg=dg, gt=gt, half=half, z=z: e.matmul(pyt[:], lhsT=dg[:], rhs=gt[:, D + 512 * half:D + 512 * half + 512], start=(z == 0), stop=(z == 127)), reads=[dgb, gb], sig=(half == 1), **kw)
                if gphase[0] == 0:
                    for _q in range(6):
                        if next(gen, None) == "dve":
                            gphase[0] = 1
                            break
                elif s_ % 4 != 3:
                    next(gen, None)
            for _ in gen:
                pass
            x2, x2b = xt, xb
            for half, (pyt, pyb) in enumerate(((py0, py0b), (py1, py1b))):
                p.dve(lambda e, x2=x2, pyt=pyt, xt=xt, half=half: e.tensor_tensor(out=x2[:, 512 * half:512 * half + 512], in0=pyt[:], in1=xt[:, 512 * half:512 * half + 512], op=ALU.add), reads=[pyb, xb], writes=[xb])
            h3, h3b, _ = h3r.next()
            rmsnorm(x2, x2b, gple, h3, h3b)
            pt, ptb, _ = ptp.next()
            transposes(p, pt, ptb, h3, h3b, ident, b_c, 8, 0)
            hT3, hT3b, _ = hTr.next()
            p.act(lambda e, hT3=hT3, pt=pt: e.activation(out=hT3[:], in_=pt[:], func=AF.Copy), reads=[ptb], writes=[hT3b])
            g3, g3b, _ = g3r.next()
            for half in range(2):
                pqt, pqb, _ = pq.next()
                for k in range(8):
                    kw = dict(writes=[pqb]) if k == 0 else dict(pwrites=[pqb])
                    p.pe(lambda e, pqt=pqt, hT3=hT3, k=k, half=half: e.matmul(pqt[:], lhsT=hT3[:, k, :], rhs=wpg[:, k, 512 * half:512 * half + 512], start=(k == 0), stop=(k == 7)), reads=[hT3b, b_wpg], sig=(k == 7), **kw)
                p.act(lambda e, g3=g3, pqt=pqt, half=half: e.activation(out=g3[:, 512 * half:512 * half + 512], in_=pqt[:], func=AF.Sigmoid), reads=[pqb], pwrites=[g3b])
            pb, pbb, _ = pbr.next()
            p.dve(lambda e, pb=pb, pp=pp: e.tensor_copy(out=pb[:], in_=pp[:]), reads=[ppb], writes=[pbb])
            pt, ptb, _ = ptp.next()
            transposes(p, pt, ptb, pb, pbb, ident, b_c, 2, 0)
            pT, pTb, _ = pTr.next()
            p.dve(lambda e, pT=pT, pt=pt: e.tensor_copy(out=pT[:], in_=pt[:, 0:2, :]), reads=[ptb], writes=[pTb])
            ot, otb, oi = outr.next()
            t3, t3b = ot, otb
            for half in range(2):
                pst, psb, _ = psc.next()
                for k in range(2):
                    kw = dict(writes=[psb]) if k == 0 else dict(pwrites=[psb])
                    p.pe(lambda e, pst=pst, pT=pT, k=k, half=half: e.matmul(pst[:], lhsT=pT[:, k, :], rhs=wpl[:, k, 512 * half:512 * half + 512], start=(k == 0), stop=(k == 1)), reads=[pTb, b_wpl], sig=(k == 1), **kw)
                p.dve(lambda e, t3=t3, pst=pst, g3=g3, half=half: e.tensor_tensor(out=t3[:, 512 * half:512 * half + 512], in0=pst[:], in1=g3[:, 512 * half:512 * half + 512], op=ALU.mult), reads=[psb, g3b], pwrites=[t3b])
            p.dve(lambda e, ot=ot, t3=t3, x2=x2: e.tensor_tensor(out=ot[:], in0=t3[:], in1=x2[:], op=ALU.add), reads=[t3b, x2b], writes=[otb])
            p.dma("sp", L(f"do{oi}"), lambda e, ot=ot, i=i: e.dma_start(out=T["out"][128 * i:128 * i + 128, :], in_=ot[:]), reads=[otb], pwrites=[T["b_out"]])
        p.drain("sp")
        p.flush()


def _rot_table():
    half = 8
    inv_freq = np.power(np.float32(500000.0), -np.arange(half, dtype=np.float32) * np.float32(2.0) / np.float32(16)).astype(np.float32)
    ang = np.arange(S, dtype=np.float32)[:, None] * inv_freq[None, :]
    return np.concatenate([np.cos(ang), np.sin(ang)], axis=1).astype(np.float32)


def build(debug=False):
    nc = bass.Bass("TRN2", target_bir_lowering=False)
    T = {}

    def din(name, shape, dt):
        T[name] = nc.dram_tensor(name, list(shape), dt, kind="ExternalInput").ap()

    din("x", [S, D], F32)
    din("p", [S, 256], F32)
    din("norm_mix", [1, D], F32)
    din("w_in", [D, INW], F32)
    din("qk_norm_na", [1, 2, 64], F32)
    din("qk_norm_dil", [1, 2, 64], F32)
    din("w_branch_na", [512, D], F32)
    din("w_branch_dil", [256, D], F32)
    din("w_out", [D, D], F32)
    din("norm_ffn", [1, D], F32)
    din("peer_w_query", [D, 2048], F32)
    din("peer_sub_keys", [2, 128, 128], F32)
    din("peer_expert_u", [16384, D], F32)
    din("peer_expert_v", [16384, D], F32)
    din("norm_ple", [1, D], F32)
    din("w_ple_gate", [D, D], F32)
    din("w_ple", [256, D], F32)
    din("ident", [128, 128], BF16)
    din("cs", [S, 16], F32)
    din("maskd", [128, 3, 256], BF16)
    din("cmask", [128, 64], F32)
    din("biasx", [128, 8, 14, 64], F32)
    din("iota16", [128, 16], F32)
    skind = "ExternalOutput" if debug else "Internal"
    T["qkv_s"] = nc.dram_tensor("qkv_s", [S, QKVW], BF16, kind=skind).ap()
    T["sg_s"] = nc.dram_tensor("sg_s", [S, 2048], BF16, kind=skind).ap()
    T["ona_s"] = nc.dram_tensor("ona_s", [S, 512], BF16, kind=skind).ap()
    T["odil_s"] = nc.dram_tensor("odil_s", [3, S, 260], F32, kind=skind).ap()
    T["x1_s"] = nc.dram_tensor("x1_s", [S, D], F32, kind=skind).ap()
    T["uv_s"] = nc.dram_tensor("uv_s", [16384, 2 * D], BF16, kind="Internal").ap()
    T["out"] = nc.dram_tensor("out", [S, D], F32, kind="ExternalOutput").ap()
    for k in ("qkv", "sg", "ona", "odil", "x1", "out", "uv"):
        T["b_" + k] = Buf(k + "_s")
    p = Prog(nc)
    ph = os.environ.get("KPH", "pabcd")
    if "a" in ph:
        phase_a(nc, p, T)
    if "b" in ph:
        phase_b(nc, p, T)
    if "c" in ph:
        phase_c(nc, p, T)
    if "d" in ph:
        phase_d(nc, p, T)
    p.drain("sp")
    p.flush()
    if os.environ.get("KDEBUG"):
        print("ops", p.nops, "sems", len(p.sems))
    return nc


def _masks():
    i = np.arange(128)[:, None]
    j = np.arange(128)[None, :]
    A = (j <= i)
    B = (j >= i)
    m = np.zeros((128, 3, 256), np.float32)
    m[:, 0, :128] = A & (i >= 64)
    m[:, 0, 128:] = B
    m[:, 1, :128] = A
    m[:, 1, 128:] = B
    m[:, 2, :128] = A
    m[:, 2, 128:] = B & (i < 64)
    kc = np.arange(64)[:, None]
    c = np.arange(64)[None, :]
    cs = np.clip(c - 8, 0, 48)
    cm = ((kc >= cs) & (kc < cs + 16)).astype(np.float32)
    cm = np.concatenate([cm, cm], axis=0)
    return m.astype(ml_dtypes.bfloat16), cm


def _biasx(rpb):
    kc = np.arange(64)[:, None]
    c = np.arange(64)[None, :]
    dc = np.clip(kc - c + 15, 0, 30)
    out = np.empty((2, 64, 8, 14, 64), np.float32)
    for a in range(2):
        for dr0 in range(14):
            out[a, :, :, dr0, :] = np.transpose(rpb[:, dr0 + a][:, dc], (1, 0, 2))
    return np.ascontiguousarray(out.reshape(128, 8, 14, 64))


_SHARED = None


def host_shared(inputs):
    f = lambda k: np.ascontiguousarray(np.asarray(inputs[k], np.float32))
    m = {
        "norm_mix": f("norm_mix"),
        "w_in": f("w_in")[0],
        "qk_norm_na": f("qk_norm_na"),
        "qk_norm_dil": f("qk_norm_dil"),
        "w_branch_na": f("w_branch_na")[0],
        "w_branch_dil": f("w_branch_dil")[0],
        "w_out": f("w_out")[0],
        "norm_ffn": f("norm_ffn"),
        "peer_w_query": f("peer_w_query")[0],
        "peer_sub_keys": f("peer_sub_keys")[0],
        "peer_expert_u": f("peer_expert_u")[0],
        "peer_expert_v": f("peer_expert_v")[0],
        "norm_ple": f("norm_ple"),
        "w_ple_gate": f("w_ple_gate")[0],
        "w_ple": f("w_ple")[0],
        "ident": np.eye(128).astype(ml_dtypes.bfloat16),
        "cs": _rot_table(),
        "maskd": _masks()[0],
        "cmask": _masks()[1],
        "biasx": _biasx(np.asarray(inputs["na_rel_bias"], np.float32)[0]),
        "iota16": np.tile(np.arange(16, dtype=np.float32), (128, 1)),
    }
    return m


def host_inputs(inputs, b, shared=None):
    m = dict(shared if shared is not None else host_shared(inputs))
    m["x"] = np.ascontiguousarray(np.asarray(inputs["x"], np.float32)[b])
    m["p"] = np.ascontiguousarray(np.asarray(inputs["p"], np.float32)[0, b])
    return m


def kernel(**inputs):
    nc = build()
    shared = host_shared(inputs)
    nb = np.asarray(inputs["x"]).shape[0]
    in_maps = [host_inputs(inputs, b, shared) for b in range(nb)]
    res = run_bass_kernel_spmd(nc, in_maps, core_ids=list(range(nb)))
    return np.stack([np.asarray(r["out"], np.float32) for r in res.results], axis=0)
```
